# Optimizing a Trainium2 kernel written in Bass

```python
import math
import jax, jax.numpy as jnp
from jax import lax
import numpy as np

D_MODEL = 1024
BATCH = 8
SEQ = 2048
DEPTH = 4

HEAD_DIM = 64
DIL_GROUPS = ((128, 1), (512, 4), (2048, 16))
N_DIL_GROUPS = 3
HEADS_PER_GROUP = 8
DIL_WIDTH = N_DIL_GROUPS * HEADS_PER_GROUP * HEAD_DIM
DIL_OUT = HEADS_PER_GROUP * HEAD_DIM
DIFF_HEADS = D_MODEL // (2 * HEAD_DIM)
DIFF_QK = DIFF_HEADS * 2 * HEAD_DIM
DIFF_V = DIFF_HEADS * 2 * HEAD_DIM
BLK = 128
NUM_BUCKETS = 32
MAX_DISTANCE = 2048
N_BIAS_HEADS = N_DIL_GROUPS * HEADS_PER_GROUP + DIFF_HEADS
IN_WIDTH = 3 * DIL_WIDTH + 2 * DIFF_QK + DIFF_V + 2 * D_MODEL
PEER_HEADS = 8
PEER_DQ = 256
PEER_DK = PEER_DQ // 2
N_KEYS = 128
N_EXPERTS = N_KEYS * N_KEYS
PEER_TOPK = 16
PEER_CHUNK = 128
NORM_EPS = 1e-6
SUBLN_EPS = 1e-5
NEG_INF = -1e30

kernel_name = 'hybrid_dilated_diff_peer_block'


def rmsnorm(x, g, eps=NORM_EPS):
    xf = x.astype(jnp.float32)
    y = xf * lax.rsqrt(jnp.mean(xf * xf, axis=-1, keepdims=True) + eps)
    return (y * g.astype(jnp.float32)).astype(x.dtype)


def t5_bucket(dist):
    max_exact = NUM_BUCKETS // 2
    d = jnp.maximum(dist, 0)
    ratio = jnp.log(jnp.maximum(d, 1).astype(jnp.float32) / max_exact) / math.log(MAX_DISTANCE / max_exact)
    large = jnp.minimum(max_exact + (ratio * (NUM_BUCKETS - max_exact)).astype(jnp.int32), NUM_BUCKETS - 1)
    return jnp.where(d < max_exact, d, large)


def dilated_group(q, k, v, bias_tab, n_back, r):
    b, s, h, dh = q.shape
    L = s // r
    Lp = -(-L // BLK) * BLK
    nb = Lp // BLK

    def strided(a):
        a = a.reshape(b, L, r, h, dh).transpose(0, 2, 1, 3, 4)
        a = jnp.pad(a, ((0, 0), (0, 0), (0, Lp - L), (0, 0), (0, 0)))
        return a.reshape(b, r, nb, BLK, h, dh)

    def with_prev(a):
        prev = jnp.pad(a, ((0, 0), (0, 0), (1, 0), (0, 0), (0, 0), (0, 0)))[:, :, :nb]
        return jnp.concatenate([prev, a], axis=3)

    qs, ks, vs = strided(q), strided(k), strided(v)
    kw, vw = with_prev(ks), with_prev(vs)
    qi = jnp.arange(BLK)[:, None]
    kj = jnp.arange(2 * BLK)[None, :]
    delta = qi + BLK - kj
    blk = jnp.arange(nb)[:, None, None]
    valid = (delta >= 0) & (delta <= n_back) & ((blk > 0) | (kj >= BLK))
    bias = bias_tab[t5_bucket(delta * r)].astype(jnp.float32).transpose(2, 0, 1)
    logits = jnp.einsum('bmnqhd,bmnkhd->bmnhqk', qs, kw).astype(jnp.float32) * (dh ** -0.5) + bias
    logits = jnp.where(valid[:, None], logits, NEG_INF)
    mx = jnp.max(logits, axis=-1, keepdims=True)
    p = jnp.exp(logits - mx)
    den = jnp.sum(p, axis=-1, keepdims=True)
    o = jnp.einsum('bmnhqk,bmnkhd->bmnqhd', p / den, vw.astype(jnp.float32))
    lse = (mx + jnp.log(den))[..., 0].transpose(0, 1, 2, 4, 3)

    def unstrided(a):
        a = a.reshape((b, r, Lp) + a.shape[4:])[:, :, :L]
        a = jnp.swapaxes(a, 1, 2)
        return a.reshape((b, s) + a.shape[3:])

    return unstrided(o), unstrided(lse)


def diff_attention(q, k, v, bias_tab, lam, subln_g, lam_init):
    b, s, h, _, dh = q.shape
    nb = s // BLK
    kpos = jnp.arange(s)
    qblocks = jnp.moveaxis(q.reshape(b, nb, BLK, h, 2, dh), 1, 0)
    vf = v.astype(jnp.float32)

    def one_block(args):
        qb, n = args
        qpos = n * BLK + jnp.arange(BLK)
        dist = qpos[:, None] - kpos[None, :]
        bias = bias_tab[t5_bucket(dist)].astype(jnp.float32).transpose(2, 0, 1)
        logits = jnp.einsum('bqhtd,bkhtd->bthqk', qb, k).astype(jnp.float32) * (dh ** -0.5) + bias
        logits = jnp.where(dist >= 0, logits, NEG_INF)
        att = jax.nn.softmax(logits, axis=-1)
        w = att[:, 0] - lam * att[:, 1]
        return jnp.einsum('bhqk,bkhd->bqhd', w, vf)

    o = lax.map(one_block, (qblocks, jnp.arange(nb)))
    o = jnp.moveaxis(o, 0, 1).reshape(b, s, h, 2 * dh)
    o = rmsnorm(o, subln_g, SUBLN_EPS) * (1.0 - lam_init)
    return o.reshape(b, s, h * 2 * dh).astype(q.dtype)


def token_mixer(h, w_in, w_pa, w_pb, w_o, lq1, lk1, lq2, lk2, subln_g, rel_bias, lam_init):
    b, s, _ = h.shape
    proj = h @ w_in
    sizes = [DIL_WIDTH, DIL_WIDTH, DIL_WIDTH, DIFF_QK, DIFF_QK, DIFF_V, D_MODEL]
    cuts = [int(i) for i in np.cumsum(sizes)]
    qa, ka, va, qb, kb, vb, ga, gb = jnp.split(proj, cuts, axis=-1)
    shp_a = (b, s, N_DIL_GROUPS, HEADS_PER_GROUP, HEAD_DIM)
    qa, ka, va = qa.reshape(shp_a), ka.reshape(shp_a), va.reshape(shp_a)
    outs, lses = [], []
    for g, (window, dil) in enumerate(DIL_GROUPS):
        tab = rel_bias[:, g * HEADS_PER_GROUP:(g + 1) * HEADS_PER_GROUP]
        o, lse = dilated_group(qa[:, :, g], ka[:, :, g], va[:, :, g], tab, window // dil, dil)
        outs.append(o)
        lses.append(lse)
    wts = jax.nn.softmax(jnp.stack(lses, 0), axis=0)
    oa = jnp.sum(wts[..., None] * jnp.stack(outs, 0), axis=0).reshape(b, s, DIL_OUT).astype(h.dtype)
    lam = jnp.exp(jnp.sum(lq1 * lk1)) - jnp.exp(jnp.sum(lq2 * lk2)) + lam_init
    ob = diff_attention(qb.reshape(b, s, DIFF_HEADS, 2, HEAD_DIM), kb.reshape(b, s, DIFF_HEADS, 2, HEAD_DIM),
                        vb.reshape(b, s, DIFF_HEADS, 2 * HEAD_DIM), rel_bias[:, N_DIL_GROUPS * HEADS_PER_GROUP:],
                        lam, subln_g, lam_init)
    merged = jax.nn.sigmoid(ga) * (oa @ w_pa) + jax.nn.sigmoid(gb) * (ob @ w_pb)
    return merged @ w_o


def peer(h, w_q, sub_keys, u_tab, v_tab):
    b, s, d = h.shape
    t = b * s
    xt = h.reshape(t, d)
    q = (xt @ w_q).reshape(t, PEER_HEADS, 2, PEER_DK)
    sc = jnp.einsum('thpd,pkd->thpk', q, sub_keys).astype(jnp.float32)
    top_s, top_i = lax.top_k(sc, PEER_TOPK)
    cand_s = (top_s[:, :, 0, :, None] + top_s[:, :, 1, None, :]).reshape(t, PEER_HEADS, PEER_TOPK * PEER_TOPK)
    cand_id = (top_i[:, :, 0, :, None] * N_KEYS + top_i[:, :, 1, None, :]).reshape(t, PEER_HEADS, PEER_TOPK * PEER_TOPK)
    best_s, best_j = lax.top_k(cand_s, PEER_TOPK)
    ids = jnp.take_along_axis(cand_id, best_j, axis=-1)
    gates = jax.nn.softmax(best_s, axis=-1)
    nc = t // PEER_CHUNK

    def chunk(args):
        xc, idc, gc = args
        u = u_tab[idc]
        act = jax.nn.gelu(jnp.einsum('cd,chkd->chk', xc, u).astype(jnp.float32))
        vv = v_tab[idc]
        return jnp.einsum('chk,chkd->cd', (gc * act).astype(xc.dtype), vv)

    out = lax.map(chunk, (xt.reshape(nc, PEER_CHUNK, d),
                          ids.reshape(nc, PEER_CHUNK, PEER_HEADS, PEER_TOPK),
                          gates.reshape(nc, PEER_CHUNK, PEER_HEADS, PEER_TOPK)))
    return out.reshape(b, s, d)


def setup_inputs(seed: int = 0) -> dict:
    key = jax.random.key(seed)
    ks = jax.random.split(key, 21)
    f32 = jnp.float32

    def nrm(k, shape, std):
        return jax.random.normal(k, shape, f32) * std

    x = nrm(ks[0], (BATCH, SEQ, D_MODEL), 1.0)
    c = nrm(ks[1], (BATCH, D_MODEL), 1.0)
    w_ada = nrm(ks[2], (DEPTH, D_MODEL, 6 * D_MODEL), 0.5 * D_MODEL ** -0.5)
    b_ada = nrm(ks[3], (DEPTH, 6 * D_MODEL), 0.02)
    norm1_g = 1.0 + nrm(ks[4], (DEPTH, D_MODEL), 0.02)
    norm2_g = 1.0 + nrm(ks[5], (DEPTH, D_MODEL), 0.02)
    w_in = nrm(ks[6], (DEPTH, D_MODEL, IN_WIDTH), D_MODEL ** -0.5)
    w_proj_a = nrm(ks[7], (DEPTH, DIL_OUT, D_MODEL), DIL_OUT ** -0.5)
    w_proj_b = nrm(ks[8], (DEPTH, DIFF_V, D_MODEL), DIFF_V ** -0.5)
    w_out = nrm(ks[9], (DEPTH, D_MODEL, D_MODEL), D_MODEL ** -0.5)
    lam_q1 = nrm(ks[10], (DEPTH, HEAD_DIM), 0.1)
    lam_k1 = nrm(ks[11], (DEPTH, HEAD_DIM), 0.1)
    lam_q2 = nrm(ks[12], (DEPTH, HEAD_DIM), 0.1)
    lam_k2 = nrm(ks[13], (DEPTH, HEAD_DIM), 0.1)
    subln_g = 1.0 + nrm(ks[14], (DEPTH, 2 * HEAD_DIM), 0.02)
    rel_bias = nrm(ks[15], (NUM_BUCKETS, N_BIAS_HEADS), 0.5)
    peer_wq = nrm(ks[16], (DEPTH, D_MODEL, PEER_HEADS * PEER_DQ), D_MODEL ** -0.5)
    peer_subkeys = nrm(ks[17], (DEPTH, 2, N_KEYS, PEER_DK), PEER_DK ** -0.5)
    peer_u = nrm(ks[18], (DEPTH, N_EXPERTS, D_MODEL), D_MODEL ** -0.5)
    peer_v = nrm(ks[19], (DEPTH, N_EXPERTS, D_MODEL), PEER_HEADS ** -0.5)
    final_g = 1.0 + nrm(ks[20], (D_MODEL,), 0.02)
    return {'x': x, 'c': c, 'w_ada': w_ada, 'b_ada': b_ada, 'norm1_g': norm1_g, 'norm2_g': norm2_g,
            'w_in': w_in, 'w_proj_a': w_proj_a, 'w_proj_b': w_proj_b, 'w_out': w_out,
            'lam_q1': lam_q1, 'lam_k1': lam_k1, 'lam_q2': lam_q2, 'lam_k2': lam_k2, 'subln_g': subln_g,
            'rel_bias': rel_bias, 'peer_wq': peer_wq, 'peer_subkeys': peer_subkeys,
            'peer_u': peer_u, 'peer_v': peer_v, 'final_g': final_g}


def reference(x, c, w_ada, b_ada, norm1_g, norm2_g, w_in, w_proj_a, w_proj_b, w_out,
              lam_q1, lam_k1, lam_q2, lam_k2, subln_g, rel_bias, peer_wq, peer_subkeys,
              peer_u, peer_v, final_g):
    cond = jax.nn.silu(c)
    for l in range(DEPTH):
        lam_init = 0.8 - 0.6 * math.exp(-0.3 * l)
        mod = (cond @ w_ada[l] + b_ada[l])[:, None, :]
        sh1, sc1, g1, sh2, sc2, g2 = jnp.split(mod, 6, axis=-1)
        h = rmsnorm(x, norm1_g[l]) * (1.0 + sc1) + sh1
        x = x + g1 * token_mixer(h, w_in[l], w_proj_a[l], w_proj_b[l], w_out[l],
                                 lam_q1[l], lam_k1[l], lam_q2[l], lam_k2[l], subln_g[l], rel_bias, lam_init)
        h = rmsnorm(x, norm2_g[l]) * (1.0 + sc2) + sh2
        x = x + g2 * peer(h, peer_wq[l], peer_subkeys[l], peer_u[l], peer_v[l])
    return rmsnorm(x, final_g)
```

```python
import math
import numpy as np
import concourse.bass as bass
import concourse.mybir as mybir
from concourse.bass_utils import run_bass_kernel_spmd

F32 = mybir.dt.float32
BF16 = mybir.dt.bfloat16
AF = mybir.ActivationFunctionType
ALU = mybir.AluOpType
AX = mybir.AxisListType

D = 1024
T = 2048
NTT = 16
DEPTH = 4
INW = 9728
NEXP = 16384
TAB_NB = [8, 11, 22, 22]
TAB_W = [128, 512, 2048, None]
TAB_R = [1, 4, 16, 1]
TAB_DMAX = [1, 4, 15, 15]
TABCOLS = sum(TAB_NB) * 128 * 8


def tab_off(typ, head):
    off = 0
    for t in range(typ):
        off += TAB_NB[t] * 128 * 8
    return off + head * TAB_NB[typ] * 128


class Buf:
    def __init__(self, name):
        self.name = name
        self.writers = {}
        self.readers = {}
        self.sem = None
        self.dma_total = 0


class Eng:
    def __init__(self, name, obj, sem):
        self.name = name
        self.obj = obj
        self.sem = sem
        self.cnt = 0
        self.waited = {}


class Sched:
    def __init__(self, nc):
        self.nc = nc
        self.sems = {}
        self.engs = {}
        for name, obj in [("pe", nc.tensor), ("act", nc.scalar), ("dve", nc.vector), ("pool", nc.gpsimd), ("sp", nc.sync)]:
            s = self.new_sem("e_" + name)
            self.engs[name] = Eng(name, obj, s)
        self.dma_bufs = []
        self.free_sems = []

    def recycle(self, b):
        if b.sem is not None:
            self.free_sems.append((b.sem, b.dma_total))
            self.dma_bufs.remove(b)
            b.sem = None

    def new_sem(self, name):
        s = self.nc.semaphore(name).__enter__()
        self.sems[id(s)] = s
        return s

    def _wait(self, eng, need):
        for sid, val in need.items():
            if eng.waited.get(sid, 0) < val:
                eng.obj.wait_ge(self.sems[sid], val)
                eng.waited[sid] = val

    def _need(self, own_sid, reads, writes, parts):
        need = {}

        def merge(d):
            for k, v in d.items():
                if need.get(k, 0) < v:
                    need[k] = v
        for b in reads:
            merge(b.writers)
        for b in writes:
            merge(b.writers)
            merge(b.readers)
        for b in parts:
            merge(b.readers)
            merge({k: v for k, v in b.writers.items() if k != own_sid})
        return need

    def _record(self, sid, val, reads, writes, parts):
        for b in reads:
            if b.readers.get(sid, 0) < val:
                b.readers[sid] = val
        for b in writes:
            b.writers = {sid: val}
            b.readers = {}
        for b in parts:
            b.writers[sid] = val

    def op(self, engname, fn, reads=(), writes=(), parts=()):
        eng = self.engs[engname]
        sid = id(eng.sem)
        self._wait(eng, self._need(sid, reads, writes, parts))
        ins = fn(eng.obj)
        ins.then_inc(eng.sem, 1)
        eng.cnt += 1
        self._record(sid, eng.cnt, reads, writes, parts)

    def mm_group(self, mms, reads, writes=(), parts=()):
        eng = self.engs["pe"]
        sid = id(eng.sem)
        self._wait(eng, self._need(sid, reads, writes, parts))
        ins = None
        for f in mms:
            ins = f(eng.obj)
        ins.then_inc(eng.sem, 1)
        eng.cnt += 1
        self._record(sid, eng.cnt, reads, writes, parts)

    def dma(self, qname, out_ap, in_ap, owner, reads=(), writes=(), parts=()):
        eng = self.engs[qname]
        if owner.sem is None:
            if self.free_sems:
                owner.sem, owner.dma_total = self.free_sems.pop()
            else:
                owner.sem = self.new_sem("d_" + owner.name)
            self.dma_bufs.append(owner)
        sid = id(owner.sem)
        self._wait(eng, self._need(sid, reads, writes, parts))
        eng.obj.dma_start(out=out_ap, in_=in_ap).then_inc(owner.sem, 16)
        owner.dma_total += 16
        self._record(sid, owner.dma_total, reads, writes, parts)

    def barrier(self):
        allv = {}
        for e in self.engs.values():
            if e.cnt:
                allv[id(e.sem)] = e.cnt
        for b in self.dma_bufs:
            allv[id(b.sem)] = b.dma_total
        for e in self.engs.values():
            self._wait(e, allv)

    def finish(self):
        self.barrier()


def build(l0, l1, first, last, stop_after=None, with_peer=True):
    nc = bass.Bass("TRN2", target_bir_lowering=False)
    S = Sched(nc)

    def din(name, shape, dt=F32):
        return nc.dram_tensor(name, list(shape), dt, kind="ExternalInput").ap()

    x_d = din("x", [T, D])
    c_d = din("c", [128, 8])
    w_ada = din("w_ada", [DEPTH, D, 6 * D])
    b_ada = din("b_ada", [DEPTH, 6 * D])
    n1g = din("norm1_g", [DEPTH, D])
    n2g = din("norm2_g", [DEPTH, D])
    w_in = din("w_in", [DEPTH, D, INW])
    w_pa = din("w_proj_a", [DEPTH, 512, D])
    w_pb = din("w_proj_b", [DEPTH, D, D])
    w_o = din("w_out", [DEPTH, D, D])
    lq1 = din("lam_q1", [DEPTH, 64])
    lk1 = din("lam_k1", [DEPTH, 64])
    lq2 = din("lam_q2", [DEPTH, 64])
    lk2 = din("lam_k2", [DEPTH, 64])
    subg = din("subln_g", [DEPTH, 128])
    btab = din("btab", [128, TABCOLS])
    p_wq = din("peer_wq", [DEPTH, D, 2048])
    p_sk = din("peer_subkeys", [DEPTH, 2, 128, 128])
    p_u = din("peer_u", [DEPTH, NEXP, D]) if with_peer else None
    p_v = din("peer_v", [DEPTH, NEXP, D]) if with_peer else None
    fin_g = din("final_g", [D])
    ident_d = din("ident", [128, 128])
    y_d = nc.dram_tensor("y", [T, D], F32, kind="ExternalOutput").ap()

    etab_d = nc.dram_tensor("etab", [128, TABCOLS], BF16, kind="Internal").ap()
    mod_d = nc.dram_tensor("modscr", [128, 6 * D], F32, kind="Internal").ap()
    ut_d = nc.dram_tensor("utscr", [128, 128, 8, 128], BF16, kind="Internal").ap()
    vb_d = nc.dram_tensor("vbscr", [128, 128, D], BF16, kind="Internal").ap()
    B_etab, B_mod, B_ut, B_vb, B_y = Buf("etab"), Buf("modscr"), Buf("utscr"), Buf("vbscr"), Buf("y")

    _ctx = []

    _uid = [0]

    def sb(name, shape, dt):
        _uid[0] += 1
        name = "s%d_%s" % (_uid[0], name)
        cm = nc.sbuf_tensor(name, list(shape), dt)
        t = cm.__enter__()
        b = Buf(name)
        _ctx.append((cm, b))
        return t, b

    def mark():
        return len(_ctx)

    def release(m):
        while len(_ctx) > m:
            cm, b = _ctx.pop()
            S.recycle(b)
            cm.__exit__(None, None, None)

    banks = []
    for i in range(8):
        t = nc.psum_tensor("bank%d" % i, [128, 512], F32).__enter__()
        banks.append((t, Buf("bank%d" % i)))

    x_t, B_x = sb("x", [128, NTT, D], F32)
    B_xt = [Buf("x%d" % i) for i in range(NTT)]
    idb, B_idb = sb("idb", [128, 128], BF16)
    idf, B_idf = sb("idf", [128, 128], F32)
    condrep, B_condrep = sb("condrep", [128, 8, 128], F32)
    lam_t, B_lam = sb("lam", [128, 4], F32)

    S.dma("pool", idb[:], ident_d[:, :], B_idb, writes=[B_idb])
    S.dma("sp", idf[:], ident_d[:, :], B_idf, writes=[B_idf])
    if first:
        for tt in range(NTT):
            S.dma("sp", x_t[:, tt, :], x_d[tt * 128:(tt + 1) * 128, :], B_xt[tt], writes=[B_xt[tt]])
    else:
        for tt in range(NTT):
            S.dma("sp", x_t[:, tt, :], x_d[tt * 128:(tt + 1) * 128, :], B_xt[tt], writes=[B_xt[tt]])

    m0 = mark()
    c_t, B_c = sb("c_t", [128, 8], F32)
    cs_t, B_cs = sb("cs_t", [128, 8], F32)
    S.dma("sp", c_t[:], c_d[:, :], B_c, writes=[B_c])
    S.op("act", lambda e: e.activation(out=cs_t[:], in_=c_t[:], func=AF.Silu), reads=[B_c], writes=[B_cs])
    S.op("dve", lambda e: e.tensor_copy(out=condrep[:], in_=cs_t[:].unsqueeze(2).to_broadcast([128, 8, 128])),
         reads=[B_cs], writes=[B_condrep])
    CH = 2048
    tb_f = [sb("tbf%d" % i, [128, CH], F32) for i in range(2)]
    tb_b = [sb("tbb%d" % i, [128, CH], BF16) for i in range(2)]
    nch = (TABCOLS + CH - 1) // CH
    for ci in range(nch):
        c0 = ci * CH
        cw = min(CH, TABCOLS - c0)
        tf, Bf = tb_f[ci % 2]
        tbb, Bb = tb_b[ci % 2]
        S.dma("sp", tf[:, :cw], btab[:, c0:c0 + cw], Bf, writes=[Bf])
        S.op("act", lambda e, tf=tf, tbb=tbb, cw=cw: e.activation(out=tbb[:, :cw], in_=tf[:, :cw], func=AF.Exp),
             reads=[Bf], writes=[Bb])
        S.dma("sp", etab_d[:, c0:c0 + cw], tbb[:, :cw], B_etab, reads=[Bb], parts=[B_etab])
    S.barrier()
    release(m0)

    def mod_phase(l):
        m = mark()
        wt = [sb("modw%d" % i, [128, 512], F32) for i in range(3)]
        bt = [sb("modb%d" % i, [128, 512], F32) for i in range(2)]
        mo = [sb("modo%d" % i, [128, 512], F32) for i in range(2)]
        k = 0
        for nchk in range(12):
            ps, Bps = banks[nchk % 2]
            btile, Bbt = bt[nchk % 2]
            S.dma("sp", btile[:], b_ada[l:l + 1, nchk * 512:(nchk + 1) * 512].partition_broadcast(128), Bbt, writes=[Bbt])
            for kc in range(8):
                w, Bw = wt[k % 3]
                k += 1
                S.dma("sp", w[:], w_ada[l, kc * 128:(kc + 1) * 128, nchk * 512:(nchk + 1) * 512], Bw, writes=[Bw])
                S.mm_group([lambda pe, kc=kc, w=w, ps=ps: pe.matmul(ps[:], lhsT=condrep[:, kc, :], rhs=w[:],
                                                                    start=(kc == 0), stop=(kc == 7))],
                           reads=[Bw, B_condrep], **({"writes": [Bps]} if kc == 0 else {"parts": [Bps]}))
            o, Bo = mo[nchk % 2]
            S.op("dve", lambda e, o=o, ps=ps, btile=btile: e.tensor_tensor(out=o[:], in0=ps[:], in1=btile[:], op=ALU.add),
                 reads=[Bps, Bbt], writes=[Bo])
            S.dma("sp", mod_d[:, nchk * 512:(nchk + 1) * 512], o[:], B_mod, reads=[Bo], parts=[B_mod])
        lt = [sb("lamv%d" % i, [128, 64], F32) for i in range(4)]
        for i, src in enumerate([lq1, lk1, lq2, lk2]):
            S.dma("sp", lt[i][0][:], src[l:l + 1, :].partition_broadcast(128), lt[i][1], writes=[lt[i][1]])
        pr, Bpr = sb("lampr", [128, 2, 64], F32)
        dd, Bdd = sb("lamdd", [128, 2], F32)
        ee, Bee = sb("lamee", [128, 2], F32)
        S.op("dve", lambda e: e.tensor_tensor(out=pr[:, 0, :], in0=lt[0][0][:], in1=lt[1][0][:], op=ALU.mult),
             reads=[lt[0][1], lt[1][1]], parts=[Bpr])
        S.op("dve", lambda e: e.tensor_tensor(out=pr[:, 1, :], in0=lt[2][0][:], in1=lt[3][0][:], op=ALU.mult),
             reads=[lt[2][1], lt[3][1]], parts=[Bpr])
        S.op("dve", lambda e: e.reduce_sum(out=dd[:], in_=pr[:], axis=AX.X), reads=[Bpr], writes=[Bdd])
        S.op("act", lambda e: e.activation(out=ee[:], in_=dd[:], func=AF.Exp), reads=[Bdd], writes=[Bee])
        lam_init = 0.8 - 0.6 * math.exp(-0.3 * l)
        S.op("dve", lambda e: e.scalar_tensor_tensor(out=lam_t[:, 0:1], in0=ee[:, 1:2], scalar=-lam_init, in1=ee[:, 0:1],
                                                     op0=ALU.add, op1=ALU.subtract),
             reads=[Bee], writes=[B_lam])
        S.barrier()
        release(m)

    def norm_tile(tt, A, BA, Bb, BBb, hT, B_hT, col0, tmp, ps_bank):
        (sq, Bsq), (ss, Bss), (rs, Brs), (hn, Bhn), (hb, Bhb) = tmp
        S.op("act", lambda e: e.activation(out=sq[:], in_=x_t[:, tt, :], func=AF.Square, accum_out=ss[:]),
             reads=[B_xt[tt]], writes=[Bsq, Bss])
        S.op("act", lambda e: e.activation(out=rs[:], in_=ss[:], func=AF.Sqrt, scale=1.0 / D, bias=EPS_T[:, 0:1]),
             reads=[Bss, B_eps], writes=[Brs])
        S.op("dve", lambda e: e.reciprocal(out=rs[:], in_=rs[:]), reads=[Brs], writes=[Brs])
        S.op("dve", lambda e: e.scalar_tensor_tensor(out=hn[:], in0=x_t[:, tt, :], scalar=rs[:, 0:1], in1=A[:],
                                                     op0=ALU.mult, op1=ALU.mult),
             reads=[B_xt[tt], Brs, BA], writes=[Bhn])
        if Bb is not None:
            S.op("pool", lambda e: e.tensor_tensor(out=hb[:], in0=hn[:], in1=Bb[:], op=ALU.add),
                 reads=[Bhn, BBb], writes=[Bhb])
        if hT is None:
            return
        ps, Bps = ps_bank
        psb = ps[:].bitcast(BF16)
        S.mm_group([lambda pe, dc=dc: pe.transpose(out=psb[:, dc * 128:(dc + 1) * 128], in_=hb[:, dc * 128:(dc + 1) * 128],
                                                   identity=idb[:]) for dc in range(8)],
                   reads=[Bhb, B_idb], writes=[Bps])
        S.op("act", lambda e: e.activation(out=hT[:, :, col0:col0 + 128],
                                           in_=psb.rearrange("p (a b) -> p a b", a=8), func=AF.Copy),
             reads=[Bps], parts=[B_hT])

    EPS_T, B_eps = sb("eps", [128, 2], F32)
    S.op("dve", lambda e: e.memset(EPS_T[:, 0:1], 1e-6), parts=[B_eps])
    S.op("dve", lambda e: e.memset(EPS_T[:, 1:2], 1e-5), parts=[B_eps])

    def load_bc(name, src_row_ap, n, q="sp"):
        t, Bt = sb(name, [128, n], F32)
        S.dma(q, t[:], src_row_ap.partition_broadcast(128), Bt, writes=[Bt])
        return t, Bt

    def norm_tmp():
        return [sb("n_sq", [128, D], BF16), sb("n_ss", [128, 1], F32), sb("n_rs", [128, 1], F32),
                sb("n_hn", [128, D], F32), sb("n_hb", [128, D], BF16)]

    def make_AB(l, which):
        base = 0 if which == 1 else 3 * D
        A, BA = sb("A_bc", [128, D], F32)
        Bt, BB = sb("B_bc", [128, D], F32)
        g, Bg = load_bc("ng_bc", (n1g if which == 1 else n2g)[l:l + 1, :], D)
        S.dma("sp", A[:], mod_d[:, base + D:base + 2 * D], BA, reads=[B_mod], writes=[BA])
        S.dma("sp", Bt[:], mod_d[:, base:base + D], BB, reads=[B_mod], writes=[BB])
        S.op("dve", lambda e: e.scalar_tensor_tensor(out=A[:], in0=A[:], scalar=1.0, in1=g[:], op0=ALU.add, op1=ALU.mult),
             reads=[BA, Bg], writes=[BA])
        return A, BA, Bt, BB

    def mixer_phase(l):
        m = mark()
        hT, B_hT = sb("hT", [128, 8, T], BF16)
        oaT, B_oaT = sb("oaT", [128, 4, T], BF16)
        obT, B_obT = sb("obT", [128, 8, T], BF16)
        m1 = mark()
        A, BA, Bt, BB = make_AB(l, 1)
        tmp = norm_tmp()
        for tt in range(NTT):
            norm_tile(tt, A, BA, Bt, BB, hT, B_hT, tt * 128, tmp, banks[7])
        S.barrier()
        release(m1)
        if stop_after == "h1":
            return m, hT
        m2 = mark()
        wq = [sb("wq%d" % i, [128, 8, 64], BF16) for i in range(2)]
        wk = [sb("wk%d" % i, [128, 8, 64], BF16) for i in range(2)]
        wv = [sb("wv%d" % i, [128, 8, 128], BF16) for i in range(2)]
        qT = [sb("qT%d" % i, [128, T], BF16) for i in range(2)]
        kT = [sb("kT%d" % i, [128, T], BF16) for i in range(2)]
        vx = [sb("vx0", [128, NTT, 129], BF16)] * 2
        tab = [sb("tab0", [128, 22 * 128], BF16)] * 2
        pe_t = [sb("pexp%d" % i, [128, 512], BF16) for i in range(2)]
        pm_t = [sb("pm%d" % i, [128, 512], BF16) for i in range(2)]
        acc_sb, B_acc = sb("accsb", [128, NTT, 65], F32)
        otmp, B_otmp = sb("otmp", [128, NTT, 128], F32)
        rr = [sb("rr%d" % i, [128, 4], F32) for i in range(2)]
        osb = [sb("osb%d" % i, [128, 128], BF16) for i in range(2)]
        oa2, B_oa2 = sb("oa2", [128, NTT, 128], BF16)
        sg, Bsg = load_bc("subg", subg[l:l + 1, :], 128)
        lam_init = 0.8 - 0.6 * math.exp(-0.3 * l)
        S.op("dve", lambda e: e.tensor_scalar(out=sg[:], in0=sg[:], scalar1=1.0 - lam_init, scalar2=None, op0=ALU.mult),
             reads=[Bsg], writes=[Bsg])
        S.op("pool", lambda e: e.memset(vx[0][0][:], 1.0), writes=[vx[0][1]])
        ctr = {"st": 0, "sc": 0, "rr": 0, "os": 0}

        def stream(qcol, kcol, vcol, dh, dv, typ, thead, fin):
            si = ctr["st"] % 2
            ctr["st"] += 1
            (wq_t, Bwq), (wk_t, Bwk), (wv_t, Bwv) = wq[si], wk[si], wv[si]
            (q_t, Bq), (k_t, Bk), (v_t, Bv), (tb_t, Btb) = qT[si], kT[si], vx[si], tab[si]
            wsrc = w_in[l].rearrange("(a p) n -> p a n", p=128)
            S.dma("pool", wq_t[:, :, :dh], wsrc[:, :, qcol:qcol + dh], Bwq, writes=[Bwq])
            S.dma("pool", wk_t[:, :, :dh], wsrc[:, :, kcol:kcol + dh], Bwk, writes=[Bwk])
            S.dma("pool", wv_t[:, :, :dv], wsrc[:, :, vcol:vcol + dv], Bwv, writes=[Bwv])
            nb = TAB_NB[typ]
            to = tab_off(typ, thead)
            S.dma("sp", tb_t[:, :nb * 128], etab_d[:, to:to + nb * 128], Btb, reads=[B_etab], writes=[Btb])
            for (w_t, Bw, o_t, Bo) in ((wq_t, Bwq, q_t, Bq), (wk_t, Bwk, k_t, Bk)):
                for qc in range(4):
                    ps, Bps = banks[4 + (qc % 2)]
                    S.mm_group([lambda pe, kc=kc, w_t=w_t, ps=ps, qc=qc: pe.matmul(
                        ps[:dh, :], lhsT=w_t[:, kc, :dh], rhs=hT[:, kc, qc * 512:(qc + 1) * 512],
                        start=(kc == 0), stop=(kc == 7)) for kc in range(8)],
                        reads=[Bw, B_hT], writes=[Bps])
                    S.op("act", lambda e, o_t=o_t, ps=ps, qc=qc: e.activation(
                        out=o_t[:dh, qc * 512:(qc + 1) * 512], in_=ps[:dh, :], func=AF.Copy),
                        reads=[Bps], parts=[Bo])
            for tt in range(NTT):
                ps, Bps = banks[4 + (tt % 2)]
                S.mm_group([lambda pe, kc=kc, ps=ps, tt=tt: pe.matmul(
                    ps[:, :dv], lhsT=hT[:, kc, tt * 128:(tt + 1) * 128], rhs=wv_t[:, kc, :dv],
                    start=(kc == 0), stop=(kc == 7)) for kc in range(8)],
                    reads=[Bwv, B_hT], writes=[Bps])
                S.op("dve", lambda e, ps=ps, tt=tt: e.tensor_copy(out=v_t[:, tt, :dv], in_=ps[:, :dv]),
                     reads=[Bps], parts=[Bv])
            if dv < 128:
                S.op("pool", lambda e: e.memset(v_t[:, :, dv:dv + 1], 1.0), parts=[Bv])
            dmax = TAB_DMAX[typ]
            for qc in range(4):
                accb = [banks[2], banks[3]]
                first_in = [True, True]
                kts = [kt for kt in range(NTT) if any(0 <= 4 * qc + j - kt <= dmax for j in range(4))]
                lastkt = {}
                for kt in kts:
                    for j in range(4):
                        if 0 <= 4 * qc + j - kt <= dmax:
                            lastkt[j] = kt
                for kt in kts:
                    pi = ctr["sc"] % 2
                    ctr["sc"] += 1
                    ps, Bps = banks[pi]
                    (pex, Bpex), (pm, Bpm) = pe_t[pi], pm_t[pi]
                    S.mm_group([lambda pe, ps=ps, kt=kt, qc=qc: pe.matmul(
                        ps[:, :], lhsT=k_t[:dh, kt * 128:(kt + 1) * 128], rhs=q_t[:dh, qc * 512:(qc + 1) * 512],
                        start=True, stop=True)], reads=[Bq, Bk], writes=[Bps])
                    S.op("act", lambda e, ps=ps, pex=pex: e.activation(out=pex[:], in_=ps[:], func=AF.Exp, scale=0.125),
                         reads=[Bps], writes=[Bpex])
                    d0 = 4 * qc - kt
                    S.op("dve", lambda e, pm=pm, pex=pex, d0=d0: e.tensor_tensor(
                        out=pm[:], in0=pex[:], in1=tb_t[:, (d0 + 3) * 128:(d0 + 3) * 128 + 512], op=ALU.mult),
                        reads=[Bpex, Btb], writes=[Bpm])
                    mms = []
                    for j in range(4):
                        dd_ = 4 * qc + j - kt
                        if not (0 <= dd_ <= dmax):
                            continue
                        bi = j // 2
                        ab, Bab = accb[bi]
                        st = first_in[bi]
                        first_in[bi] = False
                        c0 = (j % 2) * (dv + 1)
                        mms.append((bi, lambda pe, ab=ab, pm=pm, j=j, kt=kt, st=st, c0=c0: pe.matmul(
                            ab[:, c0:c0 + dv + 1], lhsT=pm[:, j * 128:(j + 1) * 128], rhs=v_t[:, kt, :dv + 1],
                            start=st, stop=(lastkt[j] == kt), skip_group_check=True)))
                    for bi in (0, 1):
                        fl = [f for (b_, f) in mms if b_ == bi]
                        if fl:
                            S.mm_group(fl, reads=[Bpm, Bv], parts=[accb[bi][1]])
                fin(qc, accb, dv)

        def fin_dil(first_s, last_s, pairslot, pair_idx):
            def f(qc, accb, dv):
                for j in range(4):
                    tt = 4 * qc + j
                    ab, Bab = accb[j // 2]
                    c0 = (j % 2) * 65
                    if first_s:
                        S.op("dve", lambda e, ab=ab, tt=tt, c0=c0: e.tensor_copy(out=acc_sb[:, tt, :65], in_=ab[:, c0:c0 + 65]),
                             reads=[Bab], parts=[B_acc])
                    else:
                        S.op("dve", lambda e, ab=ab, tt=tt, c0=c0: e.tensor_tensor(
                            out=acc_sb[:, tt, :65], in0=acc_sb[:, tt, :65], in1=ab[:, c0:c0 + 65], op=ALU.add),
                            reads=[Bab, B_acc], parts=[B_acc])
                    if last_s:
                        r, Br = rr[ctr["rr"] % 2]
                        ctr["rr"] += 1
                        S.op("dve", lambda e, r=r, tt=tt: e.reciprocal(out=r[:, 0:1], in_=acc_sb[:, tt, 64:65]),
                             reads=[B_acc], writes=[Br])
                        S.op("dve", lambda e, r=r, tt=tt: e.tensor_scalar(
                            out=oa2[:, tt, pairslot * 64:(pairslot + 1) * 64], in0=acc_sb[:, tt, :64],
                            scalar1=r[:, 0:1], scalar2=None, op0=ALU.mult),
                            reads=[Br, B_acc], parts=[B_oa2])
                        if pairslot == 1:
                            ps, Bps = banks[6]
                            psb = ps[:].bitcast(BF16)
                            S.mm_group([lambda pe, tt=tt: pe.transpose(out=psb[:, 0:128], in_=oa2[:, tt, :], identity=idb[:])],
                                       reads=[B_oa2, B_idb], writes=[Bps])
                            S.op("act", lambda e, tt=tt: e.activation(out=oaT[:, pair_idx, tt * 128:(tt + 1) * 128],
                                                                      in_=psb[:, 0:128], func=AF.Copy),
                                 reads=[Bps], parts=[B_oaT])
            return f

        def fin_diff(s, head):
            def f(qc, accb, dv):
                for j in range(4):
                    tt = 4 * qc + j
                    ab, Bab = accb[j // 2]
                    c0 = (j % 2) * 129
                    r, Br = rr[ctr["rr"] % 2]
                    ctr["rr"] += 1
                    S.op("dve", lambda e, r=r, ab=ab, c0=c0: e.reciprocal(out=r[:, 0:1], in_=ab[:, c0 + 128:c0 + 129]),
                         reads=[Bab], writes=[Br])
                    if s == 0:
                        S.op("dve", lambda e, r=r, ab=ab, c0=c0, tt=tt: e.tensor_scalar(
                            out=otmp[:, tt, :], in0=ab[:, c0:c0 + 128], scalar1=r[:, 0:1], scalar2=None, op0=ALU.mult),
                            reads=[Br, Bab], parts=[B_otmp])
                    else:
                        S.op("dve", lambda e, r=r: e.tensor_tensor(out=r[:, 1:2], in0=r[:, 0:1], in1=lam_t[:, 0:1], op=ALU.mult),
                             reads=[Br, B_lam], writes=[Br])
                        S.op("dve", lambda e, r=r, ab=ab, c0=c0, tt=tt: e.scalar_tensor_tensor(
                            out=otmp[:, tt, :], in0=ab[:, c0:c0 + 128], scalar=r[:, 1:2], in1=otmp[:, tt, :],
                            op0=ALU.mult, op1=ALU.add),
                            reads=[Br, Bab, B_otmp], parts=[B_otmp])
                        o_b, Bob = osb[ctr["os"] % 2]
                        ctr["os"] += 1
                        S.op("act", lambda e, o_b=o_b, r=r, tt=tt: e.activation(out=o_b[:], in_=otmp[:, tt, :], func=AF.Square,
                                                                                accum_out=r[:, 2:3]),
                             reads=[B_otmp], writes=[Bob, Br])
                        S.op("act", lambda e, r=r: e.activation(out=r[:, 3:4], in_=r[:, 2:3], func=AF.Sqrt, scale=1.0 / 128,
                                                                bias=EPS_T[:, 1:2]),
                             reads=[Br, B_eps], writes=[Br])
                        S.op("dve", lambda e, r=r: e.reciprocal(out=r[:, 3:4], in_=r[:, 3:4]), reads=[Br], writes=[Br])
                        S.op("dve", lambda e, o_b=o_b, r=r, tt=tt: e.scalar_tensor_tensor(
                            out=o_b[:], in0=otmp[:, tt, :], scalar=r[:, 3:4], in1=sg[:], op0=ALU.mult, op1=ALU.mult),
                            reads=[Br, B_otmp, Bsg], writes=[Bob])
                        ps, Bps = banks[6]
                        psb = ps[:].bitcast(BF16)
                        S.mm_group([lambda pe, o_b=o_b: pe.transpose(out=psb[:, 0:128], in_=o_b[:], identity=idb[:])],
                                   reads=[Bob, B_idb], writes=[Bps])
                        S.op("act", lambda e, tt=tt: e.activation(out=obT[:, head, tt * 128:(tt + 1) * 128],
                                                                  in_=psb[:, 0:128], func=AF.Copy),
                             reads=[Bps], parts=[B_obT])
            return f

        for hh in range(8):
            for g in range(3):
                qcol = g * 512 + hh * 64
                stream(qcol, 1536 + qcol, 3072 + qcol, 64, 64, g, hh, fin_dil(g == 0, g == 2, hh % 2, hh // 2))
        for hd in range(8):
            for s in range(2):
                qcol = 4608 + hd * 128 + s * 64
                stream(qcol, qcol + 1024, 6656 + hd * 128, 64, 128, 3, hd, fin_diff(s, hd))
        S.barrier()
        release(m2)
        m3 = mark()
        G1, BG1 = sb("G1", [128, D], F32)
        S.dma("sp", G1[:], mod_d[:, 2 * D:3 * D], BG1, reads=[B_mod], writes=[BG1])
        wga, Bwga = sb("wga", [128, 8, 512], BF16)
        wgb, Bwgb = sb("wgb", [128, 8, 512], BF16)
        wpa, Bwpa = sb("wpa", [128, 4, 512], BF16)
        wpb, Bwpb = sb("wpb", [128, 8, 512], BF16)
        wo, Bwo = sb("wo", [128, 4, D], BF16)
        sga = [sb("sga%d" % i, [128, 512], F32) for i in range(2)]
        sgb = [sb("sgb%d" % i, [128, 512], F32) for i in range(2)]
        mg = [sb("mg%d" % i, [128, 512], BF16) for i in range(2)]
        mgT = [sb("mgT%d" % i, [128, 4, 128], BF16) for i in range(2)]
        wsrc = w_in[l].rearrange("(a p) n -> p a n", p=128)
        for nchk in range(2):
            n0 = nchk * 512
            S.dma("pool", wga[:], wsrc[:, :, 7680 + n0:7680 + n0 + 512], Bwga, writes=[Bwga])
            S.dma("pool", wgb[:], wsrc[:, :, 8704 + n0:8704 + n0 + 512], Bwgb, writes=[Bwgb])
            S.dma("pool", wpa[:], w_pa[l].rearrange("(a p) n -> p a n", p=128)[:, :, n0:n0 + 512], Bwpa, writes=[Bwpa])
            S.dma("pool", wpb[:], w_pb[l].rearrange("(a p) n -> p a n", p=128)[:, :, n0:n0 + 512], Bwpb, writes=[Bwpb])
            S.dma("pool", wo[:], w_o[l, n0:n0 + 512, :].rearrange("(a p) n -> p a n", p=128), Bwo, writes=[Bwo])
            for tt in range(NTT):
                i2 = tt % 2
                tsl = slice(tt * 128, (tt + 1) * 128)
                (pga, Bpga), (pgb, Bpgb), (ppa, Bppa), (ppb, Bppb) = banks[0], banks[1], banks[2], banks[3]
                S.mm_group([lambda pe, kc=kc: pe.matmul(pga[:], lhsT=hT[:, kc, tsl], rhs=wga[:, kc, :], start=(kc == 0), stop=(kc == 7))
                            for kc in range(8)], reads=[B_hT, Bwga], writes=[Bpga])
                S.mm_group([lambda pe, kc=kc: pe.matmul(pgb[:], lhsT=hT[:, kc, tsl], rhs=wgb[:, kc, :], start=(kc == 0), stop=(kc == 7))
                            for kc in range(8)], reads=[B_hT, Bwgb], writes=[Bpgb])
                S.mm_group([lambda pe, kc=kc: pe.matmul(ppa[:], lhsT=oaT[:, kc, tsl], rhs=wpa[:, kc, :], start=(kc == 0), stop=(kc == 3))
                            for kc in range(4)], reads=[B_oaT, Bwpa], writes=[Bppa])
                S.mm_group([lambda pe, kc=kc: pe.matmul(ppb[:], lhsT=obT[:, kc, tsl], rhs=wpb[:, kc, :], start=(kc == 0), stop=(kc == 7))
                            for kc in range(8)], reads=[B_obT, Bwpb], writes=[Bppb])
                (a_, Ba_), (b_, Bb_), (mg_, Bmg), (mgT_, BmgT) = sga[i2], sgb[i2], mg[i2], mgT[i2]
                S.op("act", lambda e, a_=a_: e.activation(out=a_[:], in_=pga[:], func=AF.Sigmoid), reads=[Bpga], writes=[Ba_])
                S.op("act", lambda e, b_=b_: e.activation(out=b_[:], in_=pgb[:], func=AF.Sigmoid), reads=[Bpgb], writes=[Bb_])
                S.op("dve", lambda e, a_=a_: e.tensor_tensor(out=a_[:], in0=a_[:], in1=ppa[:], op=ALU.mult), reads=[Ba_, Bppa], writes=[Ba_])
                S.op("dve", lambda e, b_=b_: e.tensor_tensor(out=b_[:], in0=b_[:], in1=ppb[:], op=ALU.mult), reads=[Bb_, Bppb], writes=[Bb_])
                S.op("pool", lambda e, a_=a_, b_=b_, mg_=mg_: e.tensor_tensor(out=mg_[:], in0=a_[:], in1=b_[:], op=ALU.add),
                     reads=[Ba_, Bb_], writes=[Bmg])
                ps, Bps = banks[6]
                psb = ps[:].bitcast(BF16)
                S.mm_group([lambda pe, c=c, mg_=mg_: pe.transpose(out=psb[:, c * 128:(c + 1) * 128], in_=mg_[:, c * 128:(c + 1) * 128],
                                                                  identity=idb[:]) for c in range(4)],
                           reads=[Bmg, B_idb], writes=[Bps])
                S.op("act", lambda e, mgT_=mgT_: e.activation(out=mgT_[:], in_=psb[:, 0:512].rearrange("p (a b) -> p a b", a=4), func=AF.Copy),
                     reads=[Bps], writes=[BmgT])
                for half in range(2):
                    po, Bpo = banks[4 + half]
                    S.mm_group([lambda pe, c=c, mgT_=mgT_, po=po, half=half: pe.matmul(
                        po[:], lhsT=mgT_[:, c, :], rhs=wo[:, c, half * 512:(half + 1) * 512], start=(c == 0), stop=(c == 3))
                        for c in range(4)], reads=[BmgT, Bwo], writes=[Bpo])
                    S.op("dve", lambda e, po=po, half=half, tt=tt: e.tensor_tensor(
                        out=po[:], in0=po[:], in1=G1[:, half * 512:(half + 1) * 512], op=ALU.mult),
                        reads=[Bpo, BG1], writes=[Bpo])
                    S.op("dve", lambda e, po=po, half=half, tt=tt: e.tensor_tensor(
                        out=x_t[:, tt, half * 512:(half + 1) * 512], in0=x_t[:, tt, half * 512:(half + 1) * 512], in1=po[:], op=ALU.add),
                        reads=[Bpo, B_xt[tt]], parts=[B_xt[tt]])
        S.barrier()
        release(m3)
        return m, hT


    def peer_prep(l):
        m = mark()
        ut_f = [sb("utf%d" % i, [128, D], F32) for i in range(2)]
        ut_b = [sb("utb%d" % i, [128, 8, 128], BF16) for i in range(2)]
        v_f = [sb("vf%d" % i, [128, D], F32) for i in range(2)]
        v_b = [sb("vb%d" % i, [128, D], BF16) for i in range(2)]
        for et in range(128):
            i2 = et % 2
            (uf, Buf_), (ub, Bub), (vf, Bvf), (vb_, Bvb) = ut_f[i2], ut_b[i2], v_f[i2], v_b[i2]
            S.dma("sp", uf[:], p_u[l, et * 128:(et + 1) * 128, :], Buf_, writes=[Buf_])
            for hb_ in range(2):
                ps, Bps = banks[4 + 2 * i2 + hb_]
                S.mm_group([lambda pe, c=c, ps=ps, uf=uf, hb_=hb_: pe.transpose(
                    out=ps[:, c * 128:(c + 1) * 128], in_=uf[:, (hb_ * 4 + c) * 128:(hb_ * 4 + c + 1) * 128], identity=idf[:])
                    for c in range(4)], reads=[Buf_, B_idf], writes=[Bps])
                S.op("act", lambda e, ps=ps, ub=ub, hb_=hb_: e.activation(
                    out=ub[:, hb_ * 4:(hb_ + 1) * 4, :], in_=ps[:].rearrange("p (a b) -> p a b", a=4), func=AF.Copy),
                    reads=[Bps], parts=[Bub])
            S.dma("sp", ut_d[:, et, :, :], ub[:], B_ut, reads=[Bub], parts=[B_ut])
            S.dma("sp", vf[:], p_v[l, et * 128:(et + 1) * 128, :], Bvf, writes=[Bvf])
            S.op("dve" if et % 2 == 0 else "pool", lambda e, vb_=vb_, vf=vf: e.tensor_copy(out=vb_[:], in_=vf[:]),
                 reads=[Bvf], writes=[Bvb])
            S.dma("sp", vb_d[:, et, :], vb_[:], B_vb, reads=[Bvb], parts=[B_vb])
        S.barrier()
        release(m)

    def peer_phase(l):
        peer_prep(l)
        m = mark()
        NT = 2
        NBLK = NTT // NT
        TB = NT * 128
        A, BA, Bt, BB = make_AB(l, 2)
        G2, BG2 = sb("G2", [128, D], F32)
        S.dma("sp", G2[:], mod_d[:, 5 * D:6 * D], BG2, reads=[B_mod], writes=[BG2])
        tmp = norm_tmp()
        h2T, B_h2T = sb("h2T", [128, 8, TB], BF16)
        skT, B_skT = sb("skT", [128, 2, 128], F32)
        skl, B_skl = sb("skl", [128, 128], F32)
        for p in range(2):
            S.dma("sp", skl[:], p_sk[l, p, :, :], B_skl, writes=[B_skl])
            ps, Bps = banks[4]
            S.mm_group([lambda pe, ps=ps: pe.transpose(out=ps[:, 0:128], in_=skl[:], identity=idf[:])],
                       reads=[B_skl, B_idf], writes=[Bps])
            S.op("act", lambda e, ps=ps, p=p: e.activation(out=skT[:, p, :], in_=ps[:, 0:128], func=AF.Copy),
                 reads=[Bps], parts=[B_skT])
        wqf = [sb("wqf%d" % i, [128, 8, 128], F32) for i in range(2)]
        wqb = [sb("wqb%d" % i, [128, 8, 128], BF16) for i in range(2)]
        qTs = [sb("qTs%d" % i, [128, TB], F32) for i in range(2)]
        sc, B_sc = sb("sc", [128, NT * 16, 128], F32)
        sc2, B_sc2 = sb("sc2", [128, 128], F32)
        T16, B_T16 = sb("T16", [128, NT * 16, 16], F32)
        cand, B_cand = sb("cand", [128, NT * 8, 256], F32)
        cand2, B_cand2 = sb("cand2", [128, 256], F32)
        S24, B_S24 = sb("S24", [128, NT * 8, 24], F32)
        sm, B_sm = sb("sm", [128, NT * 8, 16], F32)
        sv_ = {}
        for nm in ["negM", "tau", "nh", "Z", "e1", "kap"]:
            sv_[nm] = sb(nm, [128, NT * 8], F32)
        NG = NT * 8
        IC = 2
        Ft = [[sb("F%d_%d" % (bf, g), [128, IC * 128], BF16) for g in range(NG)] for bf in range(2)]
        ut_ = [sb("u%d" % i, [128, IC * 128], F32) for i in range(2)]
        utc = [sb("utc%d" % i, [128, IC, 8, 128], BF16) for i in range(2)]
        vbc = [sb("vbc%d" % i, [128, IC, D], BF16) for i in range(2)]
        gl_t = [sb("gl%d" % i, [128, TB], F32) for i in range(2)]
        ga_t = [sb("ga%d" % i, [128, TB], BF16) for i in range(2)]
        xo, Bxo = sb("xo", [128, 512], F32)
        wsrc = p_wq[l].rearrange("(a p) n -> p a n", p=128)
        cn = {"w": 0, "u": 0}
        for blk in range(NBLK):
            for j in range(NT):
                norm_tile(blk * NT + j, A, BA, Bt, BB, h2T, B_h2T, j * 128, tmp, banks[7])
            for hp in range(16):
                i2 = cn["w"] % 2
                cn["w"] += 1
                (wf, Bwf), (wb, Bwb), (qs, Bqs) = wqf[i2], wqb[i2], qTs[i2]
                S.dma("sp", wf[:], wsrc[:, :, hp * 128:(hp + 1) * 128], Bwf, writes=[Bwf])
                S.op("act", lambda e, wf=wf, wb=wb: e.activation(out=wb[:], in_=wf[:], func=AF.Copy), reads=[Bwf], writes=[Bwb])
                ps, Bps = banks[4 + i2]
                S.mm_group([lambda pe, kc=kc, ps=ps, wb=wb: pe.matmul(ps[:, :TB], lhsT=wb[:, kc, :], rhs=h2T[:, kc, :],
                                                                     start=(kc == 0), stop=(kc == 7)) for kc in range(8)],
                           reads=[Bwb, B_h2T], writes=[Bps])
                S.op("dve", lambda e, ps=ps, qs=qs: e.tensor_copy(out=qs[:], in_=ps[:, :TB]), reads=[Bps], writes=[Bqs])
                ps2, Bps2 = banks[6]
                S.mm_group([lambda pe, j=j, qs=qs, hp=hp: pe.matmul(ps2[:, j * 128:(j + 1) * 128], lhsT=qs[:, j * 128:(j + 1) * 128],
                                                                   rhs=skT[:, hp % 2, :], start=True, stop=True, skip_group_check=True)
                            for j in range(NT)], reads=[Bqs, B_skT], writes=[Bps2])
                for j in range(NT):
                    S.op("act", lambda e, j=j, hp=hp: e.activation(out=sc[:, j * 16 + hp, :], in_=ps2[:, j * 128:(j + 1) * 128], func=AF.Copy),
                         reads=[Bps2], parts=[B_sc])
            for g in range(NT * 16):
                S.op("dve", lambda e, g=g: e.max(out=T16[:, g, 0:8], in_=sc[:, g, :]), reads=[B_sc], parts=[B_T16])
                S.op("dve", lambda e, g=g: e.match_replace(out=sc2[:], in_to_replace=T16[:, g, 0:8], in_values=sc[:, g, :], imm_value=-1e30),
                     reads=[B_sc, B_T16], writes=[B_sc2])
                S.op("dve", lambda e, g=g: e.max(out=T16[:, g, 8:16], in_=sc2[:]), reads=[B_sc2], parts=[B_T16])
            T16v = T16[:].rearrange("p (g two) r -> p g two r", two=2)
            S.op("dve", lambda e: e.tensor_tensor(out=cand[:].rearrange("p g (r s) -> p g r s", r=16),
                                                  in0=T16v[:, :, 0, :].unsqueeze(3).to_broadcast([128, NG, 16, 16]),
                                                  in1=T16v[:, :, 1, :].unsqueeze(2).to_broadcast([128, NG, 16, 16]), op=ALU.add),
                 reads=[B_T16], writes=[B_cand])
            for g in range(NG):
                S.op("dve", lambda e, g=g: e.max(out=S24[:, g, 0:8], in_=cand[:, g, :]), reads=[B_cand], parts=[B_S24])
                S.op("dve", lambda e, g=g: e.match_replace(out=cand2[:], in_to_replace=S24[:, g, 0:8], in_values=cand[:, g, :], imm_value=-1e30),
                     reads=[B_cand, B_S24], writes=[B_cand2])
                S.op("dve", lambda e, g=g: e.max(out=S24[:, g, 8:16], in_=cand2[:]), reads=[B_cand2], parts=[B_S24])
                S.op("dve", lambda e, g=g: e.match_replace(out=cand2[:], in_to_replace=S24[:, g, 8:16], in_values=cand2[:], imm_value=-1e30),
                     reads=[B_cand2, B_S24], writes=[B_cand2])
                S.op("dve", lambda e, g=g: e.max(out=S24[:, g, 16:24], in_=cand2[:]), reads=[B_cand2], parts=[B_S24])
            (negM, BnegM), (tau, Btau), (nh, Bnh), (Z, BZ), (e1, Be1), (kap, Bkap) = [sv_[n] for n in ["negM", "tau", "nh", "Z", "e1", "kap"]]
            S.op("dve", lambda e: e.tensor_scalar(out=negM[:], in0=S24[:, :, 0], scalar1=-1.0, scalar2=None, op0=ALU.mult),
                 reads=[B_S24], writes=[BnegM])
            S.op("dve", lambda e: e.tensor_tensor(out=tau[:], in0=S24[:, :, 15], in1=S24[:, :, 16], op=ALU.add), reads=[B_S24], writes=[Btau])
            S.op("dve", lambda e: e.tensor_scalar(out=tau[:], in0=tau[:], scalar1=0.5, scalar2=None, op0=ALU.mult), reads=[Btau], writes=[Btau])
            S.op("dve", lambda e: e.tensor_scalar(out=nh[:], in0=tau[:], scalar1=-0.5, scalar2=None, op0=ALU.mult), reads=[Btau], writes=[Bnh])
            S.op("dve", lambda e: e.tensor_tensor(out=sm[:], in0=S24[:, :, 0:16], in1=negM[:].unsqueeze(2).to_broadcast([128, NG, 16]), op=ALU.add),
                 reads=[B_S24, BnegM], writes=[B_sm])
            S.op("act", lambda e: e.activation(out=sm[:], in_=sm[:], func=AF.Exp), reads=[B_sm], writes=[B_sm])
            S.op("dve", lambda e: e.reduce_sum(out=Z[:], in_=sm[:], axis=AX.X), reads=[B_sm], writes=[BZ])
            S.op("dve", lambda e: e.tensor_tensor(out=e1[:], in0=tau[:], in1=negM[:], op=ALU.add), reads=[Btau, BnegM], writes=[Be1])
            S.op("act", lambda e: e.activation(out=e1[:], in_=e1[:], func=AF.Exp), reads=[Be1], writes=[Be1])
            S.op("dve", lambda e: e.reciprocal(out=Z[:], in_=Z[:]), reads=[BZ], writes=[BZ])
            S.op("dve", lambda e: e.tensor_tensor(out=kap[:], in0=e1[:], in1=Z[:], op=ALU.mult), reads=[Be1, BZ], writes=[Bkap])
            scv = sc[:].rearrange("p (g two) k -> p g (two k)", two=2)
            S.op("dve", lambda e: e.tensor_tensor(out=scv, in0=scv, in1=nh[:].unsqueeze(2).to_broadcast([128, NG, 256]), op=ALU.add),
                 reads=[B_sc, Bnh], writes=[B_sc])
            S.op("act", lambda e: e.activation(out=sc[:], in_=sc[:], func=AF.Exp), reads=[B_sc], writes=[B_sc])
            S.op("dve", lambda e: e.tensor_tensor(out=scv[:, :, 0:128], in0=scv[:, :, 0:128],
                                                  in1=kap[:].unsqueeze(2).to_broadcast([128, NG, 128]), op=ALU.mult),
                 reads=[B_sc, Bkap], writes=[B_sc])
            accO = [banks[0], banks[1], banks[2], banks[3]]
            for ic in range(128 // IC):
                bf = ic % 2
                (uc, Buc), (vc, Bvc) = utc[bf], vbc[bf]
                S.dma("sp", uc[:], ut_d[:, ic * IC:(ic + 1) * IC, :, :], Buc, reads=[B_ut], writes=[Buc])
                S.dma("sp", vc[:], vb_d[:, ic * IC:(ic + 1) * IC, :], Bvc, reads=[B_vb], writes=[Bvc])
                for g in range(NG):
                    (u_, Bu_) = ut_[cn["u"] % 2]
                    cn["u"] += 1
                    Fg, BFg = Ft[bf][g]
                    S.op("pool", lambda e, u_=u_, g=g, ic=ic: e.tensor_tensor(
                        out=u_[:].rearrange("p (i j) -> p i j", i=IC),
                        in0=scv[:, g, ic * IC:(ic + 1) * IC].unsqueeze(2).to_broadcast([128, IC, 128]),
                        in1=scv[:, g, 128:256].unsqueeze(1).to_broadcast([128, IC, 128]), op=ALU.mult),
                        reads=[B_sc], writes=[Bu_])
                    S.op("dve", lambda e, u_=u_, Fg=Fg, g=g: e.scalar_tensor_tensor(
                        out=Fg[:], in0=u_[:], scalar=kap[:, g:g + 1], in1=u_[:], op0=ALU.is_ge, op1=ALU.mult),
                        reads=[Bu_, Bkap], writes=[BFg])
                for il in range(IC):
                    et = ic * IC + il
                    hbk, Bhbk = banks[4 + et % 2]
                    gbk, Bgbk = banks[6 + et % 2]
                    S.mm_group([lambda pe, kc=kc, il=il, hbk=hbk, uc=uc: pe.matmul(hbk[:, :TB], lhsT=uc[:, il, kc, :], rhs=h2T[:, kc, :],
                                                                                 start=(kc == 0), stop=(kc == 7)) for kc in range(8)],
                               reads=[Buc, B_h2T], writes=[Bhbk])
                    S.mm_group([lambda pe, j=j, h=h, il=il, gbk=gbk, bf=bf: pe.matmul(
                        gbk[:, j * 128:(j + 1) * 128], lhsT=Ft[bf][j * 8 + h][0][:, il * 128:(il + 1) * 128], rhs=idb[:],
                        start=(j == 0 and h == 0), stop=(h == 7), skip_group_check=True) for j in range(NT) for h in range(8)],
                        reads=[Ft[bf][g][1] for g in range(NG)] + [B_idb], writes=[Bgbk])
                    (gl, Bgl), (ga, Bga) = gl_t[et % 2], ga_t[et % 2]
                    S.op("act", lambda e, gl=gl, hbk=hbk: e.activation(out=gl[:], in_=hbk[:, :TB], func=AF.Gelu_apprx_tanh),
                         reads=[Bhbk], writes=[Bgl])
                    S.op("dve", lambda e, gl=gl, ga=ga, gbk=gbk: e.tensor_tensor(out=ga[:], in0=gl[:], in1=gbk[:, :TB], op=ALU.mult),
                         reads=[Bgl, Bgbk], writes=[Bga])
                    for j in range(NT):
                        for half in range(2):
                            ab, Bab = accO[j * 2 + half]
                            S.mm_group([lambda pe, j=j, half=half, ab=ab, ga=ga, vc=vc, il=il, et=et: pe.matmul(
                                ab[:], lhsT=ga[:, j * 128:(j + 1) * 128], rhs=vc[:, il, half * 512:(half + 1) * 512],
                                start=(et == 0), stop=(et == 127), skip_group_check=True)],
                                reads=[Bga, Bvc], **({"writes": [Bab]} if et == 0 else {"parts": [Bab]}))
            for j in range(NT):
                tt = blk * NT + j
                for half in range(2):
                    ab, Bab = accO[j * 2 + half]
                    S.op("dve", lambda e, ab=ab, half=half: e.tensor_tensor(out=xo[:], in0=ab[:], in1=G2[:, half * 512:(half + 1) * 512], op=ALU.mult),
                         reads=[Bab, BG2], writes=[Bxo])
                    S.op("dve", lambda e, half=half, tt=tt: e.tensor_tensor(
                        out=x_t[:, tt, half * 512:(half + 1) * 512], in0=x_t[:, tt, half * 512:(half + 1) * 512], in1=xo[:], op=ALU.add),
                        reads=[Bxo, B_xt[tt]], parts=[B_xt[tt]])
        S.barrier()
        release(m)

    def final_phase():
        m = mark()
        fg, Bfg = load_bc("fg_bc", fin_g.rearrange("(o n) -> o n", o=1), D)
        tmp = norm_tmp()
        (sq, Bsq), (ss, Bss), (rs, Brs), (hn, Bhn), (hb, Bhb) = tmp
        for tt in range(NTT):
            norm_tile(tt, fg, Bfg, None, None, None, None, 0, tmp, None)
            S.dma("sp", y_d[tt * 128:(tt + 1) * 128, :], hn[:], B_y, reads=[Bhn], parts=[B_y])
        S.barrier()
        release(m)

    def dump_x():
        for tt in range(NTT):
            S.dma("sp", y_d[tt * 128:(tt + 1) * 128, :], x_t[:, tt, :], B_y, reads=[B_xt[tt]], parts=[B_y])
        S.barrier()

    for l in range(l0, l1):
        mod_phase(l)
        if stop_after == "mod":
            S.dma("sp", y_d[0:128, :].rearrange("p (a n) -> p a n", a=1)[:, 0, :], x_t[:, 0, :], B_y, reads=[B_xt[0]], parts=[B_y])
            break
        mm, hT = mixer_phase(l)
        if stop_after == "h1":
            hf, Bhf = sb("hf", [128, 8, 128], F32)
            break
        release(mm)
        if stop_after == "mix":
            break
        peer_phase(l)
    if stop_after is None and last:
        final_phase()
    else:
        dump_x()
    S.finish()
    return nc


def t5_bucket_np(d):
    d = np.maximum(d, 0)
    ratio = np.log(np.maximum(d, 1).astype(np.float32) / np.float32(16)) / np.float32(math.log(2048 / 16))
    large = np.minimum(16 + (ratio * np.float32(16)).astype(np.int32), 31)
    return np.where(d < 16, d, large)


def make_btab(rel_bias):
    ext = np.concatenate([rel_bias.astype(np.float32), np.full((1, 32), -30000.0, np.float32)], axis=0)
    out = np.empty((128, TABCOLS), np.float32)
    k = np.arange(128)[:, None, None]
    q = np.arange(128)[None, None, :]
    for typ in range(4):
        nb = TAB_NB[typ]
        Dd = (np.arange(nb) - 3)[None, :, None]
        delta = 128 * Dd + q - k
        valid = delta >= 0
        if TAB_W[typ] is not None:
            r = TAB_R[typ]
            valid = valid & (delta % r == 0) & (delta // r <= 128)
        idx = np.where(valid, t5_bucket_np(delta), 32)
        for h in range(8):
            hg = typ * 8 + h
            o = tab_off(typ, h)
            out[:, o:o + nb * 128] = ext[idx, hg].reshape(128, nb * 128)
    return out


_CACHE = {}


def run_layers(inputs, xin, l0, l1, first, last, stop_after=None, cores=8, with_peer=True):
    nc = build(l0, l1, first, last, stop_after, with_peer)
    btab = make_btab(np.asarray(inputs["rel_bias"]))
    ident = np.eye(128, dtype=np.float32)
    shared = {k: np.ascontiguousarray(np.asarray(inputs[k], dtype=np.float32)) for k in
              ["w_ada", "b_ada", "norm1_g", "norm2_g", "w_in", "w_proj_a", "w_proj_b", "w_out", "lam_q1", "lam_k1",
               "lam_q2", "lam_k2", "subln_g", "peer_wq", "peer_subkeys", "peer_u", "peer_v", "final_g"]}
    if not with_peer:
        del shared["peer_u"], shared["peer_v"]
    shared["btab"] = btab
    shared["ident"] = ident
    c = np.asarray(inputs["c"], dtype=np.float32)
    in_maps = []
    for b in range(cores):
        mp = dict(shared)
        mp["x"] = np.ascontiguousarray(xin[b])
        mp["c"] = np.ascontiguousarray(c[b].reshape(8, 128).T)
        in_maps.append(mp)
    res = run_bass_kernel_spmd(nc, in_maps, core_ids=list(range(cores)))
    return np.stack([np.asarray(r["y"]) for r in res.results], axis=0)


def kernel(**inputs):
    x = np.asarray(inputs["x"], dtype=np.float32)
    out = run_layers(inputs, x, 0, DEPTH, True, True)
    return out.astype(np.float32)
```

```python
import math
import numpy as np
import concourse.bass as bass
import concourse.mybir as mybir
from concourse.bass_utils import run_bass_kernel_spmd

F32 = mybir.dt.float32
BF16 = mybir.dt.bfloat16
AF = mybir.ActivationFunctionType
ALU = mybir.AluOpType
AX = mybir.AxisListType

D = 1024
T = 2048
NTT = 16
DEPTH = 4
INW = 9728
NEXP = 16384
TAB_NB = [8, 11, 22, 22]
TAB_W = [128, 512, 2048, None]
TAB_R = [1, 4, 16, 1]
TAB_DMAX = [1, 4, 15, 15]
TABCOLS = sum(TAB_NB) * 128 * 8


def tab_off(typ, head):
    off = 0
    for t in range(typ):
        off += TAB_NB[t] * 128 * 8
    return off + head * TAB_NB[typ] * 128


class Buf:
    def __init__(self, name):
        self.name = name
        self.writers = {}
        self.readers = {}
        self.sem = None
        self.dma_total = 0


class Eng:
    def __init__(self, name, obj, sem):
        self.name = name
        self.obj = obj
        self.sem = sem
        self.cnt = 0
        self.waited = {}


class Sched:
    def __init__(self, nc):
        self.nc = nc
        self.sems = {}
        self.engs = {}
        for name, obj in [("pe", nc.tensor), ("act", nc.scalar), ("dve", nc.vector), ("pool", nc.gpsimd), ("sp", nc.sync)]:
            s = self.new_sem("e_" + name)
            self.engs[name] = Eng(name, obj, s)
        self.dma_bufs = []
        self.free_sems = []

    def recycle(self, b):
        if b.sem is not None:
            self.free_sems.append((b.sem, b.dma_total))
            self.dma_bufs.remove(b)
            b.sem = None

    def new_sem(self, name):
        s = self.nc.semaphore(name).__enter__()
        self.sems[id(s)] = s
        return s

    def _wait(self, eng, need):
        for sid, val in need.items():
            if eng.waited.get(sid, 0) < val:
                eng.obj.wait_ge(self.sems[sid], val)
                eng.waited[sid] = val

    def _need(self, own_sid, reads, writes, parts):
        need = {}

        def merge(d):
            for k, v in d.items():
                if need.get(k, 0) < v:
                    need[k] = v
        for b in reads:
            merge(b.writers)
        for b in writes:
            merge(b.writers)
            merge(b.readers)
        for b in parts:
            merge(b.readers)
            merge({k: v for k, v in b.writers.items() if k != own_sid})
        return need

    def _record(self, sid, val, reads, writes, parts):
        for b in reads:
            if b.readers.get(sid, 0) < val:
                b.readers[sid] = val
        for b in writes:
            b.writers = {sid: val}
            b.readers = {}
        for b in parts:
            b.writers[sid] = val

    def op(self, engname, fn, reads=(), writes=(), parts=()):
        eng = self.engs[engname]
        sid = id(eng.sem)
        self._wait(eng, self._need(sid, reads, writes, parts))
        ins = fn(eng.obj)
        ins.then_inc(eng.sem, 1)
        eng.cnt += 1
        self._record(sid, eng.cnt, reads, writes, parts)

    def mm_group(self, mms, reads, writes=(), parts=()):
        eng = self.engs["pe"]
        sid = id(eng.sem)
        self._wait(eng, self._need(sid, reads, writes, parts))
        ins = None
        for f in mms:
            ins = f(eng.obj)
        ins.then_inc(eng.sem, 1)
        eng.cnt += 1
        self._record(sid, eng.cnt, reads, writes, parts)

    def dma(self, qname, out_ap, in_ap, owner, reads=(), writes=(), parts=()):
        eng = self.engs[qname]
        if owner.sem is None:
            if self.free_sems:
                owner.sem, owner.dma_total = self.free_sems.pop()
            else:
                owner.sem = self.new_sem("d_" + owner.name)
            self.dma_bufs.append(owner)
        sid = id(owner.sem)
        self._wait(eng, self._need(sid, reads, writes, parts))
        eng.obj.dma_start(out=out_ap, in_=in_ap).then_inc(owner.sem, 16)
        owner.dma_total += 16
        self._record(sid, owner.dma_total, reads, writes, parts)

    def barrier(self):
        allv = {}
        for e in self.engs.values():
            if e.cnt:
                allv[id(e.sem)] = e.cnt
        for b in self.dma_bufs:
            allv[id(b.sem)] = b.dma_total
        for e in self.engs.values():
            self._wait(e, allv)

    def finish(self):
        self.barrier()


def build(l0, l1, first, last, stop_after=None, with_peer=True):
    nc = bass.Bass("TRN2", target_bir_lowering=False)
    S = Sched(nc)

    def din(name, shape, dt=F32):
        return nc.dram_tensor(name, list(shape), dt, kind="ExternalInput").ap()

    x_d = din("x", [T, D])
    c_d = din("c", [128, 8])
    w_ada = din("w_ada", [DEPTH, D, 6 * D])
    b_ada = din("b_ada", [DEPTH, 6 * D])
    n1g = din("norm1_g", [DEPTH, D])
    n2g = din("norm2_g", [DEPTH, D])
    w_in = din("w_in", [DEPTH, D, INW])
    w_pa = din("w_proj_a", [DEPTH, 512, D])
    w_pb = din("w_proj_b", [DEPTH, D, D])
    w_o = din("w_out", [DEPTH, D, D])
    lq1 = din("lam_q1", [DEPTH, 64])
    lk1 = din("lam_k1", [DEPTH, 64])
    lq2 = din("lam_q2", [DEPTH, 64])
    lk2 = din("lam_k2", [DEPTH, 64])
    subg = din("subln_g", [DEPTH, 128])
    btab = din("btab", [128, TABCOLS])
    p_wq = din("peer_wq", [DEPTH, D, 2048])
    p_sk = din("peer_subkeys", [DEPTH, 2, 128, 128])
    p_u = din("peer_u", [DEPTH, NEXP, D]) if with_peer else None
    p_v = din("peer_v", [DEPTH, NEXP, D]) if with_peer else None
    fin_g = din("final_g", [D])
    ident_d = din("ident", [128, 128])
    y_d = nc.dram_tensor("y", [T, D], F32, kind="ExternalOutput").ap()

    etab_d = nc.dram_tensor("etab", [128, TABCOLS], BF16, kind="Internal").ap()
    mod_d = nc.dram_tensor("modscr", [128, 6 * D], F32, kind="Internal").ap()
    ut_d = nc.dram_tensor("utscr", [128, 128, 8, 128], BF16, kind="Internal").ap()
    vb_d = nc.dram_tensor("vbscr", [128, 128, D], BF16, kind="Internal").ap()
    scx_d = nc.dram_tensor("scxscr", [NTT, 128, 8, 256], F32, kind="Internal").ap()
    kap_d = nc.dram_tensor("kapscr", [NTT, 128, 8], F32, kind="Internal").ap()
    B_etab, B_mod, B_ut, B_vb, B_y = Buf("etab"), Buf("modscr"), Buf("utscr"), Buf("vbscr"), Buf("y")
    B_scx, B_kapd = Buf("scxscr"), Buf("kapscr")

    _ctx = []

    _uid = [0]

    def sb(name, shape, dt):
        _uid[0] += 1
        name = "s%d_%s" % (_uid[0], name)
        cm = nc.sbuf_tensor(name, list(shape), dt)
        t = cm.__enter__()
        b = Buf(name)
        _ctx.append((cm, b))
        return t, b

    def mark():
        return len(_ctx)

    def release(m):
        while len(_ctx) > m:
            cm, b = _ctx.pop()
            S.recycle(b)
            cm.__exit__(None, None, None)

    banks = []
    for i in range(8):
        t = nc.psum_tensor("bank%d" % i, [128, 512], F32).__enter__()
        banks.append((t, Buf("bank%d" % i)))

    x_t, B_x = sb("x", [128, NTT, D], F32)
    B_xt = [Buf("x%d" % i) for i in range(NTT)]
    idb, B_idb = sb("idb", [128, 128], BF16)
    idf, B_idf = sb("idf", [128, 128], F32)
    condrep, B_condrep = sb("condrep", [128, 8, 128], F32)
    lam_t, B_lam = sb("lam", [128, 4], F32)

    S.dma("pool", idb[:], ident_d[:, :], B_idb, writes=[B_idb])
    S.dma("sp", idf[:], ident_d[:, :], B_idf, writes=[B_idf])
    if first:
        for tt in range(NTT):
            S.dma("sp", x_t[:, tt, :], x_d[tt * 128:(tt + 1) * 128, :], B_xt[tt], writes=[B_xt[tt]])
    else:
        for tt in range(NTT):
            S.dma("sp", x_t[:, tt, :], x_d[tt * 128:(tt + 1) * 128, :], B_xt[tt], writes=[B_xt[tt]])

    m0 = mark()
    c_t, B_c = sb("c_t", [128, 8], F32)
    cs_t, B_cs = sb("cs_t", [128, 8], F32)
    S.dma("sp", c_t[:], c_d[:, :], B_c, writes=[B_c])
    S.op("act", lambda e: e.activation(out=cs_t[:], in_=c_t[:], func=AF.Silu), reads=[B_c], writes=[B_cs])
    S.op("dve", lambda e: e.tensor_copy(out=condrep[:], in_=cs_t[:].unsqueeze(2).to_broadcast([128, 8, 128])),
         reads=[B_cs], writes=[B_condrep])
    CH = 2048
    tb_f = [sb("tbf%d" % i, [128, CH], F32) for i in range(2)]
    tb_b = [sb("tbb%d" % i, [128, CH], BF16) for i in range(2)]
    nch = (TABCOLS + CH - 1) // CH
    for ci in range(nch):
        c0 = ci * CH
        cw = min(CH, TABCOLS - c0)
        tf, Bf = tb_f[ci % 2]
        tbb, Bb = tb_b[ci % 2]
        S.dma("sp", tf[:, :cw], btab[:, c0:c0 + cw], Bf, writes=[Bf])
        S.op("act", lambda e, tf=tf, tbb=tbb, cw=cw: e.activation(out=tbb[:, :cw], in_=tf[:, :cw], func=AF.Exp),
             reads=[Bf], writes=[Bb])
        S.dma("sp", etab_d[:, c0:c0 + cw], tbb[:, :cw], B_etab, reads=[Bb], parts=[B_etab])
    S.barrier()
    release(m0)

    def mod_phase(l):
        m = mark()
        wt = [sb("modw%d" % i, [128, 512], F32) for i in range(3)]
        bt = [sb("modb%d" % i, [128, 512], F32) for i in range(2)]
        mo = [sb("modo%d" % i, [128, 512], F32) for i in range(2)]
        k = 0
        for nchk in range(12):
            ps, Bps = banks[nchk % 2]
            btile, Bbt = bt[nchk % 2]
            S.dma("sp", btile[:], b_ada[l:l + 1, nchk * 512:(nchk + 1) * 512].partition_broadcast(128), Bbt, writes=[Bbt])
            for kc in range(8):
                w, Bw = wt[k % 3]
                k += 1
                S.dma("sp", w[:], w_ada[l, kc * 128:(kc + 1) * 128, nchk * 512:(nchk + 1) * 512], Bw, writes=[Bw])
                S.mm_group([lambda pe, kc=kc, w=w, ps=ps: pe.matmul(ps[:], lhsT=condrep[:, kc, :], rhs=w[:],
                                                                    start=(kc == 0), stop=(kc == 7))],
                           reads=[Bw, B_condrep], **({"writes": [Bps]} if kc == 0 else {"parts": [Bps]}))
            o, Bo = mo[nchk % 2]
            S.op("dve", lambda e, o=o, ps=ps, btile=btile: e.tensor_tensor(out=o[:], in0=ps[:], in1=btile[:], op=ALU.add),
                 reads=[Bps, Bbt], writes=[Bo])
            S.dma("sp", mod_d[:, nchk * 512:(nchk + 1) * 512], o[:], B_mod, reads=[Bo], parts=[B_mod])
        lt = [sb("lamv%d" % i, [128, 64], F32) for i in range(4)]
        for i, src in enumerate([lq1, lk1, lq2, lk2]):
            S.dma("sp", lt[i][0][:], src[l:l + 1, :].partition_broadcast(128), lt[i][1], writes=[lt[i][1]])
        pr, Bpr = sb("lampr", [128, 2, 64], F32)
        dd, Bdd = sb("lamdd", [128, 2], F32)
        ee, Bee = sb("lamee", [128, 2], F32)
        S.op("dve", lambda e: e.tensor_tensor(out=pr[:, 0, :], in0=lt[0][0][:], in1=lt[1][0][:], op=ALU.mult),
             reads=[lt[0][1], lt[1][1]], parts=[Bpr])
        S.op("dve", lambda e: e.tensor_tensor(out=pr[:, 1, :], in0=lt[2][0][:], in1=lt[3][0][:], op=ALU.mult),
             reads=[lt[2][1], lt[3][1]], parts=[Bpr])
        S.op("dve", lambda e: e.reduce_sum(out=dd[:], in_=pr[:], axis=AX.X), reads=[Bpr], writes=[Bdd])
        S.op("act", lambda e: e.activation(out=ee[:], in_=dd[:], func=AF.Exp), reads=[Bdd], writes=[Bee])
        lam_init = 0.8 - 0.6 * math.exp(-0.3 * l)
        S.op("dve", lambda e: e.scalar_tensor_tensor(out=lam_t[:, 0:1], in0=ee[:, 1:2], scalar=-lam_init, in1=ee[:, 0:1],
                                                     op0=ALU.add, op1=ALU.subtract),
             reads=[Bee], writes=[B_lam])
        S.barrier()
        release(m)

    def norm_tile(tt, A, BA, Bb, BBb, hT, B_hT, col0, tmp, ps_bank):
        (sq, Bsq), (ss, Bss), (rs, Brs), (hn, Bhn), (hb, Bhb) = tmp
        S.op("act", lambda e: e.activation(out=sq[:], in_=x_t[:, tt, :], func=AF.Square, accum_out=ss[:]),
             reads=[B_xt[tt]], writes=[Bsq, Bss])
        S.op("act", lambda e: e.activation(out=rs[:], in_=ss[:], func=AF.Sqrt, scale=1.0 / D, bias=EPS_T[:, 0:1]),
             reads=[Bss, B_eps], writes=[Brs])
        S.op("dve", lambda e: e.reciprocal(out=rs[:], in_=rs[:]), reads=[Brs], writes=[Brs])
        S.op("dve", lambda e: e.scalar_tensor_tensor(out=hn[:], in0=x_t[:, tt, :], scalar=rs[:, 0:1], in1=A[:],
                                                     op0=ALU.mult, op1=ALU.mult),
             reads=[B_xt[tt], Brs, BA], writes=[Bhn])
        if Bb is not None:
            S.op("pool", lambda e: e.tensor_tensor(out=hb[:], in0=hn[:], in1=Bb[:], op=ALU.add),
                 reads=[Bhn, BBb], writes=[Bhb])
        if hT is None:
            return
        ps, Bps = ps_bank
        psb = ps[:].bitcast(BF16)
        S.mm_group([lambda pe, dc=dc: pe.transpose(out=psb[:, dc * 128:(dc + 1) * 128], in_=hb[:, dc * 128:(dc + 1) * 128],
                                                   identity=idb[:]) for dc in range(8)],
                   reads=[Bhb, B_idb], writes=[Bps])
        S.op("act", lambda e: e.activation(out=hT[:, :, col0:col0 + 128],
                                           in_=psb.rearrange("p (a b) -> p a b", a=8), func=AF.Copy),
             reads=[Bps], parts=[B_hT])

    EPS_T, B_eps = sb("eps", [128, 2], F32)
    S.op("dve", lambda e: e.memset(EPS_T[:, 0:1], 1e-6), parts=[B_eps])
    S.op("dve", lambda e: e.memset(EPS_T[:, 1:2], 1e-5), parts=[B_eps])

    def load_bc(name, src_row_ap, n, q="sp"):
        t, Bt = sb(name, [128, n], F32)
        S.dma(q, t[:], src_row_ap.partition_broadcast(128), Bt, writes=[Bt])
        return t, Bt

    def norm_tmp():
        return [sb("n_sq", [128, D], BF16), sb("n_ss", [128, 1], F32), sb("n_rs", [128, 1], F32),
                sb("n_hn", [128, D], F32), sb("n_hb", [128, D], BF16)]

    def make_AB(l, which):
        base = 0 if which == 1 else 3 * D
        A, BA = sb("A_bc", [128, D], F32)
        Bt, BB = sb("B_bc", [128, D], F32)
        g, Bg = load_bc("ng_bc", (n1g if which == 1 else n2g)[l:l + 1, :], D)
        S.dma("sp", A[:], mod_d[:, base + D:base + 2 * D], BA, reads=[B_mod], writes=[BA])
        S.dma("sp", Bt[:], mod_d[:, base:base + D], BB, reads=[B_mod], writes=[BB])
        S.op("dve", lambda e: e.scalar_tensor_tensor(out=A[:], in0=A[:], scalar=1.0, in1=g[:], op0=ALU.add, op1=ALU.mult),
             reads=[BA, Bg], writes=[BA])
        return A, BA, Bt, BB

    def mixer_phase(l):
        m = mark()
        hT, B_hT = sb("hT", [128, 8, T], BF16)
        oaT, B_oaT = sb("oaT", [128, 4, T], BF16)
        obT, B_obT = sb("obT", [128, 8, T], BF16)
        m1 = mark()
        A, BA, Bt, BB = make_AB(l, 1)
        tmp = norm_tmp()
        for tt in range(NTT):
            norm_tile(tt, A, BA, Bt, BB, hT, B_hT, tt * 128, tmp, banks[7])
        S.barrier()
        release(m1)
        if stop_after == "h1":
            return m, hT
        m2 = mark()
        wq = [sb("wq%d" % i, [128, 8, 64], BF16) for i in range(2)]
        wk = [sb("wk%d" % i, [128, 8, 64], BF16) for i in range(2)]
        wv = [sb("wv%d" % i, [128, 8, 128], BF16) for i in range(2)]
        qT = [sb("qT%d" % i, [128, T], BF16) for i in range(2)]
        kT = [sb("kT%d" % i, [128, T], BF16) for i in range(2)]
        vx = [sb("vx0", [128, NTT, 129], BF16)] * 2
        tab = [sb("tab0", [128, 22 * 128], BF16)] * 2
        pe_t = [sb("pexp%d" % i, [128, 512], BF16) for i in range(2)]
        pm_t = [sb("pm%d" % i, [128, 512], BF16) for i in range(2)]
        acc_sb, B_acc = sb("accsb", [128, NTT, 65], F32)
        otmp, B_otmp = sb("otmp", [128, NTT, 128], F32)
        rr = [sb("rr%d" % i, [128, 4], F32) for i in range(2)]
        osb = [sb("osb%d" % i, [128, 128], BF16) for i in range(2)]
        oa2, B_oa2 = sb("oa2", [128, NTT, 128], BF16)
        sg, Bsg = load_bc("subg", subg[l:l + 1, :], 128)
        lam_init = 0.8 - 0.6 * math.exp(-0.3 * l)
        S.op("dve", lambda e: e.tensor_scalar(out=sg[:], in0=sg[:], scalar1=1.0 - lam_init, scalar2=None, op0=ALU.mult),
             reads=[Bsg], writes=[Bsg])
        S.op("pool", lambda e: e.memset(vx[0][0][:], 1.0), writes=[vx[0][1]])
        ctr = {"st": 0, "sc": 0, "rr": 0, "os": 0}

        def stream1(si, qcol, kcol, vcol, dh, dv, typ, thead, fin):
            (wq_t, Bwq), (wk_t, Bwk), (wv_t, Bwv) = wq[si], wk[si], wv[si]
            (q_t, Bq), (k_t, Bk) = qT[si], kT[si]
            wsrc = w_in[l].rearrange("(a p) n -> p a n", p=128)
            S.dma("pool", wq_t[:, :, :dh], wsrc[:, :, qcol:qcol + dh], Bwq, writes=[Bwq])
            S.dma("pool", wk_t[:, :, :dh], wsrc[:, :, kcol:kcol + dh], Bwk, writes=[Bwk])
            S.dma("pool", wv_t[:, :, :dv], wsrc[:, :, vcol:vcol + dv], Bwv, writes=[Bwv])
            for (w_t, Bw, o_t, Bo) in ((wq_t, Bwq, q_t, Bq), (wk_t, Bwk, k_t, Bk)):
                for qc in range(4):
                    ps, Bps = banks[4 + (qc % 2)]
                    S.mm_group([lambda pe, kc=kc, w_t=w_t, ps=ps, qc=qc: pe.matmul(
                        ps[:dh, :], lhsT=w_t[:, kc, :dh], rhs=hT[:, kc, qc * 512:(qc + 1) * 512],
                        start=(kc == 0), stop=(kc == 7)) for kc in range(8)],
                        reads=[Bw, B_hT], writes=[Bps])
                    S.op("act", lambda e, o_t=o_t, ps=ps, qc=qc: e.activation(
                        out=o_t[:dh, qc * 512:(qc + 1) * 512], in_=ps[:dh, :], func=AF.Copy),
                        reads=[Bps], parts=[Bo])

        def stream2(si, qcol, kcol, vcol, dh, dv, typ, thead, fin):
            (wq_t, Bwq), (wk_t, Bwk), (wv_t, Bwv) = wq[si], wk[si], wv[si]
            (q_t, Bq), (k_t, Bk), (v_t, Bv), (tb_t, Btb) = qT[si], kT[si], vx[si], tab[si]
            nb = TAB_NB[typ]
            to = tab_off(typ, thead)
            S.dma("sp", tb_t[:, :nb * 128], etab_d[:, to:to + nb * 128], Btb, reads=[B_etab], writes=[Btb])
            for tt in range(NTT):
                ps, Bps = banks[6 + (tt % 2)]
                S.mm_group([lambda pe, kc=kc, ps=ps, tt=tt: pe.matmul(
                    ps[:, :dv], lhsT=hT[:, kc, tt * 128:(tt + 1) * 128], rhs=wv_t[:, kc, :dv],
                    start=(kc == 0), stop=(kc == 7)) for kc in range(8)],
                    reads=[Bwv, B_hT], writes=[Bps])
                S.op("dve", lambda e, ps=ps, tt=tt: e.tensor_copy(out=v_t[:, tt, :dv], in_=ps[:, :dv]),
                     reads=[Bps], parts=[Bv])
            if dv < 128:
                S.op("pool", lambda e: e.memset(v_t[:, :, dv:dv + 1], 1.0), parts=[Bv])
            dmax = TAB_DMAX[typ]
            for qc in range(4):
                accb = [banks[2], banks[3]]
                first_in = [True, True]
                kts = [kt for kt in range(NTT) if any(0 <= 4 * qc + j - kt <= dmax for j in range(4))]
                lastkt = {}
                for kt in kts:
                    for j in range(4):
                        if 0 <= 4 * qc + j - kt <= dmax:
                            lastkt[j] = kt
                for kt in kts:
                    pi = ctr["sc"] % 2
                    ctr["sc"] += 1
                    ps, Bps = banks[pi]
                    (pex, Bpex), (pm, Bpm) = pe_t[pi], pm_t[pi]
                    S.mm_group([lambda pe, ps=ps, kt=kt, qc=qc: pe.matmul(
                        ps[:, :], lhsT=k_t[:dh, kt * 128:(kt + 1) * 128], rhs=q_t[:dh, qc * 512:(qc + 1) * 512],
                        start=True, stop=True)], reads=[Bq, Bk], writes=[Bps])
                    S.op("act", lambda e, ps=ps, pex=pex: e.activation(out=pex[:], in_=ps[:], func=AF.Exp, scale=0.125),
                         reads=[Bps], writes=[Bpex])
                    d0 = 4 * qc - kt
                    S.op("dve", lambda e, pm=pm, pex=pex, d0=d0: e.tensor_tensor(
                        out=pm[:], in0=pex[:], in1=tb_t[:, (d0 + 3) * 128:(d0 + 3) * 128 + 512], op=ALU.mult),
                        reads=[Bpex, Btb], writes=[Bpm])
                    mms = []
                    for j in range(4):
                        dd_ = 4 * qc + j - kt
                        if not (0 <= dd_ <= dmax):
                            continue
                        bi = j // 2
                        ab, Bab = accb[bi]
                        st = first_in[bi]
                        first_in[bi] = False
                        c0 = (j % 2) * (dv + 1)
                        mms.append((bi, lambda pe, ab=ab, pm=pm, j=j, kt=kt, st=st, c0=c0: pe.matmul(
                            ab[:, c0:c0 + dv + 1], lhsT=pm[:, j * 128:(j + 1) * 128], rhs=v_t[:, kt, :dv + 1],
                            start=st, stop=(lastkt[j] == kt), skip_group_check=True)))
                    for bi in (0, 1):
                        fl = [f for (b_, f) in mms if b_ == bi]
                        if fl:
                            S.mm_group(fl, reads=[Bpm, Bv], parts=[accb[bi][1]])
                fin(qc, accb, dv)

        def fin_dil(first_s, last_s, pairslot, pair_idx):
            def f(qc, accb, dv):
                for j in range(4):
                    tt = 4 * qc + j
                    ab, Bab = accb[j // 2]
                    c0 = (j % 2) * 65
                    if first_s:
                        S.op("dve", lambda e, ab=ab, tt=tt, c0=c0: e.tensor_copy(out=acc_sb[:, tt, :65], in_=ab[:, c0:c0 + 65]),
                             reads=[Bab], parts=[B_acc])
                    else:
                        S.op("dve", lambda e, ab=ab, tt=tt, c0=c0: e.tensor_tensor(
                            out=acc_sb[:, tt, :65], in0=acc_sb[:, tt, :65], in1=ab[:, c0:c0 + 65], op=ALU.add),
                            reads=[Bab, B_acc], parts=[B_acc])
                    if last_s:
                        r, Br = rr[ctr["rr"] % 2]
                        ctr["rr"] += 1
                        S.op("dve", lambda e, r=r, tt=tt: e.reciprocal(out=r[:, 0:1], in_=acc_sb[:, tt, 64:65]),
                             reads=[B_acc], writes=[Br])
                        S.op("dve", lambda e, r=r, tt=tt: e.tensor_scalar(
                            out=oa2[:, tt, pairslot * 64:(pairslot + 1) * 64], in0=acc_sb[:, tt, :64],
                            scalar1=r[:, 0:1], scalar2=None, op0=ALU.mult),
                            reads=[Br, B_acc], parts=[B_oa2])
                        if pairslot == 1:
                            ps, Bps = banks[6]
                            psb = ps[:].bitcast(BF16)
                            S.mm_group([lambda pe, tt=tt: pe.transpose(out=psb[:, 0:128], in_=oa2[:, tt, :], identity=idb[:])],
                                       reads=[B_oa2, B_idb], writes=[Bps])
                            S.op("act", lambda e, tt=tt: e.activation(out=oaT[:, pair_idx, tt * 128:(tt + 1) * 128],
                                                                      in_=psb[:, 0:128], func=AF.Copy),
                                 reads=[Bps], parts=[B_oaT])
            return f

        def fin_diff(s, head):
            def f(qc, accb, dv):
                for j in range(4):
                    tt = 4 * qc + j
                    ab, Bab = accb[j // 2]
                    c0 = (j % 2) * 129
                    r, Br = rr[ctr["rr"] % 2]
                    ctr["rr"] += 1
                    S.op("dve", lambda e, r=r, ab=ab, c0=c0: e.reciprocal(out=r[:, 0:1], in_=ab[:, c0 + 128:c0 + 129]),
                         reads=[Bab], writes=[Br])
                    if s == 0:
                        S.op("dve", lambda e, r=r, ab=ab, c0=c0, tt=tt: e.tensor_scalar(
                            out=otmp[:, tt, :], in0=ab[:, c0:c0 + 128], scalar1=r[:, 0:1], scalar2=None, op0=ALU.mult),
                            reads=[Br, Bab], parts=[B_otmp])
                    else:
                        S.op("dve", lambda e, r=r: e.tensor_tensor(out=r[:, 1:2], in0=r[:, 0:1], in1=lam_t[:, 0:1], op=ALU.mult),
                             reads=[Br, B_lam], writes=[Br])
                        S.op("dve", lambda e, r=r, ab=ab, c0=c0, tt=tt: e.scalar_tensor_tensor(
                            out=otmp[:, tt, :], in0=ab[:, c0:c0 + 128], scalar=r[:, 1:2], in1=otmp[:, tt, :],
                            op0=ALU.mult, op1=ALU.add),
                            reads=[Br, Bab, B_otmp], parts=[B_otmp])
                        o_b, Bob = osb[ctr["os"] % 2]
                        ctr["os"] += 1
                        S.op("act", lambda e, o_b=o_b, r=r, tt=tt: e.activation(out=o_b[:], in_=otmp[:, tt, :], func=AF.Square,
                                                                                accum_out=r[:, 2:3]),
                             reads=[B_otmp], writes=[Bob, Br])
                        S.op("act", lambda e, r=r: e.activation(out=r[:, 3:4], in_=r[:, 2:3], func=AF.Sqrt, scale=1.0 / 128,
                                                                bias=EPS_T[:, 1:2]),
                             reads=[Br, B_eps], writes=[Br])
                        S.op("dve", lambda e, r=r: e.reciprocal(out=r[:, 3:4], in_=r[:, 3:4]), reads=[Br], writes=[Br])
                        S.op("dve", lambda e, o_b=o_b, r=r, tt=tt: e.scalar_tensor_tensor(
                            out=o_b[:], in0=otmp[:, tt, :], scalar=r[:, 3:4], in1=sg[:], op0=ALU.mult, op1=ALU.mult),
                            reads=[Br, B_otmp, Bsg], writes=[Bob])
                        ps, Bps = banks[6]
                        psb = ps[:].bitcast(BF16)
                        S.mm_group([lambda pe, o_b=o_b: pe.transpose(out=psb[:, 0:128], in_=o_b[:], identity=idb[:])],
                                   reads=[Bob, B_idb], writes=[Bps])
                        S.op("act", lambda e, tt=tt: e.activation(out=obT[:, head, tt * 128:(tt + 1) * 128],
                                                                  in_=psb[:, 0:128], func=AF.Copy),
                             reads=[Bps], parts=[B_obT])
            return f

        jobs = []
        for hh in range(8):
            for g in range(3):
                qcol = g * 512 + hh * 64
                jobs.append((qcol, 1536 + qcol, 3072 + qcol, 64, 64, g, hh, fin_dil(g == 0, g == 2, hh % 2, hh // 2)))
        for hd in range(8):
            for s in range(2):
                qcol = 4608 + hd * 128 + s * 64
                jobs.append((qcol, qcol + 1024, 6656 + hd * 128, 64, 128, 3, hd, fin_diff(s, hd)))
        stream1(0, *jobs[0])
        for ji, jb in enumerate(jobs):
            if ji + 1 < len(jobs):
                stream1((ji + 1) % 2, *jobs[ji + 1])
            stream2(ji % 2, *jb)
        S.barrier()
        release(m2)
        m3 = mark()
        G1, BG1 = sb("G1", [128, D], F32)
        S.dma("sp", G1[:], mod_d[:, 2 * D:3 * D], BG1, reads=[B_mod], writes=[BG1])
        wga, Bwga = sb("wga", [128, 8, 512], BF16)
        wgb, Bwgb = sb("wgb", [128, 8, 512], BF16)
        wpa, Bwpa = sb("wpa", [128, 4, 512], BF16)
        wpb, Bwpb = sb("wpb", [128, 8, 512], BF16)
        wo, Bwo = sb("wo", [128, 4, D], BF16)
        sga = [sb("sga%d" % i, [128, 512], F32) for i in range(2)]
        sgb = [sb("sgb%d" % i, [128, 512], F32) for i in range(2)]
        mg = [sb("mg%d" % i, [128, 512], BF16) for i in range(2)]
        mgT = [sb("mgT%d" % i, [128, 4, 128], BF16) for i in range(2)]
        wsrc = w_in[l].rearrange("(a p) n -> p a n", p=128)
        for nchk in range(2):
            n0 = nchk * 512
            S.dma("pool", wga[:], wsrc[:, :, 7680 + n0:7680 + n0 + 512], Bwga, writes=[Bwga])
            S.dma("pool", wgb[:], wsrc[:, :, 8704 + n0:8704 + n0 + 512], Bwgb, writes=[Bwgb])
            S.dma("pool", wpa[:], w_pa[l].rearrange("(a p) n -> p a n", p=128)[:, :, n0:n0 + 512], Bwpa, writes=[Bwpa])
            S.dma("pool", wpb[:], w_pb[l].rearrange("(a p) n -> p a n", p=128)[:, :, n0:n0 + 512], Bwpb, writes=[Bwpb])
            S.dma("pool", wo[:], w_o[l, n0:n0 + 512, :].rearrange("(a p) n -> p a n", p=128), Bwo, writes=[Bwo])
            for tt in range(NTT):
                i2 = tt % 2
                tsl = slice(tt * 128, (tt + 1) * 128)
                (pga, Bpga), (pgb, Bpgb), (ppa, Bppa), (ppb, Bppb) = banks[0], banks[1], banks[2], banks[3]
                S.mm_group([lambda pe, kc=kc: pe.matmul(pga[:], lhsT=hT[:, kc, tsl], rhs=wga[:, kc, :], start=(kc == 0), stop=(kc == 7))
                            for kc in range(8)], reads=[B_hT, Bwga], writes=[Bpga])
                S.mm_group([lambda pe, kc=kc: pe.matmul(pgb[:], lhsT=hT[:, kc, tsl], rhs=wgb[:, kc, :], start=(kc == 0), stop=(kc == 7))
                            for kc in range(8)], reads=[B_hT, Bwgb], writes=[Bpgb])
                S.mm_group([lambda pe, kc=kc: pe.matmul(ppa[:], lhsT=oaT[:, kc, tsl], rhs=wpa[:, kc, :], start=(kc == 0), stop=(kc == 3))
                            for kc in range(4)], reads=[B_oaT, Bwpa], writes=[Bppa])
                S.mm_group([lambda pe, kc=kc: pe.matmul(ppb[:], lhsT=obT[:, kc, tsl], rhs=wpb[:, kc, :], start=(kc == 0), stop=(kc == 7))
                            for kc in range(8)], reads=[B_obT, Bwpb], writes=[Bppb])
                (a_, Ba_), (b_, Bb_), (mg_, Bmg), (mgT_, BmgT) = sga[i2], sgb[i2], mg[i2], mgT[i2]
                S.op("act", lambda e, a_=a_: e.activation(out=a_[:], in_=pga[:], func=AF.Sigmoid), reads=[Bpga], writes=[Ba_])
                S.op("act", lambda e, b_=b_: e.activation(out=b_[:], in_=pgb[:], func=AF.Sigmoid), reads=[Bpgb], writes=[Bb_])
                S.op("dve", lambda e, a_=a_: e.tensor_tensor(out=a_[:], in0=a_[:], in1=ppa[:], op=ALU.mult), reads=[Ba_, Bppa], writes=[Ba_])
                S.op("dve", lambda e, b_=b_: e.tensor_tensor(out=b_[:], in0=b_[:], in1=ppb[:], op=ALU.mult), reads=[Bb_, Bppb], writes=[Bb_])
                S.op("pool", lambda e, a_=a_, b_=b_, mg_=mg_: e.tensor_tensor(out=mg_[:], in0=a_[:], in1=b_[:], op=ALU.add),
                     reads=[Ba_, Bb_], writes=[Bmg])
                ps, Bps = banks[6]
                psb = ps[:].bitcast(BF16)
                S.mm_group([lambda pe, c=c, mg_=mg_: pe.transpose(out=psb[:, c * 128:(c + 1) * 128], in_=mg_[:, c * 128:(c + 1) * 128],
                                                                  identity=idb[:]) for c in range(4)],
                           reads=[Bmg, B_idb], writes=[Bps])
                S.op("act", lambda e, mgT_=mgT_: e.activation(out=mgT_[:], in_=psb[:, 0:512].rearrange("p (a b) -> p a b", a=4), func=AF.Copy),
                     reads=[Bps], writes=[BmgT])
                for half in range(2):
                    po, Bpo = banks[4 + half]
                    S.mm_group([lambda pe, c=c, mgT_=mgT_, po=po, half=half: pe.matmul(
                        po[:], lhsT=mgT_[:, c, :], rhs=wo[:, c, half * 512:(half + 1) * 512], start=(c == 0), stop=(c == 3))
                        for c in range(4)], reads=[BmgT, Bwo], writes=[Bpo])
                    S.op("dve", lambda e, po=po, half=half, tt=tt: e.tensor_tensor(
                        out=po[:], in0=po[:], in1=G1[:, half * 512:(half + 1) * 512], op=ALU.mult),
                        reads=[Bpo, BG1], writes=[Bpo])
                    S.op("dve", lambda e, po=po, half=half, tt=tt: e.tensor_tensor(
                        out=x_t[:, tt, half * 512:(half + 1) * 512], in0=x_t[:, tt, half * 512:(half + 1) * 512], in1=po[:], op=ALU.add),
                        reads=[Bpo, B_xt[tt]], parts=[B_xt[tt]])
        S.barrier()
        release(m3)
        return m, hT


    def peer_prep_gen(l, bufs):
        ut_f, ut_b, v_f, v_b = bufs
        NB_ = len(ut_f)
        for et in range(128):
            i2 = et % NB_
            (uf, Buf_), (ub, Bub), (vf, Bvf), (vb_, Bvb) = ut_f[i2], ut_b[i2], v_f[i2], v_b[i2]
            S.dma("sp", uf[:], p_u[l, et * 128:(et + 1) * 128, :], Buf_, writes=[Buf_])
            S.dma("sp", vf[:], p_v[l, et * 128:(et + 1) * 128, :], Bvf, writes=[Bvf])
            for hb_ in range(2):
                ps, Bps = banks[2 + hb_]
                S.mm_group([lambda pe, c=c, ps=ps, uf=uf, hb_=hb_: pe.transpose(
                    out=ps[:, c * 128:(c + 1) * 128], in_=uf[:, (hb_ * 4 + c) * 128:(hb_ * 4 + c + 1) * 128], identity=idf[:])
                    for c in range(4)], reads=[Buf_, B_idf], writes=[Bps])
                S.op("act", lambda e, ps=ps, ub=ub, hb_=hb_: e.activation(
                    out=ub[:, hb_ * 4:(hb_ + 1) * 4, :], in_=ps[:].rearrange("p (a b) -> p a b", a=4), func=AF.Copy),
                    reads=[Bps], parts=[Bub])
            S.dma("sp", ut_d[:, et, :, :], ub[:], B_ut, reads=[Bub], parts=[B_ut])
            S.op("pool", lambda e, vb_=vb_, vf=vf: e.tensor_copy(out=vb_[:], in_=vf[:]), reads=[Bvf], writes=[Bvb])
            S.dma("sp", vb_d[:, et, :], vb_[:], B_vb, reads=[Bvb], parts=[B_vb])
            yield

    def peer_phase(l):
        m = mark()
        h2T, B_h2T = sb("h2T", [128, 8, T], BF16)
        G2, BG2 = sb("G2", [128, D], F32)
        S.dma("sp", G2[:], mod_d[:, 5 * D:6 * D], BG2, reads=[B_mod], writes=[BG2])
        mA = mark()
        pbufs = ([sb("utf%d" % i, [128, D], F32) for i in range(2)], [sb("utb%d" % i, [128, 8, 128], BF16) for i in range(2)],
                 [sb("vf%d" % i, [128, D], F32) for i in range(2)], [sb("vb%d" % i, [128, D], BF16) for i in range(2)])
        prep = peer_prep_gen(l, pbufs)
        A, BA, Bt, BB = make_AB(l, 2)
        tmp = norm_tmp()
        skT, B_skT = sb("skT", [128, 2, 128], F32)
        skl, B_skl = sb("skl", [128, 128], F32)
        for p in range(2):
            S.dma("sp", skl[:], p_sk[l, p, :, :], B_skl, writes=[B_skl])
            ps, Bps = banks[4]
            S.mm_group([lambda pe, ps=ps: pe.transpose(out=ps[:, 0:128], in_=skl[:], identity=idf[:])],
                       reads=[B_skl, B_idf], writes=[Bps])
            S.op("act", lambda e, ps=ps, p=p: e.activation(out=skT[:, p, :], in_=ps[:, 0:128], func=AF.Copy),
                 reads=[Bps], parts=[B_skT])
        wqf = [sb("wqf%d" % i, [128, 8, 128], F32) for i in range(2)]
        wqb = [sb("wqb%d" % i, [128, 8, 128], BF16) for i in range(2)]
        qTs = [sb("qTs%d" % i, [128, 128], F32) for i in range(2)]
        sc_l = [sb("sc%d" % i, [128, 16, 128], F32) for i in range(2)]
        sc2, B_sc2 = sb("sc2", [128, 16, 128], F32)
        T16, B_T16 = sb("T16", [128, 16, 16], F32)
        cand, B_cand = sb("cand", [128, 8, 256], F32)
        S24, B_S24 = sb("S24", [128, 8, 24], F32)
        sm, B_sm = sb("sm", [128, 8, 16], F32)
        sv_ = {}
        for nm in ["negM", "tau", "nh", "Z", "e1"]:
            sv_[nm] = sb(nm, [128, 8], F32)
        kap_l = [sb("kapA%d" % i, [128, 8], F32) for i in range(2)]
        wsrc = p_wq[l].rearrange("(a p) n -> p a n", p=128)
        cand2v = sc2[:].rearrange("p (g two) k -> p g (two k)", two=2)
        cn = {"w": 0}
        NG = 8
        for tt in range(NTT):
            for _ in range(8):
                next(prep, None)
            norm_tile(tt, A, BA, Bt, BB, h2T, B_h2T, tt * 128, tmp, banks[7])
            sc, B_sc = sc_l[tt % 2]
            kap, Bkap = kap_l[tt % 2]
            for hp in range(16):
                i2 = cn["w"] % 2
                cn["w"] += 1
                (wf, Bwf), (wb, Bwb), (qs, Bqs) = wqf[i2], wqb[i2], qTs[i2]
                S.dma("sp", wf[:], wsrc[:, :, hp * 128:(hp + 1) * 128], Bwf, writes=[Bwf])
                S.op("act", lambda e, wf=wf, wb=wb: e.activation(out=wb[:], in_=wf[:], func=AF.Copy), reads=[Bwf], writes=[Bwb])
                ps, Bps = banks[4 + i2]
                S.mm_group([lambda pe, kc=kc, ps=ps, wb=wb: pe.matmul(ps[:, :128], lhsT=wb[:, kc, :], rhs=h2T[:, kc, tt * 128:(tt + 1) * 128],
                                                                     start=(kc == 0), stop=(kc == 7)) for kc in range(8)],
                           reads=[Bwb, B_h2T], writes=[Bps])
                S.op("dve", lambda e, ps=ps, qs=qs: e.tensor_copy(out=qs[:], in_=ps[:, :128]), reads=[Bps], writes=[Bqs])
                ps2, Bps2 = banks[6 + (hp % 2)]
                S.mm_group([lambda pe, qs=qs, hp=hp, ps2=ps2: pe.matmul(ps2[:, 0:128], lhsT=qs[:], rhs=skT[:, hp % 2, :], start=True, stop=True)],
                           reads=[Bqs, B_skT], writes=[Bps2])
                S.op("act", lambda e, hp=hp, ps2=ps2, sc=sc: e.activation(out=sc[:, hp, :], in_=ps2[:, 0:128], func=AF.Copy),
                     reads=[Bps2], parts=[B_sc])
            for g in range(16):
                S.op("dve", lambda e, g=g: e.max(out=T16[:, g, 0:8], in_=sc[:, g, :]), reads=[B_sc], parts=[B_T16])
            for g in range(16):
                S.op("dve", lambda e, g=g: e.match_replace(out=sc2[:, g, :], in_to_replace=T16[:, g, 0:8], in_values=sc[:, g, :], imm_value=-1e30),
                     reads=[B_sc, B_T16], parts=[B_sc2])
            for g in range(16):
                S.op("dve", lambda e, g=g: e.max(out=T16[:, g, 8:16], in_=sc2[:, g, :]), reads=[B_sc2], parts=[B_T16])
            T16v = T16[:].rearrange("p (g two) r -> p g two r", two=2)
            S.op("dve", lambda e: e.tensor_tensor(out=cand[:].rearrange("p g (r s) -> p g r s", r=16),
                                                  in0=T16v[:, :, 0, :].unsqueeze(3).to_broadcast([128, NG, 16, 16]),
                                                  in1=T16v[:, :, 1, :].unsqueeze(2).to_broadcast([128, NG, 16, 16]), op=ALU.add),
                 reads=[B_T16], writes=[B_cand])
            for g in range(NG):
                S.op("dve", lambda e, g=g: e.max(out=S24[:, g, 0:8], in_=cand[:, g, :]), reads=[B_cand], parts=[B_S24])
            for g in range(NG):
                S.op("dve", lambda e, g=g: e.match_replace(out=cand2v[:, g, :], in_to_replace=S24[:, g, 0:8], in_values=cand[:, g, :], imm_value=-1e30),
                     reads=[B_cand, B_S24, B_sc2], parts=[B_sc2])
            for g in range(NG):
                S.op("dve", lambda e, g=g: e.max(out=S24[:, g, 8:16], in_=cand2v[:, g, :]), reads=[B_sc2], parts=[B_S24])
            for g in range(NG):
                S.op("dve", lambda e, g=g: e.match_replace(out=cand2v[:, g, :], in_to_replace=S24[:, g, 8:16], in_values=cand2v[:, g, :], imm_value=-1e30),
                     reads=[B_sc2, B_S24], parts=[B_sc2])
            for g in range(NG):
                S.op("dve", lambda e, g=g: e.max(out=S24[:, g, 16:24], in_=cand2v[:, g, :]), reads=[B_sc2], parts=[B_S24])
            (negM, BnegM), (tau, Btau), (nh, Bnh), (Z, BZ), (e1, Be1) = [sv_[n] for n in ["negM", "tau", "nh", "Z", "e1"]]
            S.op("dve", lambda e: e.tensor_scalar(out=negM[:], in0=S24[:, :, 0], scalar1=-1.0, scalar2=None, op0=ALU.mult),
                 reads=[B_S24], writes=[BnegM])
            S.op("dve", lambda e: e.tensor_tensor(out=tau[:], in0=S24[:, :, 15], in1=S24[:, :, 16], op=ALU.add), reads=[B_S24], writes=[Btau])
            S.op("dve", lambda e: e.tensor_scalar(out=tau[:], in0=tau[:], scalar1=0.5, scalar2=None, op0=ALU.mult), reads=[Btau], writes=[Btau])
            S.op("dve", lambda e: e.tensor_scalar(out=nh[:], in0=tau[:], scalar1=-0.5, scalar2=None, op0=ALU.mult), reads=[Btau], writes=[Bnh])
            S.op("dve", lambda e: e.tensor_tensor(out=sm[:], in0=S24[:, :, 0:16], in1=negM[:].unsqueeze(2).to_broadcast([128, NG, 16]), op=ALU.add),
                 reads=[B_S24, BnegM], writes=[B_sm])
            S.op("act", lambda e: e.activation(out=sm[:], in_=sm[:], func=AF.Exp), reads=[B_sm], writes=[B_sm])
            S.op("dve", lambda e: e.reduce_sum(out=Z[:], in_=sm[:], axis=AX.X), reads=[B_sm], writes=[BZ])
            S.op("dve", lambda e: e.tensor_tensor(out=e1[:], in0=tau[:], in1=negM[:], op=ALU.add), reads=[Btau, BnegM], writes=[Be1])
            S.op("act", lambda e: e.activation(out=e1[:], in_=e1[:], func=AF.Exp), reads=[Be1], writes=[Be1])
            S.op("dve", lambda e: e.reciprocal(out=Z[:], in_=Z[:]), reads=[BZ], writes=[BZ])
            S.op("dve", lambda e, kap=kap: e.tensor_tensor(out=kap[:], in0=e1[:], in1=Z[:], op=ALU.mult), reads=[Be1, BZ], writes=[Bkap])
            scv = sc[:].rearrange("p (g two) k -> p g (two k)", two=2)
            S.op("dve", lambda e, scv=scv: e.tensor_tensor(out=scv, in0=scv, in1=nh[:].unsqueeze(2).to_broadcast([128, NG, 256]), op=ALU.add),
                 reads=[B_sc, Bnh], writes=[B_sc])
            S.op("act", lambda e, sc=sc: e.activation(out=sc[:], in_=sc[:], func=AF.Exp), reads=[B_sc], writes=[B_sc])
            S.op("dve", lambda e, scv=scv, kap=kap: e.tensor_tensor(out=scv[:, :, 0:128], in0=scv[:, :, 0:128],
                                                                  in1=kap[:].unsqueeze(2).to_broadcast([128, NG, 128]), op=ALU.mult),
                 reads=[B_sc, Bkap], writes=[B_sc])
            S.dma("sp", scx_d[tt], scv, B_scx, reads=[B_sc], parts=[B_scx])
            S.dma("sp", kap_d[tt], kap[:], B_kapd, reads=[Bkap], parts=[B_kapd])
        for _ in prep:
            pass
        S.barrier()
        release(mA)
        NT = 2
        NBLK = NTT // NT
        TB = NT * 128
        NGB = NT * 8
        IC = 4
        scx, B_scxs = sb("scx", [128, NGB, 256], F32)
        kapB, B_kapB = sb("kapB", [128, NGB], F32)
        Ft = [[sb("F%d_%d" % (bf, g), [128, IC * 128], BF16) for g in range(NGB)] for bf in range(2)]
        u_pool = [sb("up%d" % i, [128, IC * 128], F32) for i in range(3)]
        u_act = [sb("ua%d" % i, [128, IC * 128], F32) for i in range(3)]
        utc = [sb("utc%d" % i, [128, IC, 8, 128], BF16) for i in range(2)]
        vbc = [sb("vbc%d" % i, [128, IC, D], BF16) for i in range(2)]
        gl_t = [sb("gl%d" % i, [128, TB], F32) for i in range(2)]
        ga_t = [sb("ga%d" % i, [128, TB], BF16) for i in range(2)]
        xo, Bxo = sb("xo", [128, 512], F32)
        accO = [banks[0], banks[1], banks[2], banks[3]]
        cu = {"p": 0, "a": 0}
        NCH = 128 // IC
        for blk in range(NBLK):
            for j in range(NT):
                S.dma("sp", scx[:, j * 8:(j + 1) * 8, :], scx_d[blk * NT + j], B_scxs, reads=[B_scx],
                      **({"writes": [B_scxs]} if j == 0 else {"parts": [B_scxs]}))
                S.dma("sp", kapB[:, j * 8:(j + 1) * 8], kap_d[blk * NT + j], B_kapB, reads=[B_kapd],
                      **({"writes": [B_kapB]} if j == 0 else {"parts": [B_kapB]}))
            hcols = slice(blk * TB, (blk + 1) * TB)

            def load_chunk(ic):
                bf = ic % 2
                (uc, Buc) = utc[bf]
                S.dma("sp", uc[:], ut_d[:, ic * IC:(ic + 1) * IC, :, :], Buc, reads=[B_ut], writes=[Buc])

            def load_chunk_v(ic):
                bf = ic % 2
                (vc, Bvc) = vbc[bf]
                S.dma("sp", vc[:], vb_d[:, ic * IC:(ic + 1) * IC, :], Bvc, reads=[B_vb], writes=[Bvc])

            def build_quarter(ic, q):
                bf = ic % 2
                for g in range(q * 4, q * 4 + 4):
                    Fg, BFg = Ft[bf][g]
                    if g % 2 == 0:
                        u_, Bu_ = u_pool[cu["p"] % 3]
                        cu["p"] += 1
                        S.op("pool", lambda e, u_=u_, g=g, ic=ic: e.tensor_tensor(
                            out=u_[:].rearrange("p (i j) -> p i j", i=IC),
                            in0=scx[:, g, ic * IC:(ic + 1) * IC].unsqueeze(2).to_broadcast([128, IC, 128]),
                            in1=scx[:, g, 128:256].unsqueeze(1).to_broadcast([128, IC, 128]), op=ALU.mult),
                            reads=[B_scxs], writes=[Bu_])
                    else:
                        u_, Bu_ = u_act[cu["a"] % 3]
                        cu["a"] += 1
                        for i in range(IC):
                            S.op("act", lambda e, u_=u_, g=g, ic=ic, i=i: e.activation(
                                out=u_[:, i * 128:(i + 1) * 128], in_=scx[:, g, 128:256], func=AF.Copy,
                                scale=scx[:, g, ic * IC + i:ic * IC + i + 1]),
                                reads=[B_scxs], **({"writes": [Bu_]} if i == 0 else {"parts": [Bu_]}))
                    S.op("dve", lambda e, u_=u_, Fg=Fg, g=g: e.scalar_tensor_tensor(
                        out=Fg[:], in0=u_[:], scalar=kapB[:, g:g + 1], in1=u_[:], op0=ALU.is_ge, op1=ALU.mult),
                        reads=[Bu_, B_kapB], writes=[BFg])

            def emit_out(et):
                ic, il = et // IC, et % IC
                vc, Bvc = vbc[ic % 2]
                ga, Bga = ga_t[et % 2]
                for j in range(NT):
                    for half in range(2):
                        ab, Bab = accO[j * 2 + half]
                        S.mm_group([lambda pe, j=j, half=half, ab=ab, ga=ga, vc=vc, il=il, et=et: pe.matmul(
                            ab[:], lhsT=ga[:, j * 128:(j + 1) * 128], rhs=vc[:, il, half * 512:(half + 1) * 512],
                            start=(et == 0), stop=(et == 127), skip_group_check=True)],
                            reads=[Bga, Bvc], **({"writes": [Bab]} if et == 0 else {"parts": [Bab]}))

            load_chunk(0)
            load_chunk_v(0)
            for q in range(4):
                build_quarter(0, q)
            for et in range(128):
                ic, il = et // IC, et % IC
                bf = ic % 2
                if il == 0 and ic + 1 < NCH:
                    load_chunk(ic + 1)
                if ic + 1 < NCH:
                    build_quarter(ic + 1, il)
                uc, Buc = utc[bf]
                hbk, Bhbk = banks[4 + et % 2]
                gbk, Bgbk = banks[6 + et % 2]
                S.mm_group([lambda pe, kc=kc, il=il, hbk=hbk, uc=uc: pe.matmul(hbk[:, :TB], lhsT=uc[:, il, kc, :], rhs=h2T[:, kc, hcols],
                                                                             start=(kc == 0), stop=(kc == 7)) for kc in range(8)],
                           reads=[Buc, B_h2T], writes=[Bhbk])
                S.mm_group([lambda pe, j=j, h=h, il=il, gbk=gbk, bf=bf: pe.matmul(
                    gbk[:, j * 128:(j + 1) * 128], lhsT=Ft[bf][j * 8 + h][0][:, il * 128:(il + 1) * 128], rhs=idb[:],
                    start=(j == 0 and h == 0), stop=(h == 7), skip_group_check=True) for j in range(NT) for h in range(8)],
                    reads=[Ft[bf][g][1] for g in range(NGB)] + [B_idb], writes=[Bgbk])
                (gl, Bgl), (ga, Bga) = gl_t[et % 2], ga_t[et % 2]
                S.op("act", lambda e, gl=gl, hbk=hbk: e.activation(out=gl[:], in_=hbk[:, :TB], func=AF.Gelu_apprx_tanh),
                     reads=[Bhbk], writes=[Bgl])
                S.op("dve", lambda e, gl=gl, ga=ga, gbk=gbk: e.tensor_tensor(out=ga[:], in0=gl[:], in1=gbk[:, :TB], op=ALU.mult),
                     reads=[Bgl, Bgbk], writes=[Bga])
                if et > 0:
                    emit_out(et - 1)
                if il == 0 and ic + 1 < NCH:
                    load_chunk_v(ic + 1)
            emit_out(127)
            for j in range(NT):
                tt = blk * NT + j
                for half in range(2):
                    ab, Bab = accO[j * 2 + half]
                    S.op("dve", lambda e, ab=ab, half=half: e.tensor_tensor(out=xo[:], in0=ab[:], in1=G2[:, half * 512:(half + 1) * 512], op=ALU.mult),
                         reads=[Bab, BG2], writes=[Bxo])
                    S.op("dve", lambda e, half=half, tt=tt: e.tensor_tensor(
                        out=x_t[:, tt, half * 512:(half + 1) * 512], in0=x_t[:, tt, half * 512:(half + 1) * 512], in1=xo[:], op=ALU.add),
                        reads=[Bxo, B_xt[tt]], parts=[B_xt[tt]])
        S.barrier()
        release(m)

    def final_phase():
        m = mark()
        fg, Bfg = load_bc("fg_bc", fin_g.rearrange("(o n) -> o n", o=1), D)
        tmp = norm_tmp()
        (sq, Bsq), (ss, Bss), (rs, Brs), (hn, Bhn), (hb, Bhb) = tmp
        for tt in range(NTT):
            norm_tile(tt, fg, Bfg, None, None, None, None, 0, tmp, None)
            S.dma("sp", y_d[tt * 128:(tt + 1) * 128, :], hn[:], B_y, reads=[Bhn], parts=[B_y])
        S.barrier()
        release(m)

    def dump_x():
        for tt in range(NTT):
            S.dma("sp", y_d[tt * 128:(tt + 1) * 128, :], x_t[:, tt, :], B_y, reads=[B_xt[tt]], parts=[B_y])
        S.barrier()

    for l in range(l0, l1):
        mod_phase(l)
        if stop_after == "mod":
            S.dma("sp", y_d[0:128, :].rearrange("p (a n) -> p a n", a=1)[:, 0, :], x_t[:, 0, :], B_y, reads=[B_xt[0]], parts=[B_y])
            break
        mm, hT = mixer_phase(l)
        if stop_after == "h1":
            hf, Bhf = sb("hf", [128, 8, 128], F32)
            break
        release(mm)
        if stop_after == "mix":
            break
        peer_phase(l)
    if stop_after is None and last:
        final_phase()
    else:
        dump_x()
    S.finish()
    return nc


def t5_bucket_np(d):
    d = np.maximum(d, 0)
    ratio = np.log(np.maximum(d, 1).astype(np.float32) / np.float32(16)) / np.float32(math.log(2048 / 16))
    large = np.minimum(16 + (ratio * np.float32(16)).astype(np.int32), 31)
    return np.where(d < 16, d, large)


def make_btab(rel_bias):
    ext = np.concatenate([rel_bias.astype(np.float32), np.full((1, 32), -30000.0, np.float32)], axis=0)
    out = np.empty((128, TABCOLS), np.float32)
    k = np.arange(128)[:, None, None]
    q = np.arange(128)[None, None, :]
    for typ in range(4):
        nb = TAB_NB[typ]
        Dd = (np.arange(nb) - 3)[None, :, None]
        delta = 128 * Dd + q - k
        valid = delta >= 0
        if TAB_W[typ] is not None:
            r = TAB_R[typ]
            valid = valid & (delta % r == 0) & (delta // r <= 128)
        idx = np.where(valid, t5_bucket_np(delta), 32)
        for h in range(8):
            hg = typ * 8 + h
            o = tab_off(typ, h)
            out[:, o:o + nb * 128] = ext[idx, hg].reshape(128, nb * 128)
    return out


_CACHE = {}


def run_layers(inputs, xin, l0, l1, first, last, stop_after=None, cores=8, with_peer=True):
    nc = build(l0, l1, first, last, stop_after, with_peer)
    btab = make_btab(np.asarray(inputs["rel_bias"]))
    ident = np.eye(128, dtype=np.float32)
    shared = {k: np.ascontiguousarray(np.asarray(inputs[k], dtype=np.float32)) for k in
              ["w_ada", "b_ada", "norm1_g", "norm2_g", "w_in", "w_proj_a", "w_proj_b", "w_out", "lam_q1", "lam_k1",
               "lam_q2", "lam_k2", "subln_g", "peer_wq", "peer_subkeys", "peer_u", "peer_v", "final_g"]}
    if not with_peer:
        del shared["peer_u"], shared["peer_v"]
    shared["btab"] = btab
    shared["ident"] = ident
    c = np.asarray(inputs["c"], dtype=np.float32)
    in_maps = []
    for b in range(cores):
        mp = dict(shared)
        mp["x"] = np.ascontiguousarray(xin[b])
        mp["c"] = np.ascontiguousarray(c[b].reshape(8, 128).T)
        in_maps.append(mp)
    res = run_bass_kernel_spmd(nc, in_maps, core_ids=list(range(cores)))
    return np.stack([np.asarray(r["y"]) for r in res.results], axis=0)


def kernel(**inputs):
    x = np.asarray(inputs["x"], dtype=np.float32)
    out = run_layers(inputs, x, 0, DEPTH, True, True)
    return out.astype(np.float32)
```

```python
import math
import numpy as np
import concourse.bass as bass
import concourse.mybir as mybir
from concourse.bass_utils import run_bass_kernel_spmd

F32 = mybir.dt.float32
BF16 = mybir.dt.bfloat16
AF = mybir.ActivationFunctionType
ALU = mybir.AluOpType
AX = mybir.AxisListType

D = 1024
T = 2048
NTT = 16
DEPTH = 4
INW = 9728
NEXP = 16384
TAB_NB = [8, 11, 22, 22]
TAB_W = [128, 512, 2048, None]
TAB_R = [1, 4, 16, 1]
TAB_DMAX = [1, 4, 15, 15]
TABCOLS = sum(TAB_NB) * 128 * 8


def tab_off(typ, head):
    off = 0
    for t in range(typ):
        off += TAB_NB[t] * 128 * 8
    return off + head * TAB_NB[typ] * 128


class Buf:
    def __init__(self, name):
        self.name = name
        self.writers = {}
        self.readers = {}
        self.sem = None
        self.dma_total = 0


class Eng:
    def __init__(self, name, obj, sem):
        self.name = name
        self.obj = obj
        self.sem = sem
        self.cnt = 0
        self.waited = {}


class Sched:
    def __init__(self, nc):
        self.nc = nc
        self.sems = {}
        self.engs = {}
        for name, obj in [("pe", nc.tensor), ("act", nc.scalar), ("dve", nc.vector), ("pool", nc.gpsimd), ("sp", nc.sync)]:
            s = self.new_sem("e_" + name)
            self.engs[name] = Eng(name, obj, s)
        self.dma_bufs = []
        self.free_sems = []

    def recycle(self, b):
        if b.sem is not None:
            self.free_sems.append((b.sem, b.dma_total))
            self.dma_bufs.remove(b)
            b.sem = None

    def new_sem(self, name):
        s = self.nc.semaphore(name).__enter__()
        self.sems[id(s)] = s
        return s

    def _wait(self, eng, need):
        for sid, val in need.items():
            if eng.waited.get(sid, 0) < val:
                eng.obj.wait_ge(self.sems[sid], val)
                eng.waited[sid] = val

    def _need(self, own_sid, reads, writes, parts):
        need = {}

        def merge(d):
            for k, v in d.items():
                if need.get(k, 0) < v:
                    need[k] = v
        for b in reads:
            merge(b.writers)
        for b in writes:
            merge(b.writers)
            merge(b.readers)
        for b in parts:
            merge(b.readers)
            merge({k: v for k, v in b.writers.items() if k != own_sid})
        return need

    def _record(self, sid, val, reads, writes, parts):
        for b in reads:
            if b.readers.get(sid, 0) < val:
                b.readers[sid] = val
        for b in writes:
            b.writers = {sid: val}
            b.readers = {}
        for b in parts:
            b.writers[sid] = val

    def op(self, engname, fn, reads=(), writes=(), parts=()):
        eng = self.engs[engname]
        sid = id(eng.sem)
        self._wait(eng, self._need(sid, reads, writes, parts))
        ins = fn(eng.obj)
        ins.then_inc(eng.sem, 1)
        eng.cnt += 1
        self._record(sid, eng.cnt, reads, writes, parts)

    def mm_group(self, mms, reads, writes=(), parts=()):
        eng = self.engs["pe"]
        sid = id(eng.sem)
        self._wait(eng, self._need(sid, reads, writes, parts))
        ins = None
        for f in mms:
            ins = f(eng.obj)
        ins.then_inc(eng.sem, 1)
        eng.cnt += 1
        self._record(sid, eng.cnt, reads, writes, parts)

    def dma(self, qname, out_ap, in_ap, owner, reads=(), writes=(), parts=()):
        eng = self.engs[qname]
        if owner.sem is None:
            if self.free_sems:
                owner.sem, owner.dma_total = self.free_sems.pop()
            else:
                owner.sem = self.new_sem("d_" + owner.name)
            self.dma_bufs.append(owner)
        sid = id(owner.sem)
        self._wait(eng, self._need(sid, reads, writes, parts))
        eng.obj.dma_start(out=out_ap, in_=in_ap).then_inc(owner.sem, 16)
        owner.dma_total += 16
        self._record(sid, owner.dma_total, reads, writes, parts)

    def barrier(self):
        allv = {}
        for e in self.engs.values():
            if e.cnt:
                allv[id(e.sem)] = e.cnt
        for b in self.dma_bufs:
            allv[id(b.sem)] = b.dma_total
        for e in self.engs.values():
            self._wait(e, allv)

    def finish(self):
        self.barrier()


def build(l0, l1, first, last, stop_after=None, with_peer=True):
    nc = bass.Bass("TRN2", target_bir_lowering=False)
    S = Sched(nc)

    def din(name, shape, dt=F32):
        return nc.dram_tensor(name, list(shape), dt, kind="ExternalInput").ap()

    x_d = din("x", [T, D])
    c_d = din("c", [128, 8])
    w_ada = din("w_ada", [DEPTH, D, 6 * D])
    b_ada = din("b_ada", [DEPTH, 6 * D])
    n1g = din("norm1_g", [DEPTH, D])
    n2g = din("norm2_g", [DEPTH, D])
    w_in = din("w_in", [DEPTH, D, INW])
    w_pa = din("w_proj_a", [DEPTH, 512, D])
    w_pb = din("w_proj_b", [DEPTH, D, D])
    w_o = din("w_out", [DEPTH, D, D])
    lq1 = din("lam_q1", [DEPTH, 64])
    lk1 = din("lam_k1", [DEPTH, 64])
    lq2 = din("lam_q2", [DEPTH, 64])
    lk2 = din("lam_k2", [DEPTH, 64])
    subg = din("subln_g", [DEPTH, 128])
    btab = din("btab", [128, TABCOLS])
    p_wq = din("peer_wq", [DEPTH, D, 2048])
    p_sk = din("peer_subkeys", [DEPTH, 2, 128, 128])
    p_u = din("peer_u", [DEPTH, NEXP, D]) if with_peer else None
    p_v = din("peer_v", [DEPTH, NEXP, D]) if with_peer else None
    fin_g = din("final_g", [D])
    ident_d = din("ident", [128, 128])
    y_d = nc.dram_tensor("y", [T, D], F32, kind="ExternalOutput").ap()

    etab_d = nc.dram_tensor("etab", [128, TABCOLS], BF16, kind="Internal").ap()
    mod_d = nc.dram_tensor("modscr", [128, 6 * D], F32, kind="Internal").ap()
    ut_d = nc.dram_tensor("utscr", [128, 128, 8, 128], BF16, kind="Internal").ap()
    vb_d = nc.dram_tensor("vbscr", [128, 128, D], BF16, kind="Internal").ap()
    scx_d = nc.dram_tensor("scxscr", [NTT, 128, 8, 256], F32, kind="Internal").ap()
    kap_d = nc.dram_tensor("kapscr", [NTT, 128, 8], F32, kind="Internal").ap()
    B_etab, B_mod, B_ut, B_vb, B_y = Buf("etab"), Buf("modscr"), Buf("utscr"), Buf("vbscr"), Buf("y")
    B_scx, B_kapd = Buf("scxscr"), Buf("kapscr")

    _ctx = []

    _uid = [0]

    def sb(name, shape, dt):
        _uid[0] += 1
        name = "s%d_%s" % (_uid[0], name)
        cm = nc.sbuf_tensor(name, list(shape), dt)
        t = cm.__enter__()
        b = Buf(name)
        _ctx.append((cm, b))
        return t, b

    def mark():
        return len(_ctx)

    def release(m):
        while len(_ctx) > m:
            cm, b = _ctx.pop()
            S.recycle(b)
            cm.__exit__(None, None, None)

    banks = []
    for i in range(8):
        t = nc.psum_tensor("bank%d" % i, [128, 512], F32).__enter__()
        banks.append((t, Buf("bank%d" % i)))

    x_t, B_x = sb("x", [128, NTT, D], F32)
    B_xt = [Buf("x%d" % i) for i in range(NTT)]
    idb, B_idb = sb("idb", [128, 128], BF16)
    idf, B_idf = sb("idf", [128, 128], F32)
    condrep, B_condrep = sb("condrep", [128, 8, 128], F32)
    lam_t, B_lam = sb("lam", [128, 4], F32)

    S.dma("pool", idb[:], ident_d[:, :], B_idb, writes=[B_idb])
    S.dma("sp", idf[:], ident_d[:, :], B_idf, writes=[B_idf])
    if first:
        for tt in range(NTT):
            S.dma("sp", x_t[:, tt, :], x_d[tt * 128:(tt + 1) * 128, :], B_xt[tt], writes=[B_xt[tt]])
    else:
        for tt in range(NTT):
            S.dma("sp", x_t[:, tt, :], x_d[tt * 128:(tt + 1) * 128, :], B_xt[tt], writes=[B_xt[tt]])

    m0 = mark()
    c_t, B_c = sb("c_t", [128, 8], F32)
    cs_t, B_cs = sb("cs_t", [128, 8], F32)
    S.dma("sp", c_t[:], c_d[:, :], B_c, writes=[B_c])
    S.op("act", lambda e: e.activation(out=cs_t[:], in_=c_t[:], func=AF.Silu), reads=[B_c], writes=[B_cs])
    S.op("dve", lambda e: e.tensor_copy(out=condrep[:], in_=cs_t[:].unsqueeze(2).to_broadcast([128, 8, 128])),
         reads=[B_cs], writes=[B_condrep])
    CH = 2048
    tb_f = [sb("tbf%d" % i, [128, CH], F32) for i in range(2)]
    tb_b = [sb("tbb%d" % i, [128, CH], BF16) for i in range(2)]
    nch = (TABCOLS + CH - 1) // CH
    for ci in range(nch):
        c0 = ci * CH
        cw = min(CH, TABCOLS - c0)
        tf, Bf = tb_f[ci % 2]
        tbb, Bb = tb_b[ci % 2]
        S.dma("sp", tf[:, :cw], btab[:, c0:c0 + cw], Bf, writes=[Bf])
        S.op("act", lambda e, tf=tf, tbb=tbb, cw=cw: e.activation(out=tbb[:, :cw], in_=tf[:, :cw], func=AF.Exp),
             reads=[Bf], writes=[Bb])
        S.dma("sp", etab_d[:, c0:c0 + cw], tbb[:, :cw], B_etab, reads=[Bb], parts=[B_etab])
    S.barrier()
    release(m0)

    def mod_phase(l):
        m = mark()
        wt = [sb("modw%d" % i, [128, 512], F32) for i in range(3)]
        bt = [sb("modb%d" % i, [128, 512], F32) for i in range(2)]
        mo = [sb("modo%d" % i, [128, 512], F32) for i in range(2)]
        k = 0
        for nchk in range(12):
            ps, Bps = banks[nchk % 2]
            btile, Bbt = bt[nchk % 2]
            S.dma("sp", btile[:], b_ada[l:l + 1, nchk * 512:(nchk + 1) * 512].partition_broadcast(128), Bbt, writes=[Bbt])
            for kc in range(8):
                w, Bw = wt[k % 3]
                k += 1
                S.dma("sp", w[:], w_ada[l, kc * 128:(kc + 1) * 128, nchk * 512:(nchk + 1) * 512], Bw, writes=[Bw])
                S.mm_group([lambda pe, kc=kc, w=w, ps=ps: pe.matmul(ps[:], lhsT=condrep[:, kc, :], rhs=w[:],
                                                                    start=(kc == 0), stop=(kc == 7))],
                           reads=[Bw, B_condrep], **({"writes": [Bps]} if kc == 0 else {"parts": [Bps]}))
            o, Bo = mo[nchk % 2]
            S.op("dve", lambda e, o=o, ps=ps, btile=btile: e.tensor_tensor(out=o[:], in0=ps[:], in1=btile[:], op=ALU.add),
                 reads=[Bps, Bbt], writes=[Bo])
            S.dma("sp", mod_d[:, nchk * 512:(nchk + 1) * 512], o[:], B_mod, reads=[Bo], parts=[B_mod])
        lt = [sb("lamv%d" % i, [128, 64], F32) for i in range(4)]
        for i, src in enumerate([lq1, lk1, lq2, lk2]):
            S.dma("sp", lt[i][0][:], src[l:l + 1, :].partition_broadcast(128), lt[i][1], writes=[lt[i][1]])
        pr, Bpr = sb("lampr", [128, 2, 64], F32)
        dd, Bdd = sb("lamdd", [128, 2], F32)
        ee, Bee = sb("lamee", [128, 2], F32)
        S.op("dve", lambda e: e.tensor_tensor(out=pr[:, 0, :], in0=lt[0][0][:], in1=lt[1][0][:], op=ALU.mult),
             reads=[lt[0][1], lt[1][1]], parts=[Bpr])
        S.op("dve", lambda e: e.tensor_tensor(out=pr[:, 1, :], in0=lt[2][0][:], in1=lt[3][0][:], op=ALU.mult),
             reads=[lt[2][1], lt[3][1]], parts=[Bpr])
        S.op("dve", lambda e: e.reduce_sum(out=dd[:], in_=pr[:], axis=AX.X), reads=[Bpr], writes=[Bdd])
        S.op("act", lambda e: e.activation(out=ee[:], in_=dd[:], func=AF.Exp), reads=[Bdd], writes=[Bee])
        lam_init = 0.8 - 0.6 * math.exp(-0.3 * l)
        S.op("dve", lambda e: e.scalar_tensor_tensor(out=lam_t[:, 0:1], in0=ee[:, 1:2], scalar=-lam_init, in1=ee[:, 0:1],
                                                     op0=ALU.add, op1=ALU.subtract),
             reads=[Bee], writes=[B_lam])
        S.barrier()
        release(m)

    def norm_tile(tt, A, BA, Bb, BBb, hT, B_hT, col0, tmp, ps_bank):
        (sq, Bsq), (ss, Bss), (rs, Brs), (hn, Bhn), (hb, Bhb) = tmp
        S.op("act", lambda e: e.activation(out=sq[:], in_=x_t[:, tt, :], func=AF.Square, accum_out=ss[:]),
             reads=[B_xt[tt]], writes=[Bsq, Bss])
        S.op("act", lambda e: e.activation(out=rs[:], in_=ss[:], func=AF.Sqrt, scale=1.0 / D, bias=EPS_T[:, 0:1]),
             reads=[Bss, B_eps], writes=[Brs])
        S.op("dve", lambda e: e.reciprocal(out=rs[:], in_=rs[:]), reads=[Brs], writes=[Brs])
        S.op("dve", lambda e: e.scalar_tensor_tensor(out=hn[:], in0=x_t[:, tt, :], scalar=rs[:, 0:1], in1=A[:],
                                                     op0=ALU.mult, op1=ALU.mult),
             reads=[B_xt[tt], Brs, BA], writes=[Bhn])
        if Bb is not None:
            S.op("pool", lambda e: e.tensor_tensor(out=hb[:], in0=hn[:], in1=Bb[:], op=ALU.add),
                 reads=[Bhn, BBb], writes=[Bhb])
        if hT is None:
            return
        ps, Bps = ps_bank
        psb = ps[:].bitcast(BF16)
        S.mm_group([lambda pe, dc=dc: pe.transpose(out=psb[:, dc * 128:(dc + 1) * 128], in_=hb[:, dc * 128:(dc + 1) * 128],
                                                   identity=idb[:]) for dc in range(8)],
                   reads=[Bhb, B_idb], writes=[Bps])
        S.op("act", lambda e: e.activation(out=hT[:, :, col0:col0 + 128],
                                           in_=psb.rearrange("p (a b) -> p a b", a=8), func=AF.Copy),
             reads=[Bps], parts=[B_hT])

    EPS_T, B_eps = sb("eps", [128, 2], F32)
    S.op("dve", lambda e: e.memset(EPS_T[:, 0:1], 1e-6), parts=[B_eps])
    S.op("dve", lambda e: e.memset(EPS_T[:, 1:2], 1e-5), parts=[B_eps])

    def load_bc(name, src_row_ap, n, q="sp"):
        t, Bt = sb(name, [128, n], F32)
        S.dma(q, t[:], src_row_ap.partition_broadcast(128), Bt, writes=[Bt])
        return t, Bt

    def norm_tmp():
        return [sb("n_sq", [128, D], BF16), sb("n_ss", [128, 1], F32), sb("n_rs", [128, 1], F32),
                sb("n_hn", [128, D], F32), sb("n_hb", [128, D], BF16)]

    def make_AB(l, which):
        base = 0 if which == 1 else 3 * D
        A, BA = sb("A_bc", [128, D], F32)
        Bt, BB = sb("B_bc", [128, D], F32)
        g, Bg = load_bc("ng_bc", (n1g if which == 1 else n2g)[l:l + 1, :], D)
        S.dma("sp", A[:], mod_d[:, base + D:base + 2 * D], BA, reads=[B_mod], writes=[BA])
        S.dma("sp", Bt[:], mod_d[:, base:base + D], BB, reads=[B_mod], writes=[BB])
        S.op("dve", lambda e: e.scalar_tensor_tensor(out=A[:], in0=A[:], scalar=1.0, in1=g[:], op0=ALU.add, op1=ALU.mult),
             reads=[BA, Bg], writes=[BA])
        return A, BA, Bt, BB

    def mixer_phase(l):
        m = mark()
        hT, B_hT = sb("hT", [128, 8, T], BF16)
        oaT, B_oaT = sb("oaT", [128, 4, T], BF16)
        obT, B_obT = sb("obT", [128, 8, T], BF16)
        m1 = mark()
        A, BA, Bt, BB = make_AB(l, 1)
        tmp = norm_tmp()
        for tt in range(NTT):
            norm_tile(tt, A, BA, Bt, BB, hT, B_hT, tt * 128, tmp, banks[7])
        S.barrier()
        release(m1)
        if stop_after == "h1":
            return m, hT
        m2 = mark()
        wq = [sb("wq%d" % i, [128, 8, 64], BF16) for i in range(2)]
        wk = [sb("wk%d" % i, [128, 8, 64], BF16) for i in range(2)]
        wv = [sb("wv%d" % i, [128, 8, 128], BF16) for i in range(2)]
        qT = [sb("qT%d" % i, [128, T], BF16) for i in range(2)]
        kT = [sb("kT%d" % i, [128, T], BF16) for i in range(2)]
        vx = [sb("vx0", [128, NTT, 129], BF16)] * 2
        tab = [sb("tab0", [128, 22 * 128], BF16)] * 2
        pe_t = [sb("pexp%d" % i, [128, 512], BF16) for i in range(2)]
        pm_t = [sb("pm%d" % i, [128, 512], BF16) for i in range(2)]
        acc_sb, B_acc = sb("accsb", [128, NTT, 65], F32)
        otmp, B_otmp = sb("otmp", [128, NTT, 128], F32)
        rr = [sb("rr%d" % i, [128, 4], F32) for i in range(2)]
        osb = [sb("osb%d" % i, [128, 128], BF16) for i in range(2)]
        oa2, B_oa2 = sb("oa2", [128, NTT, 128], BF16)
        sg, Bsg = load_bc("subg", subg[l:l + 1, :], 128)
        lam_init = 0.8 - 0.6 * math.exp(-0.3 * l)
        S.op("dve", lambda e: e.tensor_scalar(out=sg[:], in0=sg[:], scalar1=1.0 - lam_init, scalar2=None, op0=ALU.mult),
             reads=[Bsg], writes=[Bsg])
        S.op("pool", lambda e: e.memset(vx[0][0][:], 1.0), writes=[vx[0][1]])
        ctr = {"st": 0, "sc": 0, "rr": 0, "os": 0}

        def stream1(si, qcol, kcol, vcol, dh, dv, typ, thead, fin):
            (wq_t, Bwq), (wk_t, Bwk), (wv_t, Bwv) = wq[si], wk[si], wv[si]
            (q_t, Bq), (k_t, Bk) = qT[si], kT[si]
            wsrc = w_in[l].rearrange("(a p) n -> p a n", p=128)
            S.dma("pool", wq_t[:, :, :dh], wsrc[:, :, qcol:qcol + dh], Bwq, writes=[Bwq])
            S.dma("pool", wk_t[:, :, :dh], wsrc[:, :, kcol:kcol + dh], Bwk, writes=[Bwk])
            S.dma("pool", wv_t[:, :, :dv], wsrc[:, :, vcol:vcol + dv], Bwv, writes=[Bwv])
            for (w_t, Bw, o_t, Bo) in ((wq_t, Bwq, q_t, Bq), (wk_t, Bwk, k_t, Bk)):
                for qc in range(4):
                    ps, Bps = banks[4 + (qc % 2)]
                    S.mm_group([lambda pe, kc=kc, w_t=w_t, ps=ps, qc=qc: pe.matmul(
                        ps[:dh, :], lhsT=w_t[:, kc, :dh], rhs=hT[:, kc, qc * 512:(qc + 1) * 512],
                        start=(kc == 0), stop=(kc == 7)) for kc in range(8)],
                        reads=[Bw, B_hT], writes=[Bps])
                    S.op("act", lambda e, o_t=o_t, ps=ps, qc=qc: e.activation(
                        out=o_t[:dh, qc * 512:(qc + 1) * 512], in_=ps[:dh, :], func=AF.Copy),
                        reads=[Bps], parts=[Bo])

        def stream2(si, qcol, kcol, vcol, dh, dv, typ, thead, fin):
            (wq_t, Bwq), (wk_t, Bwk), (wv_t, Bwv) = wq[si], wk[si], wv[si]
            (q_t, Bq), (k_t, Bk), (v_t, Bv), (tb_t, Btb) = qT[si], kT[si], vx[si], tab[si]
            nb = TAB_NB[typ]
            to = tab_off(typ, thead)
            S.dma("sp", tb_t[:, :nb * 128], etab_d[:, to:to + nb * 128], Btb, reads=[B_etab], writes=[Btb])
            for tt in range(NTT):
                ps, Bps = banks[6 + (tt % 2)]
                S.mm_group([lambda pe, kc=kc, ps=ps, tt=tt: pe.matmul(
                    ps[:, :dv], lhsT=hT[:, kc, tt * 128:(tt + 1) * 128], rhs=wv_t[:, kc, :dv],
                    start=(kc == 0), stop=(kc == 7)) for kc in range(8)],
                    reads=[Bwv, B_hT], writes=[Bps])
                S.op("dve", lambda e, ps=ps, tt=tt: e.tensor_copy(out=v_t[:, tt, :dv], in_=ps[:, :dv]),
                     reads=[Bps], parts=[Bv])
            if dv < 128:
                S.op("pool", lambda e: e.memset(v_t[:, :, dv:dv + 1], 1.0), parts=[Bv])
            dmax = TAB_DMAX[typ]
            items = []
            for qc in range(4):
                kts = [kt for kt in range(NTT) if any(0 <= 4 * qc + j - kt <= dmax for j in range(4))]
                lastkt = {}
                for kt in kts:
                    for j in range(4):
                        if 0 <= 4 * qc + j - kt <= dmax:
                            lastkt[j] = kt
                for idx, kt in enumerate(kts):
                    items.append((qc, kt, idx == len(kts) - 1, lastkt))
            first_in = {}
            slot = {}

            def front(i):
                qc, kt, _, _ = items[i]
                pi = ctr["sc"] % 2
                ctr["sc"] += 1
                slot[i] = pi
                ps, Bps = banks[pi]
                (pex, Bpex), (pm, Bpm) = pe_t[pi], pm_t[pi]
                S.mm_group([lambda pe: pe.matmul(
                    ps[:, :], lhsT=k_t[:dh, kt * 128:(kt + 1) * 128], rhs=q_t[:dh, qc * 512:(qc + 1) * 512],
                    start=True, stop=True)], reads=[Bq, Bk], writes=[Bps])
                S.op("act", lambda e: e.activation(out=pex[:], in_=ps[:], func=AF.Exp, scale=0.125),
                     reads=[Bps], writes=[Bpex])
                d0 = 4 * qc - kt
                S.op("dve", lambda e: e.tensor_tensor(
                    out=pm[:], in0=pex[:], in1=tb_t[:, (d0 + 3) * 128:(d0 + 3) * 128 + 512], op=ALU.mult),
                    reads=[Bpex, Btb], writes=[Bpm])

            def back(i):
                qc, kt, is_last, lastkt = items[i]
                accb = [banks[2], banks[3]] if qc % 2 == 0 else [banks[4], banks[5]]
                pm, Bpm = pm_t[slot[i]]
                mms = []
                for j in range(4):
                    dd_ = 4 * qc + j - kt
                    if not (0 <= dd_ <= dmax):
                        continue
                    bi = j // 2
                    ab, Bab = accb[bi]
                    st = first_in.get((qc, bi), True)
                    first_in[(qc, bi)] = False
                    c0 = (j % 2) * (dv + 1)
                    mms.append((bi, lambda pe, ab=ab, j=j, st=st, c0=c0: pe.matmul(
                        ab[:, c0:c0 + dv + 1], lhsT=pm[:, j * 128:(j + 1) * 128], rhs=v_t[:, kt, :dv + 1],
                        start=st, stop=(lastkt[j] == kt), skip_group_check=True)))
                for bi in (0, 1):
                    fl = [f for (b_, f) in mms if b_ == bi]
                    if fl:
                        S.mm_group(fl, reads=[Bpm, Bv], parts=[accb[bi][1]])
                if is_last:
                    fin(qc, accb, dv)

            front(0)
            for i in range(len(items)):
                if i + 1 < len(items):
                    front(i + 1)
                back(i)

        def fin_dil(first_s, last_s, pairslot, pair_idx):
            def f(qc, accb, dv):
                for j in range(4):
                    tt = 4 * qc + j
                    ab, Bab = accb[j // 2]
                    c0 = (j % 2) * 65
                    if first_s:
                        S.op("dve", lambda e, ab=ab, tt=tt, c0=c0: e.tensor_copy(out=acc_sb[:, tt, :65], in_=ab[:, c0:c0 + 65]),
                             reads=[Bab], parts=[B_acc])
                    else:
                        S.op("dve", lambda e, ab=ab, tt=tt, c0=c0: e.tensor_tensor(
                            out=acc_sb[:, tt, :65], in0=acc_sb[:, tt, :65], in1=ab[:, c0:c0 + 65], op=ALU.add),
                            reads=[Bab, B_acc], parts=[B_acc])
                    if last_s:
                        r, Br = rr[ctr["rr"] % 2]
                        ctr["rr"] += 1
                        S.op("dve", lambda e, r=r, tt=tt: e.reciprocal(out=r[:, 0:1], in_=acc_sb[:, tt, 64:65]),
                             reads=[B_acc], writes=[Br])
                        S.op("dve", lambda e, r=r, tt=tt: e.tensor_scalar(
                            out=oa2[:, tt, pairslot * 64:(pairslot + 1) * 64], in0=acc_sb[:, tt, :64],
                            scalar1=r[:, 0:1], scalar2=None, op0=ALU.mult),
                            reads=[Br, B_acc], parts=[B_oa2])
                        if pairslot == 1:
                            ps, Bps = banks[6]
                            psb = ps[:].bitcast(BF16)
                            S.mm_group([lambda pe, tt=tt: pe.transpose(out=psb[:, 0:128], in_=oa2[:, tt, :], identity=idb[:])],
                                       reads=[B_oa2, B_idb], writes=[Bps])
                            S.op("act", lambda e, tt=tt: e.activation(out=oaT[:, pair_idx, tt * 128:(tt + 1) * 128],
                                                                      in_=psb[:, 0:128], func=AF.Copy),
                                 reads=[Bps], parts=[B_oaT])
            return f

        def fin_diff(s, head):
            def f(qc, accb, dv):
                for j in range(4):
                    tt = 4 * qc + j
                    ab, Bab = accb[j // 2]
                    c0 = (j % 2) * 129
                    r, Br = rr[ctr["rr"] % 2]
                    ctr["rr"] += 1
                    S.op("dve", lambda e, r=r, ab=ab, c0=c0: e.reciprocal(out=r[:, 0:1], in_=ab[:, c0 + 128:c0 + 129]),
                         reads=[Bab], writes=[Br])
                    if s == 0:
                        S.op("dve", lambda e, r=r, ab=ab, c0=c0, tt=tt: e.tensor_scalar(
                            out=otmp[:, tt, :], in0=ab[:, c0:c0 + 128], scalar1=r[:, 0:1], scalar2=None, op0=ALU.mult),
                            reads=[Br, Bab], parts=[B_otmp])
                    else:
                        S.op("dve", lambda e, r=r: e.tensor_tensor(out=r[:, 1:2], in0=r[:, 0:1], in1=lam_t[:, 0:1], op=ALU.mult),
                             reads=[Br, B_lam], writes=[Br])
                        S.op("dve", lambda e, r=r, ab=ab, c0=c0, tt=tt: e.scalar_tensor_tensor(
                            out=otmp[:, tt, :], in0=ab[:, c0:c0 + 128], scalar=r[:, 1:2], in1=otmp[:, tt, :],
                            op0=ALU.mult, op1=ALU.add),
                            reads=[Br, Bab, B_otmp], parts=[B_otmp])
                        o_b, Bob = osb[ctr["os"] % 2]
                        ctr["os"] += 1
                        S.op("act", lambda e, o_b=o_b, r=r, tt=tt: e.activation(out=o_b[:], in_=otmp[:, tt, :], func=AF.Square,
                                                                                accum_out=r[:, 2:3]),
                             reads=[B_otmp], writes=[Bob, Br])
                        S.op("act", lambda e, r=r: e.activation(out=r[:, 3:4], in_=r[:, 2:3], func=AF.Sqrt, scale=1.0 / 128,
                                                                bias=EPS_T[:, 1:2]),
                             reads=[Br, B_eps], writes=[Br])
                        S.op("dve", lambda e, r=r: e.reciprocal(out=r[:, 3:4], in_=r[:, 3:4]), reads=[Br], writes=[Br])
                        S.op("dve", lambda e, o_b=o_b, r=r, tt=tt: e.scalar_tensor_tensor(
                            out=o_b[:], in0=otmp[:, tt, :], scalar=r[:, 3:4], in1=sg[:], op0=ALU.mult, op1=ALU.mult),
                            reads=[Br, B_otmp, Bsg], writes=[Bob])
                        ps, Bps = banks[6]
                        psb = ps[:].bitcast(BF16)
                        S.mm_group([lambda pe, o_b=o_b: pe.transpose(out=psb[:, 0:128], in_=o_b[:], identity=idb[:])],
                                   reads=[Bob, B_idb], writes=[Bps])
                        S.op("act", lambda e, tt=tt: e.activation(out=obT[:, head, tt * 128:(tt + 1) * 128],
                                                                  in_=psb[:, 0:128], func=AF.Copy),
                             reads=[Bps], parts=[B_obT])
            return f

        jobs = []
        for hh in range(8):
            for g in range(3):
                qcol = g * 512 + hh * 64
                jobs.append((qcol, 1536 + qcol, 3072 + qcol, 64, 64, g, hh, fin_dil(g == 0, g == 2, hh % 2, hh // 2)))
        for hd in range(8):
            for s in range(2):
                qcol = 4608 + hd * 128 + s * 64
                jobs.append((qcol, qcol + 1024, 6656 + hd * 128, 64, 128, 3, hd, fin_diff(s, hd)))
        stream1(0, *jobs[0])
        for ji, jb in enumerate(jobs):
            if ji + 1 < len(jobs):
                stream1((ji + 1) % 2, *jobs[ji + 1])
            stream2(ji % 2, *jb)
        S.barrier()
        release(m2)
        m3 = mark()
        G1, BG1 = sb("G1", [128, D], F32)
        S.dma("sp", G1[:], mod_d[:, 2 * D:3 * D], BG1, reads=[B_mod], writes=[BG1])
        wga, Bwga = sb("wga", [128, 8, 512], BF16)
        wgb, Bwgb = sb("wgb", [128, 8, 512], BF16)
        wpa, Bwpa = sb("wpa", [128, 4, 512], BF16)
        wpb, Bwpb = sb("wpb", [128, 8, 512], BF16)
        wo, Bwo = sb("wo", [128, 4, D], BF16)
        sga = [sb("sga%d" % i, [128, 512], F32) for i in range(2)]
        sgb = [sb("sgb%d" % i, [128, 512], F32) for i in range(2)]
        mg = [sb("mg%d" % i, [128, 512], BF16) for i in range(2)]
        mgT = [sb("mgT%d" % i, [128, 4, 128], BF16) for i in range(2)]
        wsrc = w_in[l].rearrange("(a p) n -> p a n", p=128)
        for nchk in range(2):
            n0 = nchk * 512
            S.dma("pool", wga[:], wsrc[:, :, 7680 + n0:7680 + n0 + 512], Bwga, writes=[Bwga])
            S.dma("pool", wgb[:], wsrc[:, :, 8704 + n0:8704 + n0 + 512], Bwgb, writes=[Bwgb])
            S.dma("pool", wpa[:], w_pa[l].rearrange("(a p) n -> p a n", p=128)[:, :, n0:n0 + 512], Bwpa, writes=[Bwpa])
            S.dma("pool", wpb[:], w_pb[l].rearrange("(a p) n -> p a n", p=128)[:, :, n0:n0 + 512], Bwpb, writes=[Bwpb])
            S.dma("pool", wo[:], w_o[l, n0:n0 + 512, :].rearrange("(a p) n -> p a n", p=128), Bwo, writes=[Bwo])
            for tt in range(NTT):
                i2 = tt % 2
                tsl = slice(tt * 128, (tt + 1) * 128)
                (pga, Bpga), (pgb, Bpgb), (ppa, Bppa), (ppb, Bppb) = banks[0], banks[1], banks[2], banks[3]
                S.mm_group([lambda pe, kc=kc: pe.matmul(pga[:], lhsT=hT[:, kc, tsl], rhs=wga[:, kc, :], start=(kc == 0), stop=(kc == 7))
                            for kc in range(8)], reads=[B_hT, Bwga], writes=[Bpga])
                S.mm_group([lambda pe, kc=kc: pe.matmul(pgb[:], lhsT=hT[:, kc, tsl], rhs=wgb[:, kc, :], start=(kc == 0), stop=(kc == 7))
                            for kc in range(8)], reads=[B_hT, Bwgb], writes=[Bpgb])
                S.mm_group([lambda pe, kc=kc: pe.matmul(ppa[:], lhsT=oaT[:, kc, tsl], rhs=wpa[:, kc, :], start=(kc == 0), stop=(kc == 3))
                            for kc in range(4)], reads=[B_oaT, Bwpa], writes=[Bppa])
                S.mm_group([lambda pe, kc=kc: pe.matmul(ppb[:], lhsT=obT[:, kc, tsl], rhs=wpb[:, kc, :], start=(kc == 0), stop=(kc == 7))
                            for kc in range(8)], reads=[B_obT, Bwpb], writes=[Bppb])
                (a_, Ba_), (b_, Bb_), (mg_, Bmg), (mgT_, BmgT) = sga[i2], sgb[i2], mg[i2], mgT[i2]
                S.op("act", lambda e, a_=a_: e.activation(out=a_[:], in_=pga[:], func=AF.Sigmoid), reads=[Bpga], writes=[Ba_])
                S.op("act", lambda e, b_=b_: e.activation(out=b_[:], in_=pgb[:], func=AF.Sigmoid), reads=[Bpgb], writes=[Bb_])
                S.op("dve", lambda e, a_=a_: e.tensor_tensor(out=a_[:], in0=a_[:], in1=ppa[:], op=ALU.mult), reads=[Ba_, Bppa], writes=[Ba_])
                S.op("dve", lambda e, b_=b_: e.tensor_tensor(out=b_[:], in0=b_[:], in1=ppb[:], op=ALU.mult), reads=[Bb_, Bppb], writes=[Bb_])
                S.op("pool", lambda e, a_=a_, b_=b_, mg_=mg_: e.tensor_tensor(out=mg_[:], in0=a_[:], in1=b_[:], op=ALU.add),
                     reads=[Ba_, Bb_], writes=[Bmg])
                ps, Bps = banks[6]
                psb = ps[:].bitcast(BF16)
                S.mm_group([lambda pe, c=c, mg_=mg_: pe.transpose(out=psb[:, c * 128:(c + 1) * 128], in_=mg_[:, c * 128:(c + 1) * 128],
                                                                  identity=idb[:]) for c in range(4)],
                           reads=[Bmg, B_idb], writes=[Bps])
                S.op("act", lambda e, mgT_=mgT_: e.activation(out=mgT_[:], in_=psb[:, 0:512].rearrange("p (a b) -> p a b", a=4), func=AF.Copy),
                     reads=[Bps], writes=[BmgT])
                for half in range(2):
                    po, Bpo = banks[4 + half]
                    S.mm_group([lambda pe, c=c, mgT_=mgT_, po=po, half=half: pe.matmul(
                        po[:], lhsT=mgT_[:, c, :], rhs=wo[:, c, half * 512:(half + 1) * 512], start=(c == 0), stop=(c == 3))
                        for c in range(4)], reads=[BmgT, Bwo], writes=[Bpo])
                    S.op("dve", lambda e, po=po, half=half, tt=tt: e.tensor_tensor(
                        out=po[:], in0=po[:], in1=G1[:, half * 512:(half + 1) * 512], op=ALU.mult),
                        reads=[Bpo, BG1], writes=[Bpo])
                    S.op("dve", lambda e, po=po, half=half, tt=tt: e.tensor_tensor(
                        out=x_t[:, tt, half * 512:(half + 1) * 512], in0=x_t[:, tt, half * 512:(half + 1) * 512], in1=po[:], op=ALU.add),
                        reads=[Bpo, B_xt[tt]], parts=[B_xt[tt]])
        S.barrier()
        release(m3)
        return m, hT


    def peer_prep_gen(l, bufs):
        ut_f, ut_b, v_f, v_b = bufs
        NB_ = len(ut_f)
        for et in range(128):
            i2 = et % NB_
            (uf, Buf_), (ub, Bub), (vf, Bvf), (vb_, Bvb) = ut_f[i2], ut_b[i2], v_f[i2], v_b[i2]
            S.dma("sp", uf[:], p_u[l, et * 128:(et + 1) * 128, :], Buf_, writes=[Buf_])
            S.dma("sp", vf[:], p_v[l, et * 128:(et + 1) * 128, :], Bvf, writes=[Bvf])
            for hb_ in range(2):
                ps, Bps = banks[2 + hb_]
                S.mm_group([lambda pe, c=c, ps=ps, uf=uf, hb_=hb_: pe.transpose(
                    out=ps[:, c * 128:(c + 1) * 128], in_=uf[:, (hb_ * 4 + c) * 128:(hb_ * 4 + c + 1) * 128], identity=idf[:])
                    for c in range(4)], reads=[Buf_, B_idf], writes=[Bps])
                S.op("act", lambda e, ps=ps, ub=ub, hb_=hb_: e.activation(
                    out=ub[:, hb_ * 4:(hb_ + 1) * 4, :], in_=ps[:].rearrange("p (a b) -> p a b", a=4), func=AF.Copy),
                    reads=[Bps], parts=[Bub])
            S.dma("sp", ut_d[:, et, :, :], ub[:], B_ut, reads=[Bub], parts=[B_ut])
            S.op("pool", lambda e, vb_=vb_, vf=vf: e.tensor_copy(out=vb_[:], in_=vf[:]), reads=[Bvf], writes=[Bvb])
            S.dma("sp", vb_d[:, et, :], vb_[:], B_vb, reads=[Bvb], parts=[B_vb])
            yield

    def peer_phase(l):
        m = mark()
        h2T, B_h2T = sb("h2T", [128, 8, T], BF16)
        G2, BG2 = sb("G2", [128, D], F32)
        S.dma("sp", G2[:], mod_d[:, 5 * D:6 * D], BG2, reads=[B_mod], writes=[BG2])
        mA = mark()
        pbufs = ([sb("utf%d" % i, [128, D], F32) for i in range(2)], [sb("utb%d" % i, [128, 8, 128], BF16) for i in range(2)],
                 [sb("vf%d" % i, [128, D], F32) for i in range(2)], [sb("vb%d" % i, [128, D], BF16) for i in range(2)])
        prep = peer_prep_gen(l, pbufs)
        A, BA, Bt, BB = make_AB(l, 2)
        tmp = norm_tmp()
        skT, B_skT = sb("skT", [128, 2, 128], F32)
        skl, B_skl = sb("skl", [128, 128], F32)
        for p in range(2):
            S.dma("sp", skl[:], p_sk[l, p, :, :], B_skl, writes=[B_skl])
            ps, Bps = banks[4]
            S.mm_group([lambda pe, ps=ps: pe.transpose(out=ps[:, 0:128], in_=skl[:], identity=idf[:])],
                       reads=[B_skl, B_idf], writes=[Bps])
            S.op("act", lambda e, ps=ps, p=p: e.activation(out=skT[:, p, :], in_=ps[:, 0:128], func=AF.Copy),
                 reads=[Bps], parts=[B_skT])
        wqb = [sb("wqb%d" % i, [128, 8, 128], BF16) for i in range(4)]
        qTs = [sb("qTs%d" % i, [128, 128], F32) for i in range(2)]
        sc_l = [sb("sc%d" % i, [128, 16, 128], F32) for i in range(2)]
        sc2, B_sc2 = sb("sc2", [128, 16, 128], F32)
        T16, B_T16 = sb("T16", [128, 16, 16], F32)
        cand, B_cand = sb("cand", [128, 8, 256], F32)
        S24, B_S24 = sb("S24", [128, 8, 24], F32)
        sm, B_sm = sb("sm", [128, 8, 16], F32)
        sv_ = {}
        for nm in ["negM", "tau", "nh", "Z", "e1"]:
            sv_[nm] = sb(nm, [128, 8], F32)
        kap_l = [sb("kapA%d" % i, [128, 8], F32) for i in range(2)]
        wsrc = p_wq[l].rearrange("(a p) n -> p a n", p=128)
        cand2v = sc2[:].rearrange("p (g two) k -> p g (two k)", two=2)
        cn = {"w": 0}
        NG = 8
        for tt in range(NTT):
            for _ in range(8):
                next(prep, None)
            norm_tile(tt, A, BA, Bt, BB, h2T, B_h2T, tt * 128, tmp, banks[7])
            sc, B_sc = sc_l[tt % 2]
            kap, Bkap = kap_l[tt % 2]
            for hp in range(16):
                i2 = cn["w"] % 2
                (wb, Bwb), (qs, Bqs) = wqb[cn["w"] % 4], qTs[i2]
                cn["w"] += 1
                S.dma("pool", wb[:], wsrc[:, :, hp * 128:(hp + 1) * 128], Bwb, writes=[Bwb])
                ps, Bps = banks[4 + i2]
                S.mm_group([lambda pe, kc=kc, ps=ps, wb=wb: pe.matmul(ps[:, :128], lhsT=wb[:, kc, :], rhs=h2T[:, kc, tt * 128:(tt + 1) * 128],
                                                                     start=(kc == 0), stop=(kc == 7)) for kc in range(8)],
                           reads=[Bwb, B_h2T], writes=[Bps])
                S.op("dve", lambda e, ps=ps, qs=qs: e.tensor_copy(out=qs[:], in_=ps[:, :128]), reads=[Bps], writes=[Bqs])
                ps2, Bps2 = banks[6 + (hp % 2)]
                S.mm_group([lambda pe, qs=qs, hp=hp, ps2=ps2: pe.matmul(ps2[:, 0:128], lhsT=qs[:], rhs=skT[:, hp % 2, :], start=True, stop=True)],
                           reads=[Bqs, B_skT], writes=[Bps2])
                S.op("act", lambda e, hp=hp, ps2=ps2, sc=sc: e.activation(out=sc[:, hp, :], in_=ps2[:, 0:128], func=AF.Copy),
                     reads=[Bps2], parts=[B_sc])
            for g in range(16):
                S.op("dve", lambda e, g=g: e.max(out=T16[:, g, 0:8], in_=sc[:, g, :]), reads=[B_sc], parts=[B_T16])
            for g in range(16):
                S.op("dve", lambda e, g=g: e.match_replace(out=sc2[:, g, :], in_to_replace=T16[:, g, 0:8], in_values=sc[:, g, :], imm_value=-1e30),
                     reads=[B_sc, B_T16], parts=[B_sc2])
            for g in range(16):
                S.op("dve", lambda e, g=g: e.max(out=T16[:, g, 8:16], in_=sc2[:, g, :]), reads=[B_sc2], parts=[B_T16])
            T16v = T16[:].rearrange("p (g two) r -> p g two r", two=2)
            S.op("dve", lambda e: e.tensor_tensor(out=cand[:].rearrange("p g (r s) -> p g r s", r=16),
                                                  in0=T16v[:, :, 0, :].unsqueeze(3).to_broadcast([128, NG, 16, 16]),
                                                  in1=T16v[:, :, 1, :].unsqueeze(2).to_broadcast([128, NG, 16, 16]), op=ALU.add),
                 reads=[B_T16], writes=[B_cand])
            for g in range(NG):
                S.op("dve", lambda e, g=g: e.max(out=S24[:, g, 0:8], in_=cand[:, g, :]), reads=[B_cand], parts=[B_S24])
            for g in range(NG):
                S.op("dve", lambda e, g=g: e.match_replace(out=cand2v[:, g, :], in_to_replace=S24[:, g, 0:8], in_values=cand[:, g, :], imm_value=-1e30),
                     reads=[B_cand, B_S24, B_sc2], parts=[B_sc2])
            for g in range(NG):
                S.op("dve", lambda e, g=g: e.max(out=S24[:, g, 8:16], in_=cand2v[:, g, :]), reads=[B_sc2], parts=[B_S24])
            for g in range(NG):
                S.op("dve", lambda e, g=g: e.match_replace(out=cand2v[:, g, :], in_to_replace=S24[:, g, 8:16], in_values=cand2v[:, g, :], imm_value=-1e30),
                     reads=[B_sc2, B_S24], parts=[B_sc2])
            for g in range(NG):
                S.op("dve", lambda e, g=g: e.max(out=S24[:, g, 16:24], in_=cand2v[:, g, :]), reads=[B_sc2], parts=[B_S24])
            (negM, BnegM), (tau, Btau), (nh, Bnh), (Z, BZ), (e1, Be1) = [sv_[n] for n in ["negM", "tau", "nh", "Z", "e1"]]
            S.op("dve", lambda e: e.tensor_scalar(out=negM[:], in0=S24[:, :, 0], scalar1=-1.0, scalar2=None, op0=ALU.mult),
                 reads=[B_S24], writes=[BnegM])
            S.op("dve", lambda e: e.tensor_tensor(out=tau[:], in0=S24[:, :, 15], in1=S24[:, :, 16], op=ALU.add), reads=[B_S24], writes=[Btau])
            S.op("dve", lambda e: e.tensor_scalar(out=tau[:], in0=tau[:], scalar1=0.5, scalar2=None, op0=ALU.mult), reads=[Btau], writes=[Btau])
            S.op("dve", lambda e: e.tensor_scalar(out=nh[:], in0=tau[:], scalar1=-0.5, scalar2=None, op0=ALU.mult), reads=[Btau], writes=[Bnh])
            S.op("dve", lambda e: e.tensor_tensor(out=sm[:], in0=S24[:, :, 0:16], in1=negM[:].unsqueeze(2).to_broadcast([128, NG, 16]), op=ALU.add),
                 reads=[B_S24, BnegM], writes=[B_sm])
            S.op("act", lambda e: e.activation(out=sm[:], in_=sm[:], func=AF.Exp), reads=[B_sm], writes=[B_sm])
            S.op("dve", lambda e: e.reduce_sum(out=Z[:], in_=sm[:], axis=AX.X), reads=[B_sm], writes=[BZ])
            S.op("dve", lambda e: e.tensor_tensor(out=e1[:], in0=tau[:], in1=negM[:], op=ALU.add), reads=[Btau, BnegM], writes=[Be1])
            S.op("act", lambda e: e.activation(out=e1[:], in_=e1[:], func=AF.Exp), reads=[Be1], writes=[Be1])
            S.op("dve", lambda e: e.reciprocal(out=Z[:], in_=Z[:]), reads=[BZ], writes=[BZ])
            S.op("dve", lambda e, kap=kap: e.tensor_tensor(out=kap[:], in0=e1[:], in1=Z[:], op=ALU.mult), reads=[Be1, BZ], writes=[Bkap])
            scv = sc[:].rearrange("p (g two) k -> p g (two k)", two=2)
            S.op("dve", lambda e, scv=scv: e.tensor_tensor(out=scv, in0=scv, in1=nh[:].unsqueeze(2).to_broadcast([128, NG, 256]), op=ALU.add),
                 reads=[B_sc, Bnh], writes=[B_sc])
            S.op("act", lambda e, sc=sc: e.activation(out=sc[:], in_=sc[:], func=AF.Exp), reads=[B_sc], writes=[B_sc])
            S.op("dve", lambda e, scv=scv, kap=kap: e.tensor_tensor(out=scv[:, :, 0:128], in0=scv[:, :, 0:128],
                                                                  in1=kap[:].unsqueeze(2).to_broadcast([128, NG, 128]), op=ALU.mult),
                 reads=[B_sc, Bkap], writes=[B_sc])
            S.dma("act", scx_d[tt], scv, B_scx, reads=[B_sc], parts=[B_scx])
            S.dma("act", kap_d[tt], kap[:], B_kapd, reads=[Bkap], parts=[B_kapd])
        for _ in prep:
            pass
        S.barrier()
        release(mA)
        NT = 2
        NBLK = NTT // NT
        TB = NT * 128
        NGB = NT * 8
        IC = 4
        scx, B_scxs = sb("scx", [128, NGB, 256], F32)
        kapB, B_kapB = sb("kapB", [128, NGB], F32)
        Ft = [[sb("F%d_%d" % (bf, g), [128, IC * 128], BF16) for g in range(NGB)] for bf in range(2)]
        u_pool = [sb("up%d" % i, [128, IC * 128], F32) for i in range(3)]
        u_act = [sb("ua%d" % i, [128, IC * 128], F32) for i in range(3)]
        utc = [sb("utc%d" % i, [128, IC, 8, 128], BF16) for i in range(2)]
        vbc = [sb("vbc%d" % i, [128, IC, D], BF16) for i in range(2)]
        gl_t = [sb("gl%d" % i, [128, TB], F32) for i in range(2)]
        ga_t = [sb("ga%d" % i, [128, TB], BF16) for i in range(2)]
        xo, Bxo = sb("xo", [128, 512], F32)
        accO = [banks[0], banks[1], banks[2], banks[3]]
        cu = {"p": 0, "a": 0}
        NCH = 128 // IC
        for blk in range(NBLK):
            for j in range(NT):
                S.dma("sp", scx[:, j * 8:(j + 1) * 8, :], scx_d[blk * NT + j], B_scxs, reads=[B_scx],
                      **({"writes": [B_scxs]} if j == 0 else {"parts": [B_scxs]}))
                S.dma("sp", kapB[:, j * 8:(j + 1) * 8], kap_d[blk * NT + j], B_kapB, reads=[B_kapd],
                      **({"writes": [B_kapB]} if j == 0 else {"parts": [B_kapB]}))
            hcols = slice(blk * TB, (blk + 1) * TB)

            def load_chunk(ic):
                bf = ic % 2
                (uc, Buc) = utc[bf]
                S.dma("sp", uc[:], ut_d[:, ic * IC:(ic + 1) * IC, :, :], Buc, reads=[B_ut], writes=[Buc])

            def load_chunk_v(ic):
                bf = ic % 2
                (vc, Bvc) = vbc[bf]
                S.dma("sp", vc[:], vb_d[:, ic * IC:(ic + 1) * IC, :], Bvc, reads=[B_vb], writes=[Bvc])

            def build_quarter(ic, q):
                bf = ic % 2
                for g in range(q * 4, q * 4 + 4):
                    Fg, BFg = Ft[bf][g]
                    if g % 2 == 0:
                        u_, Bu_ = u_pool[cu["p"] % 3]
                        cu["p"] += 1
                        S.op("pool", lambda e, u_=u_, g=g, ic=ic: e.tensor_tensor(
                            out=u_[:].rearrange("p (i j) -> p i j", i=IC),
                            in0=scx[:, g, ic * IC:(ic + 1) * IC].unsqueeze(2).to_broadcast([128, IC, 128]),
                            in1=scx[:, g, 128:256].unsqueeze(1).to_broadcast([128, IC, 128]), op=ALU.mult),
                            reads=[B_scxs], writes=[Bu_])
                    else:
                        u_, Bu_ = u_act[cu["a"] % 3]
                        cu["a"] += 1
                        for i in range(IC):
                            S.op("act", lambda e, u_=u_, g=g, ic=ic, i=i: e.activation(
                                out=u_[:, i * 128:(i + 1) * 128], in_=scx[:, g, 128:256], func=AF.Copy,
                                scale=scx[:, g, ic * IC + i:ic * IC + i + 1]),
                                reads=[B_scxs], **({"writes": [Bu_]} if i == 0 else {"parts": [Bu_]}))
                    S.op("dve", lambda e, u_=u_, Fg=Fg, g=g: e.scalar_tensor_tensor(
                        out=Fg[:], in0=u_[:], scalar=kapB[:, g:g + 1], in1=u_[:], op0=ALU.is_ge, op1=ALU.mult),
                        reads=[Bu_, B_kapB], writes=[BFg])

            def emit_out(et):
                ic, il = et // IC, et % IC
                vc, Bvc = vbc[ic % 2]
                ga, Bga = ga_t[et % 2]
                for j in range(NT):
                    for half in range(2):
                        ab, Bab = accO[j * 2 + half]
                        S.mm_group([lambda pe, j=j, half=half, ab=ab, ga=ga, vc=vc, il=il, et=et: pe.matmul(
                            ab[:], lhsT=ga[:, j * 128:(j + 1) * 128], rhs=vc[:, il, half * 512:(half + 1) * 512],
                            start=(et == 0), stop=(et == 127), skip_group_check=True)],
                            reads=[Bga, Bvc], **({"writes": [Bab]} if et == 0 else {"parts": [Bab]}))

            load_chunk(0)
            load_chunk_v(0)
            for q in range(4):
                build_quarter(0, q)
            for et in range(128):
                ic, il = et // IC, et % IC
                bf = ic % 2
                if il == 0 and ic + 1 < NCH:
                    load_chunk(ic + 1)
                if ic + 1 < NCH:
                    build_quarter(ic + 1, il)
                uc, Buc = utc[bf]
                hbk, Bhbk = banks[4 + et % 2]
                gbk, Bgbk = banks[6 + et % 2]
                S.mm_group([lambda pe, kc=kc, il=il, hbk=hbk, uc=uc: pe.matmul(hbk[:, :TB], lhsT=uc[:, il, kc, :], rhs=h2T[:, kc, hcols],
                                                                             start=(kc == 0), stop=(kc == 7)) for kc in range(8)],
                           reads=[Buc, B_h2T], writes=[Bhbk])
                S.mm_group([lambda pe, j=j, h=h, il=il, gbk=gbk, bf=bf: pe.matmul(
                    gbk[:, j * 128:(j + 1) * 128], lhsT=Ft[bf][j * 8 + h][0][:, il * 128:(il + 1) * 128], rhs=idb[:],
                    start=(j == 0 and h == 0), stop=(h == 7), skip_group_check=True) for j in range(NT) for h in range(8)],
                    reads=[Ft[bf][g][1] for g in range(NGB)] + [B_idb], writes=[Bgbk])
                (gl, Bgl), (ga, Bga) = gl_t[et % 2], ga_t[et % 2]
                S.op("act", lambda e, gl=gl, hbk=hbk: e.activation(out=gl[:], in_=hbk[:, :TB], func=AF.Gelu_apprx_tanh),
                     reads=[Bhbk], writes=[Bgl])
                S.op("dve", lambda e, gl=gl, ga=ga, gbk=gbk: e.tensor_tensor(out=ga[:], in0=gl[:], in1=gbk[:, :TB], op=ALU.mult),
                     reads=[Bgl, Bgbk], writes=[Bga])
                if et > 0:
                    emit_out(et - 1)
                if il == 0 and ic + 1 < NCH:
                    load_chunk_v(ic + 1)
            emit_out(127)
            for j in range(NT):
                tt = blk * NT + j
                for half in range(2):
                    ab, Bab = accO[j * 2 + half]
                    S.op("dve", lambda e, ab=ab, half=half: e.tensor_tensor(out=xo[:], in0=ab[:], in1=G2[:, half * 512:(half + 1) * 512], op=ALU.mult),
                         reads=[Bab, BG2], writes=[Bxo])
                    S.op("dve", lambda e, half=half, tt=tt: e.tensor_tensor(
                        out=x_t[:, tt, half * 512:(half + 1) * 512], in0=x_t[:, tt, half * 512:(half + 1) * 512], in1=xo[:], op=ALU.add),
                        reads=[Bxo, B_xt[tt]], parts=[B_xt[tt]])
        S.barrier()
        release(m)

    def final_phase():
        m = mark()
        fg, Bfg = load_bc("fg_bc", fin_g.rearrange("(o n) -> o n", o=1), D)
        tmp = norm_tmp()
        (sq, Bsq), (ss, Bss), (rs, Brs), (hn, Bhn), (hb, Bhb) = tmp
        for tt in range(NTT):
            norm_tile(tt, fg, Bfg, None, None, None, None, 0, tmp, None)
            S.dma("sp", y_d[tt * 128:(tt + 1) * 128, :], hn[:], B_y, reads=[Bhn], parts=[B_y])
        S.barrier()
        release(m)

    def dump_x():
        for tt in range(NTT):
            S.dma("sp", y_d[tt * 128:(tt + 1) * 128, :], x_t[:, tt, :], B_y, reads=[B_xt[tt]], parts=[B_y])
        S.barrier()

    for l in range(l0, l1):
        mod_phase(l)
        if stop_after == "mod":
            S.dma("sp", y_d[0:128, :].rearrange("p (a n) -> p a n", a=1)[:, 0, :], x_t[:, 0, :], B_y, reads=[B_xt[0]], parts=[B_y])
            break
        mm, hT = mixer_phase(l)
        if stop_after == "h1":
            hf, Bhf = sb("hf", [128, 8, 128], F32)
            break
        release(mm)
        if stop_after == "mix":
            break
        peer_phase(l)
    if stop_after is None and last:
        final_phase()
    else:
        dump_x()
    S.finish()
    return nc


def t5_bucket_np(d):
    d = np.maximum(d, 0)
    ratio = np.log(np.maximum(d, 1).astype(np.float32) / np.float32(16)) / np.float32(math.log(2048 / 16))
    large = np.minimum(16 + (ratio * np.float32(16)).astype(np.int32), 31)
    return np.where(d < 16, d, large)


def make_btab(rel_bias):
    ext = np.concatenate([rel_bias.astype(np.float32), np.full((1, 32), -30000.0, np.float32)], axis=0)
    out = np.empty((128, TABCOLS), np.float32)
    k = np.arange(128)[:, None, None]
    q = np.arange(128)[None, None, :]
    for typ in range(4):
        nb = TAB_NB[typ]
        Dd = (np.arange(nb) - 3)[None, :, None]
        delta = 128 * Dd + q - k
        valid = delta >= 0
        if TAB_W[typ] is not None:
            r = TAB_R[typ]
            valid = valid & (delta % r == 0) & (delta // r <= 128)
        idx = np.where(valid, t5_bucket_np(delta), 32)
        for h in range(8):
            hg = typ * 8 + h
            o = tab_off(typ, h)
            out[:, o:o + nb * 128] = ext[idx, hg].reshape(128, nb * 128)
    return out


_CACHE = {}


def run_layers(inputs, xin, l0, l1, first, last, stop_after=None, cores=8, with_peer=True):
    nc = build(l0, l1, first, last, stop_after, with_peer)
    btab = make_btab(np.asarray(inputs["rel_bias"]))
    ident = np.eye(128, dtype=np.float32)
    shared = {k: np.ascontiguousarray(np.asarray(inputs[k], dtype=np.float32)) for k in
              ["w_ada", "b_ada", "norm1_g", "norm2_g", "w_in", "w_proj_a", "w_proj_b", "w_out", "lam_q1", "lam_k1",
               "lam_q2", "lam_k2", "subln_g", "peer_wq", "peer_subkeys", "peer_u", "peer_v", "final_g"]}
    if not with_peer:
        del shared["peer_u"], shared["peer_v"]
    shared["btab"] = btab
    shared["ident"] = ident
    c = np.asarray(inputs["c"], dtype=np.float32)
    in_maps = []
    for b in range(cores):
        mp = dict(shared)
        mp["x"] = np.ascontiguousarray(xin[b])
        mp["c"] = np.ascontiguousarray(c[b].reshape(8, 128).T)
        in_maps.append(mp)
    res = run_bass_kernel_spmd(nc, in_maps, core_ids=list(range(cores)))
    return np.stack([np.asarray(r["y"]) for r in res.results], axis=0)


def kernel(**inputs):
    x = np.asarray(inputs["x"], dtype=np.float32)
    out = run_layers(inputs, x, 0, DEPTH, True, True)
    return out.astype(np.float32)
```

```python
import math
import numpy as np
import concourse.bass as bass
import concourse.mybir as mybir
from concourse.bass_utils import run_bass_kernel_spmd

F32 = mybir.dt.float32
BF16 = mybir.dt.bfloat16
AF = mybir.ActivationFunctionType
ALU = mybir.AluOpType
AX = mybir.AxisListType

D = 1024
T = 2048
NTT = 16
DEPTH = 4
INW = 9728
NEXP = 16384
TAB_NB = [8, 11, 22, 22]
TAB_W = [128, 512, 2048, None]
TAB_R = [1, 4, 16, 1]
TAB_DMAX = [1, 4, 15, 15]
TABCOLS = sum(TAB_NB) * 128 * 8


def tab_off(typ, head):
    off = 0
    for t in range(typ):
        off += TAB_NB[t] * 128 * 8
    return off + head * TAB_NB[typ] * 128


class Buf:
    def __init__(self, name):
        self.name = name
        self.writers = {}
        self.readers = {}
        self.sem = None
        self.dma_total = 0


class Eng:
    def __init__(self, name, obj, sem):
        self.name = name
        self.obj = obj
        self.sem = sem
        self.cnt = 0
        self.waited = {}


class Sched:
    def __init__(self, nc):
        self.nc = nc
        self.sems = {}
        self.engs = {}
        for name, obj in [("pe", nc.tensor), ("act", nc.scalar), ("dve", nc.vector), ("pool", nc.gpsimd), ("sp", nc.sync)]:
            s = self.new_sem("e_" + name)
            self.engs[name] = Eng(name, obj, s)
        self.dma_bufs = []
        self.free_sems = []

    def recycle(self, b):
        if b.sem is not None:
            self.free_sems.append((b.sem, b.dma_total))
            self.dma_bufs.remove(b)
            b.sem = None

    def new_sem(self, name):
        s = self.nc.semaphore(name).__enter__()
        self.sems[id(s)] = s
        return s

    def _wait(self, eng, need):
        for sid, val in need.items():
            if eng.waited.get(sid, 0) < val:
                eng.obj.wait_ge(self.sems[sid], val)
                eng.waited[sid] = val

    def _need(self, own_sid, reads, writes, parts):
        need = {}

        def merge(d):
            for k, v in d.items():
                if need.get(k, 0) < v:
                    need[k] = v
        for b in reads:
            merge(b.writers)
        for b in writes:
            merge(b.writers)
            merge(b.readers)
        for b in parts:
            merge(b.readers)
            merge({k: v for k, v in b.writers.items() if k != own_sid})
        return need

    def _record(self, sid, val, reads, writes, parts):
        for b in reads:
            if b.readers.get(sid, 0) < val:
                b.readers[sid] = val
        for b in writes:
            b.writers = {sid: val}
            b.readers = {}
        for b in parts:
            b.writers[sid] = val

    def op(self, engname, fn, reads=(), writes=(), parts=()):
        eng = self.engs[engname]
        sid = id(eng.sem)
        self._wait(eng, self._need(sid, reads, writes, parts))
        ins = fn(eng.obj)
        ins.then_inc(eng.sem, 1)
        eng.cnt += 1
        self._record(sid, eng.cnt, reads, writes, parts)

    def mm_group(self, mms, reads, writes=(), parts=()):
        eng = self.engs["pe"]
        sid = id(eng.sem)
        self._wait(eng, self._need(sid, reads, writes, parts))
        ins = None
        for f in mms:
            ins = f(eng.obj)
        ins.then_inc(eng.sem, 1)
        eng.cnt += 1
        self._record(sid, eng.cnt, reads, writes, parts)

    def dma(self, qname, out_ap, in_ap, owner, reads=(), writes=(), parts=()):
        eng = self.engs[qname]
        if owner.sem is None:
            if self.free_sems:
                owner.sem, owner.dma_total = self.free_sems.pop()
            else:
                owner.sem = self.new_sem("d_" + owner.name)
            self.dma_bufs.append(owner)
        sid = id(owner.sem)
        self._wait(eng, self._need(sid, reads, writes, parts))
        eng.obj.dma_start(out=out_ap, in_=in_ap).then_inc(owner.sem, 16)
        owner.dma_total += 16
        self._record(sid, owner.dma_total, reads, writes, parts)

    def barrier(self):
        allv = {}
        for e in self.engs.values():
            if e.cnt:
                allv[id(e.sem)] = e.cnt
        for b in self.dma_bufs:
            allv[id(b.sem)] = b.dma_total
        for e in self.engs.values():
            self._wait(e, allv)

    def finish(self):
        self.barrier()


def build(l0, l1, first, last, stop_after=None, with_peer=True):
    nc = bass.Bass("TRN2", target_bir_lowering=False)
    S = Sched(nc)

    def din(name, shape, dt=F32):
        return nc.dram_tensor(name, list(shape), dt, kind="ExternalInput").ap()

    x_d = din("x", [T, D])
    c_d = din("c", [128, 8])
    w_ada = din("w_ada", [DEPTH, D, 6 * D])
    b_ada = din("b_ada", [DEPTH, 6 * D])
    n1g = din("norm1_g", [DEPTH, D])
    n2g = din("norm2_g", [DEPTH, D])
    w_in = din("w_in", [DEPTH, D, INW])
    w_pa = din("w_proj_a", [DEPTH, 512, D])
    w_pb = din("w_proj_b", [DEPTH, D, D])
    w_o = din("w_out", [DEPTH, D, D])
    lq1 = din("lam_q1", [DEPTH, 64])
    lk1 = din("lam_k1", [DEPTH, 64])
    lq2 = din("lam_q2", [DEPTH, 64])
    lk2 = din("lam_k2", [DEPTH, 64])
    subg = din("subln_g", [DEPTH, 128])
    btab = din("btab", [128, TABCOLS])
    p_wq = din("peer_wq", [DEPTH, D, 2048])
    p_sk = din("peer_subkeys", [DEPTH, 2, 128, 128])
    p_u = din("peer_u", [DEPTH, NEXP, D]) if with_peer else None
    p_v = din("peer_v", [DEPTH, NEXP, D]) if with_peer else None
    fin_g = din("final_g", [D])
    ident_d = din("ident", [128, 128])
    y_d = nc.dram_tensor("y", [T, D], F32, kind="ExternalOutput").ap()

    etab_d = nc.dram_tensor("etab", [128, TABCOLS], BF16, kind="Internal").ap()
    mod_d = nc.dram_tensor("modscr", [128, 6 * D], F32, kind="Internal").ap()
    ut_d = nc.dram_tensor("utscr", [128, 128, 8, 128], BF16, kind="Internal").ap()
    vb_d = nc.dram_tensor("vbscr", [128, 128, D], BF16, kind="Internal").ap()
    scx_d = nc.dram_tensor("scxscr", [NTT, 128, 8, 256], F32, kind="Internal").ap()
    kap_d = nc.dram_tensor("kapscr", [NTT, 128, 8], F32, kind="Internal").ap()
    B_etab, B_mod, B_ut, B_vb, B_y = Buf("etab"), Buf("modscr"), Buf("utscr"), Buf("vbscr"), Buf("y")
    B_scx, B_kapd = Buf("scxscr"), Buf("kapscr")

    _ctx = []

    _uid = [0]

    def sb(name, shape, dt):
        _uid[0] += 1
        name = "s%d_%s" % (_uid[0], name)
        cm = nc.sbuf_tensor(name, list(shape), dt)
        t = cm.__enter__()
        b = Buf(name)
        _ctx.append((cm, b))
        return t, b

    def mark():
        return len(_ctx)

    def release(m):
        while len(_ctx) > m:
            cm, b = _ctx.pop()
            S.recycle(b)
            cm.__exit__(None, None, None)

    banks = []
    for i in range(8):
        t = nc.psum_tensor("bank%d" % i, [128, 512], F32).__enter__()
        banks.append((t, Buf("bank%d" % i)))

    x_t, B_x = sb("x", [128, NTT, D], F32)
    B_xt = [Buf("x%d" % i) for i in range(NTT)]
    idb, B_idb = sb("idb", [128, 128], BF16)
    idf, B_idf = sb("idf", [128, 128], F32)
    condrep, B_condrep = sb("condrep", [128, 8, 128], F32)
    lam_t, B_lam = sb("lam", [128, 4], F32)

    S.dma("pool", idb[:], ident_d[:, :], B_idb, writes=[B_idb])
    S.dma("sp", idf[:], ident_d[:, :], B_idf, writes=[B_idf])
    if first:
        for tt in range(NTT):
            S.dma("sp", x_t[:, tt, :], x_d[tt * 128:(tt + 1) * 128, :], B_xt[tt], writes=[B_xt[tt]])
    else:
        for tt in range(NTT):
            S.dma("sp", x_t[:, tt, :], x_d[tt * 128:(tt + 1) * 128, :], B_xt[tt], writes=[B_xt[tt]])

    m0 = mark()
    c_t, B_c = sb("c_t", [128, 8], F32)
    cs_t, B_cs = sb("cs_t", [128, 8], F32)
    S.dma("sp", c_t[:], c_d[:, :], B_c, writes=[B_c])
    S.op("act", lambda e: e.activation(out=cs_t[:], in_=c_t[:], func=AF.Silu), reads=[B_c], writes=[B_cs])
    S.op("dve", lambda e: e.tensor_copy(out=condrep[:], in_=cs_t[:].unsqueeze(2).to_broadcast([128, 8, 128])),
         reads=[B_cs], writes=[B_condrep])
    CH = 2048
    tb_f = [sb("tbf%d" % i, [128, CH], F32) for i in range(2)]
    tb_b = [sb("tbb%d" % i, [128, CH], BF16) for i in range(2)]
    nch = (TABCOLS + CH - 1) // CH
    for ci in range(nch):
        c0 = ci * CH
        cw = min(CH, TABCOLS - c0)
        tf, Bf = tb_f[ci % 2]
        tbb, Bb = tb_b[ci % 2]
        S.dma("sp", tf[:, :cw], btab[:, c0:c0 + cw], Bf, writes=[Bf])
        S.op("act", lambda e, tf=tf, tbb=tbb, cw=cw: e.activation(out=tbb[:, :cw], in_=tf[:, :cw], func=AF.Exp),
             reads=[Bf], writes=[Bb])
        S.dma("sp", etab_d[:, c0:c0 + cw], tbb[:, :cw], B_etab, reads=[Bb], parts=[B_etab])
    S.barrier()
    release(m0)

    def mod_phase(l):
        m = mark()
        wt = [sb("modw%d" % i, [128, 512], F32) for i in range(3)]
        bt = [sb("modb%d" % i, [128, 512], F32) for i in range(2)]
        mo = [sb("modo%d" % i, [128, 512], F32) for i in range(2)]
        k = 0
        for nchk in range(12):
            ps, Bps = banks[nchk % 2]
            btile, Bbt = bt[nchk % 2]
            S.dma("sp", btile[:], b_ada[l:l + 1, nchk * 512:(nchk + 1) * 512].partition_broadcast(128), Bbt, writes=[Bbt])
            for kc in range(8):
                w, Bw = wt[k % 3]
                k += 1
                S.dma("sp", w[:], w_ada[l, kc * 128:(kc + 1) * 128, nchk * 512:(nchk + 1) * 512], Bw, writes=[Bw])
                S.mm_group([lambda pe, kc=kc, w=w, ps=ps: pe.matmul(ps[:], lhsT=condrep[:, kc, :], rhs=w[:],
                                                                    start=(kc == 0), stop=(kc == 7))],
                           reads=[Bw, B_condrep], **({"writes": [Bps]} if kc == 0 else {"parts": [Bps]}))
            o, Bo = mo[nchk % 2]
            S.op("dve", lambda e, o=o, ps=ps, btile=btile: e.tensor_tensor(out=o[:], in0=ps[:], in1=btile[:], op=ALU.add),
                 reads=[Bps, Bbt], writes=[Bo])
            S.dma("sp", mod_d[:, nchk * 512:(nchk + 1) * 512], o[:], B_mod, reads=[Bo], parts=[B_mod])
        lt = [sb("lamv%d" % i, [128, 64], F32) for i in range(4)]
        for i, src in enumerate([lq1, lk1, lq2, lk2]):
            S.dma("sp", lt[i][0][:], src[l:l + 1, :].partition_broadcast(128), lt[i][1], writes=[lt[i][1]])
        pr, Bpr = sb("lampr", [128, 2, 64], F32)
        dd, Bdd = sb("lamdd", [128, 2], F32)
        ee, Bee = sb("lamee", [128, 2], F32)
        S.op("dve", lambda e: e.tensor_tensor(out=pr[:, 0, :], in0=lt[0][0][:], in1=lt[1][0][:], op=ALU.mult),
             reads=[lt[0][1], lt[1][1]], parts=[Bpr])
        S.op("dve", lambda e: e.tensor_tensor(out=pr[:, 1, :], in0=lt[2][0][:], in1=lt[3][0][:], op=ALU.mult),
             reads=[lt[2][1], lt[3][1]], parts=[Bpr])
        S.op("dve", lambda e: e.reduce_sum(out=dd[:], in_=pr[:], axis=AX.X), reads=[Bpr], writes=[Bdd])
        S.op("act", lambda e: e.activation(out=ee[:], in_=dd[:], func=AF.Exp), reads=[Bdd], writes=[Bee])
        lam_init = 0.8 - 0.6 * math.exp(-0.3 * l)
        S.op("dve", lambda e: e.scalar_tensor_tensor(out=lam_t[:, 0:1], in0=ee[:, 1:2], scalar=-lam_init, in1=ee[:, 0:1],
                                                     op0=ALU.add, op1=ALU.subtract),
             reads=[Bee], writes=[B_lam])
        S.barrier()
        release(m)

    def norm_tile(tt, A, BA, Bb, BBb, hT, B_hT, col0, tmp, ps_bank):
        (sq, Bsq), (ss, Bss), (rs, Brs), (hn, Bhn), (hb, Bhb) = tmp
        S.op("act", lambda e: e.activation(out=sq[:], in_=x_t[:, tt, :], func=AF.Square, accum_out=ss[:]),
             reads=[B_xt[tt]], writes=[Bsq, Bss])
        S.op("act", lambda e: e.activation(out=rs[:], in_=ss[:], func=AF.Sqrt, scale=1.0 / D, bias=EPS_T[:, 0:1]),
             reads=[Bss, B_eps], writes=[Brs])
        S.op("dve", lambda e: e.reciprocal(out=rs[:], in_=rs[:]), reads=[Brs], writes=[Brs])
        S.op("dve", lambda e: e.scalar_tensor_tensor(out=hn[:], in0=x_t[:, tt, :], scalar=rs[:, 0:1], in1=A[:],
                                                     op0=ALU.mult, op1=ALU.mult),
             reads=[B_xt[tt], Brs, BA], writes=[Bhn])
        if Bb is not None:
            S.op("pool", lambda e: e.tensor_tensor(out=hb[:], in0=hn[:], in1=Bb[:], op=ALU.add),
                 reads=[Bhn, BBb], writes=[Bhb])
        if hT is None:
            return
        ps, Bps = ps_bank
        psb = ps[:].bitcast(BF16)
        S.mm_group([lambda pe, dc=dc: pe.transpose(out=psb[:, dc * 128:(dc + 1) * 128], in_=hb[:, dc * 128:(dc + 1) * 128],
                                                   identity=idb[:]) for dc in range(8)],
                   reads=[Bhb, B_idb], writes=[Bps])
        S.op("act", lambda e: e.activation(out=hT[:, :, col0:col0 + 128],
                                           in_=psb.rearrange("p (a b) -> p a b", a=8), func=AF.Copy),
             reads=[Bps], parts=[B_hT])

    EPS_T, B_eps = sb("eps", [128, 2], F32)
    S.op("dve", lambda e: e.memset(EPS_T[:, 0:1], 1e-6), parts=[B_eps])
    S.op("dve", lambda e: e.memset(EPS_T[:, 1:2], 1e-5), parts=[B_eps])

    def load_bc(name, src_row_ap, n, q="sp"):
        t, Bt = sb(name, [128, n], F32)
        S.dma(q, t[:], src_row_ap.partition_broadcast(128), Bt, writes=[Bt])
        return t, Bt

    def norm_tmp():
        return [sb("n_sq", [128, D], BF16), sb("n_ss", [128, 1], F32), sb("n_rs", [128, 1], F32),
                sb("n_hn", [128, D], F32), sb("n_hb", [128, D], BF16)]

    def make_AB(l, which):
        base = 0 if which == 1 else 3 * D
        A, BA = sb("A_bc", [128, D], F32)
        Bt, BB = sb("B_bc", [128, D], F32)
        g, Bg = load_bc("ng_bc", (n1g if which == 1 else n2g)[l:l + 1, :], D)
        S.dma("sp", A[:], mod_d[:, base + D:base + 2 * D], BA, reads=[B_mod], writes=[BA])
        S.dma("sp", Bt[:], mod_d[:, base:base + D], BB, reads=[B_mod], writes=[BB])
        S.op("dve", lambda e: e.scalar_tensor_tensor(out=A[:], in0=A[:], scalar=1.0, in1=g[:], op0=ALU.add, op1=ALU.mult),
             reads=[BA, Bg], writes=[BA])
        return A, BA, Bt, BB

    def mixer_phase(l):
        m = mark()
        hT, B_hT = sb("hT", [128, 8, T], BF16)
        oaT, B_oaT = sb("oaT", [128, 4, T], BF16)
        obT, B_obT = sb("obT", [128, 8, T], BF16)
        m1 = mark()
        A, BA, Bt, BB = make_AB(l, 1)
        tmp = norm_tmp()
        for tt in range(NTT):
            norm_tile(tt, A, BA, Bt, BB, hT, B_hT, tt * 128, tmp, banks[7])
        S.barrier()
        release(m1)
        if stop_after == "h1":
            return m, hT
        m2 = mark()
        wq = [sb("wq%d" % i, [128, 8, 64], BF16) for i in range(2)]
        wk = [sb("wk%d" % i, [128, 8, 64], BF16) for i in range(2)]
        wv = [sb("wv%d" % i, [128, 8, 128], BF16) for i in range(2)]
        qT = [sb("qT%d" % i, [128, T], BF16) for i in range(2)]
        kT = [sb("kT%d" % i, [128, T], BF16) for i in range(2)]
        vx = [sb("vx0", [128, NTT, 129], BF16)] * 2
        tab = [sb("tab0", [128, 22 * 128], BF16)] * 2
        pe_t = [sb("pexp%d" % i, [128, 512], BF16) for i in range(2)]
        pm_t = [sb("pm%d" % i, [128, 512], BF16) for i in range(2)]
        acc_sb, B_acc = sb("accsb", [128, NTT, 65], F32)
        otmp, B_otmp = sb("otmp", [128, NTT, 128], F32)
        rr = [sb("rr%d" % i, [128, 4], F32) for i in range(2)]
        osb = [sb("osb%d" % i, [128, 128], BF16) for i in range(2)]
        oa2, B_oa2 = sb("oa2", [128, NTT, 128], BF16)
        sg, Bsg = load_bc("subg", subg[l:l + 1, :], 128)
        lam_init = 0.8 - 0.6 * math.exp(-0.3 * l)
        S.op("dve", lambda e: e.tensor_scalar(out=sg[:], in0=sg[:], scalar1=1.0 - lam_init, scalar2=None, op0=ALU.mult),
             reads=[Bsg], writes=[Bsg])
        S.op("pool", lambda e: e.memset(vx[0][0][:], 1.0), writes=[vx[0][1]])
        ctr = {"st": 0, "sc": 0, "rr": 0, "os": 0}

        def stream1(si, qcol, kcol, vcol, dh, dv, typ, thead, fin):
            (wq_t, Bwq), (wk_t, Bwk), (wv_t, Bwv) = wq[si], wk[si], wv[si]
            (q_t, Bq), (k_t, Bk) = qT[si], kT[si]
            wsrc = w_in[l].rearrange("(a p) n -> p a n", p=128)
            S.dma("pool", wq_t[:, :, :dh], wsrc[:, :, qcol:qcol + dh], Bwq, writes=[Bwq])
            S.dma("pool", wk_t[:, :, :dh], wsrc[:, :, kcol:kcol + dh], Bwk, writes=[Bwk])
            S.dma("pool", wv_t[:, :, :dv], wsrc[:, :, vcol:vcol + dv], Bwv, writes=[Bwv])
            for (w_t, Bw, o_t, Bo) in ((wq_t, Bwq, q_t, Bq), (wk_t, Bwk, k_t, Bk)):
                for qc in range(4):
                    ps, Bps = banks[4 + (qc % 2)]
                    S.mm_group([lambda pe, kc=kc, w_t=w_t, ps=ps, qc=qc: pe.matmul(
                        ps[:dh, :], lhsT=w_t[:, kc, :dh], rhs=hT[:, kc, qc * 512:(qc + 1) * 512],
                        start=(kc == 0), stop=(kc == 7)) for kc in range(8)],
                        reads=[Bw, B_hT], writes=[Bps])
                    S.op("act", lambda e, o_t=o_t, ps=ps, qc=qc: e.activation(
                        out=o_t[:dh, qc * 512:(qc + 1) * 512], in_=ps[:dh, :], func=AF.Copy),
                        reads=[Bps], parts=[Bo])

        def stream2(si, qcol, kcol, vcol, dh, dv, typ, thead, fin):
            (wq_t, Bwq), (wk_t, Bwk), (wv_t, Bwv) = wq[si], wk[si], wv[si]
            (q_t, Bq), (k_t, Bk), (v_t, Bv), (tb_t, Btb) = qT[si], kT[si], vx[si], tab[si]
            nb = TAB_NB[typ]
            to = tab_off(typ, thead)
            S.dma("sp", tb_t[:, :nb * 128], etab_d[:, to:to + nb * 128], Btb, reads=[B_etab], writes=[Btb])
            for tt in range(NTT):
                ps, Bps = banks[6 + (tt % 2)]
                S.mm_group([lambda pe, kc=kc, ps=ps, tt=tt: pe.matmul(
                    ps[:, :dv], lhsT=hT[:, kc, tt * 128:(tt + 1) * 128], rhs=wv_t[:, kc, :dv],
                    start=(kc == 0), stop=(kc == 7)) for kc in range(8)],
                    reads=[Bwv, B_hT], writes=[Bps])
                S.op("dve", lambda e, ps=ps, tt=tt: e.tensor_copy(out=v_t[:, tt, :dv], in_=ps[:, :dv]),
                     reads=[Bps], parts=[Bv])
            if dv < 128:
                S.op("pool", lambda e: e.memset(v_t[:, :, dv:dv + 1], 1.0), parts=[Bv])
            dmax = TAB_DMAX[typ]
            items = []
            for qc in range(4):
                kts = [kt for kt in range(NTT) if any(0 <= 4 * qc + j - kt <= dmax for j in range(4))]
                lastkt = {}
                for kt in kts:
                    for j in range(4):
                        if 0 <= 4 * qc + j - kt <= dmax:
                            lastkt[j] = kt
                for idx, kt in enumerate(kts):
                    items.append((qc, kt, idx == len(kts) - 1, lastkt))
            first_in = {}
            slot = {}

            def front(i):
                qc, kt, _, _ = items[i]
                pi = ctr["sc"] % 2
                ctr["sc"] += 1
                slot[i] = pi
                ps, Bps = banks[pi]
                (pex, Bpex), (pm, Bpm) = pe_t[pi], pm_t[pi]
                S.mm_group([lambda pe: pe.matmul(
                    ps[:, :], lhsT=k_t[:dh, kt * 128:(kt + 1) * 128], rhs=q_t[:dh, qc * 512:(qc + 1) * 512],
                    start=True, stop=True)], reads=[Bq, Bk], writes=[Bps])
                S.op("act", lambda e: e.activation(out=pex[:], in_=ps[:], func=AF.Exp, scale=0.125),
                     reads=[Bps], writes=[Bpex])
                d0 = 4 * qc - kt
                S.op("dve", lambda e: e.tensor_tensor(
                    out=pm[:], in0=pex[:], in1=tb_t[:, (d0 + 3) * 128:(d0 + 3) * 128 + 512], op=ALU.mult),
                    reads=[Bpex, Btb], writes=[Bpm])

            def back(i):
                qc, kt, is_last, lastkt = items[i]
                accb = [banks[2], banks[3]] if qc % 2 == 0 else [banks[4], banks[5]]
                pm, Bpm = pm_t[slot[i]]
                mms = []
                for j in range(4):
                    dd_ = 4 * qc + j - kt
                    if not (0 <= dd_ <= dmax):
                        continue
                    bi = j // 2
                    ab, Bab = accb[bi]
                    st = first_in.get((qc, bi), True)
                    first_in[(qc, bi)] = False
                    c0 = (j % 2) * (dv + 1)
                    mms.append((bi, lambda pe, ab=ab, j=j, st=st, c0=c0: pe.matmul(
                        ab[:, c0:c0 + dv + 1], lhsT=pm[:, j * 128:(j + 1) * 128], rhs=v_t[:, kt, :dv + 1],
                        start=st, stop=(lastkt[j] == kt), skip_group_check=True)))
                for bi in (0, 1):
                    fl = [f for (b_, f) in mms if b_ == bi]
                    if fl:
                        S.mm_group(fl, reads=[Bpm, Bv], parts=[accb[bi][1]])
                if is_last:
                    fin(qc, accb, dv)

            front(0)
            for i in range(len(items)):
                if i + 1 < len(items):
                    front(i + 1)
                back(i)

        def fin_dil(first_s, last_s, pairslot, pair_idx):
            def f(qc, accb, dv):
                for j in range(4):
                    tt = 4 * qc + j
                    ab, Bab = accb[j // 2]
                    c0 = (j % 2) * 65
                    if first_s:
                        S.op("dve", lambda e, ab=ab, tt=tt, c0=c0: e.tensor_copy(out=acc_sb[:, tt, :65], in_=ab[:, c0:c0 + 65]),
                             reads=[Bab], parts=[B_acc])
                    else:
                        S.op("dve", lambda e, ab=ab, tt=tt, c0=c0: e.tensor_tensor(
                            out=acc_sb[:, tt, :65], in0=acc_sb[:, tt, :65], in1=ab[:, c0:c0 + 65], op=ALU.add),
                            reads=[Bab, B_acc], parts=[B_acc])
                    if last_s:
                        r, Br = rr[ctr["rr"] % 2]
                        ctr["rr"] += 1
                        S.op("dve", lambda e, r=r, tt=tt: e.reciprocal(out=r[:, 0:1], in_=acc_sb[:, tt, 64:65]),
                             reads=[B_acc], writes=[Br])
                        S.op("dve", lambda e, r=r, tt=tt: e.tensor_scalar(
                            out=oa2[:, tt, pairslot * 64:(pairslot + 1) * 64], in0=acc_sb[:, tt, :64],
                            scalar1=r[:, 0:1], scalar2=None, op0=ALU.mult),
                            reads=[Br, B_acc], parts=[B_oa2])
                        if pairslot == 1:
                            ps, Bps = banks[6]
                            psb = ps[:].bitcast(BF16)
                            S.mm_group([lambda pe, tt=tt: pe.transpose(out=psb[:, 0:128], in_=oa2[:, tt, :], identity=idb[:])],
                                       reads=[B_oa2, B_idb], writes=[Bps])
                            S.op("act", lambda e, tt=tt: e.activation(out=oaT[:, pair_idx, tt * 128:(tt + 1) * 128],
                                                                      in_=psb[:, 0:128], func=AF.Copy),
                                 reads=[Bps], parts=[B_oaT])
            return f

        def fin_diff(s, head):
            def f(qc, accb, dv):
                for j in range(4):
                    tt = 4 * qc + j
                    ab, Bab = accb[j // 2]
                    c0 = (j % 2) * 129
                    r, Br = rr[ctr["rr"] % 2]
                    ctr["rr"] += 1
                    S.op("dve", lambda e, r=r, ab=ab, c0=c0: e.reciprocal(out=r[:, 0:1], in_=ab[:, c0 + 128:c0 + 129]),
                         reads=[Bab], writes=[Br])
                    if s == 0:
                        S.op("dve", lambda e, r=r, ab=ab, c0=c0, tt=tt: e.tensor_scalar(
                            out=otmp[:, tt, :], in0=ab[:, c0:c0 + 128], scalar1=r[:, 0:1], scalar2=None, op0=ALU.mult),
                            reads=[Br, Bab], parts=[B_otmp])
                    else:
                        S.op("dve", lambda e, r=r: e.tensor_tensor(out=r[:, 1:2], in0=r[:, 0:1], in1=lam_t[:, 0:1], op=ALU.mult),
                             reads=[Br, B_lam], writes=[Br])
                        S.op("dve", lambda e, r=r, ab=ab, c0=c0, tt=tt: e.scalar_tensor_tensor(
                            out=otmp[:, tt, :], in0=ab[:, c0:c0 + 128], scalar=r[:, 1:2], in1=otmp[:, tt, :],
                            op0=ALU.mult, op1=ALU.add),
                            reads=[Br, Bab, B_otmp], parts=[B_otmp])
                        o_b, Bob = osb[ctr["os"] % 2]
                        ctr["os"] += 1
                        S.op("act", lambda e, o_b=o_b, r=r, tt=tt: e.activation(out=o_b[:], in_=otmp[:, tt, :], func=AF.Square,
                                                                                accum_out=r[:, 2:3]),
                             reads=[B_otmp], writes=[Bob, Br])
                        S.op("act", lambda e, r=r: e.activation(out=r[:, 3:4], in_=r[:, 2:3], func=AF.Sqrt, scale=1.0 / 128,
                                                                bias=EPS_T[:, 1:2]),
                             reads=[Br, B_eps], writes=[Br])
                        S.op("dve", lambda e, r=r: e.reciprocal(out=r[:, 3:4], in_=r[:, 3:4]), reads=[Br], writes=[Br])
                        S.op("dve", lambda e, o_b=o_b, r=r, tt=tt: e.scalar_tensor_tensor(
                            out=o_b[:], in0=otmp[:, tt, :], scalar=r[:, 3:4], in1=sg[:], op0=ALU.mult, op1=ALU.mult),
                            reads=[Br, B_otmp, Bsg], writes=[Bob])
                        ps, Bps = banks[6]
                        psb = ps[:].bitcast(BF16)
                        S.mm_group([lambda pe, o_b=o_b: pe.transpose(out=psb[:, 0:128], in_=o_b[:], identity=idb[:])],
                                   reads=[Bob, B_idb], writes=[Bps])
                        S.op("act", lambda e, tt=tt: e.activation(out=obT[:, head, tt * 128:(tt + 1) * 128],
                                                                  in_=psb[:, 0:128], func=AF.Copy),
                             reads=[Bps], parts=[B_obT])
            return f

        jobs = []
        for hh in range(8):
            for g in range(3):
                qcol = g * 512 + hh * 64
                jobs.append((qcol, 1536 + qcol, 3072 + qcol, 64, 64, g, hh, fin_dil(g == 0, g == 2, hh % 2, hh // 2)))
        for hd in range(8):
            for s in range(2):
                qcol = 4608 + hd * 128 + s * 64
                jobs.append((qcol, qcol + 1024, 6656 + hd * 128, 64, 128, 3, hd, fin_diff(s, hd)))
        stream1(0, *jobs[0])
        for ji, jb in enumerate(jobs):
            if ji + 1 < len(jobs):
                stream1((ji + 1) % 2, *jobs[ji + 1])
            stream2(ji % 2, *jb)
        S.barrier()
        release(m2)
        m3 = mark()
        G1, BG1 = sb("G1", [128, D], F32)
        S.dma("sp", G1[:], mod_d[:, 2 * D:3 * D], BG1, reads=[B_mod], writes=[BG1])
        wga, Bwga = sb("wga", [128, 8, 512], BF16)
        wgb, Bwgb = sb("wgb", [128, 8, 512], BF16)
        wpa, Bwpa = sb("wpa", [128, 4, 512], BF16)
        wpb, Bwpb = sb("wpb", [128, 8, 512], BF16)
        wo, Bwo = sb("wo", [128, 4, D], BF16)
        sga = [sb("sga%d" % i, [128, 512], F32) for i in range(2)]
        sgb = [sb("sgb%d" % i, [128, 512], F32) for i in range(2)]
        mg = [sb("mg%d" % i, [128, 512], BF16) for i in range(2)]
        mgT = [sb("mgT%d" % i, [128, 4, 128], BF16) for i in range(2)]
        wsrc = w_in[l].rearrange("(a p) n -> p a n", p=128)
        for nchk in range(2):
            n0 = nchk * 512
            S.dma("pool", wga[:], wsrc[:, :, 7680 + n0:7680 + n0 + 512], Bwga, writes=[Bwga])
            S.dma("pool", wgb[:], wsrc[:, :, 8704 + n0:8704 + n0 + 512], Bwgb, writes=[Bwgb])
            S.dma("pool", wpa[:], w_pa[l].rearrange("(a p) n -> p a n", p=128)[:, :, n0:n0 + 512], Bwpa, writes=[Bwpa])
            S.dma("pool", wpb[:], w_pb[l].rearrange("(a p) n -> p a n", p=128)[:, :, n0:n0 + 512], Bwpb, writes=[Bwpb])
            S.dma("pool", wo[:], w_o[l, n0:n0 + 512, :].rearrange("(a p) n -> p a n", p=128), Bwo, writes=[Bwo])
            for tt in range(NTT):
                i2 = tt % 2
                tsl = slice(tt * 128, (tt + 1) * 128)
                (pga, Bpga), (pgb, Bpgb), (ppa, Bppa), (ppb, Bppb) = banks[0], banks[1], banks[2], banks[3]
                S.mm_group([lambda pe, kc=kc: pe.matmul(pga[:], lhsT=hT[:, kc, tsl], rhs=wga[:, kc, :], start=(kc == 0), stop=(kc == 7))
                            for kc in range(8)], reads=[B_hT, Bwga], writes=[Bpga])
                S.mm_group([lambda pe, kc=kc: pe.matmul(pgb[:], lhsT=hT[:, kc, tsl], rhs=wgb[:, kc, :], start=(kc == 0), stop=(kc == 7))
                            for kc in range(8)], reads=[B_hT, Bwgb], writes=[Bpgb])
                S.mm_group([lambda pe, kc=kc: pe.matmul(ppa[:], lhsT=oaT[:, kc, tsl], rhs=wpa[:, kc, :], start=(kc == 0), stop=(kc == 3))
                            for kc in range(4)], reads=[B_oaT, Bwpa], writes=[Bppa])
                S.mm_group([lambda pe, kc=kc: pe.matmul(ppb[:], lhsT=obT[:, kc, tsl], rhs=wpb[:, kc, :], start=(kc == 0), stop=(kc == 7))
                            for kc in range(8)], reads=[B_obT, Bwpb], writes=[Bppb])
                (a_, Ba_), (b_, Bb_), (mg_, Bmg), (mgT_, BmgT) = sga[i2], sgb[i2], mg[i2], mgT[i2]
                S.op("act", lambda e, a_=a_: e.activation(out=a_[:], in_=pga[:], func=AF.Sigmoid), reads=[Bpga], writes=[Ba_])
                S.op("act", lambda e, b_=b_: e.activation(out=b_[:], in_=pgb[:], func=AF.Sigmoid), reads=[Bpgb], writes=[Bb_])
                S.op("dve", lambda e, a_=a_: e.tensor_tensor(out=a_[:], in0=a_[:], in1=ppa[:], op=ALU.mult), reads=[Ba_, Bppa], writes=[Ba_])
                S.op("dve", lambda e, b_=b_: e.tensor_tensor(out=b_[:], in0=b_[:], in1=ppb[:], op=ALU.mult), reads=[Bb_, Bppb], writes=[Bb_])
                S.op("pool", lambda e, a_=a_, b_=b_, mg_=mg_: e.tensor_tensor(out=mg_[:], in0=a_[:], in1=b_[:], op=ALU.add),
                     reads=[Ba_, Bb_], writes=[Bmg])
                ps, Bps = banks[6]
                psb = ps[:].bitcast(BF16)
                S.mm_group([lambda pe, c=c, mg_=mg_: pe.transpose(out=psb[:, c * 128:(c + 1) * 128], in_=mg_[:, c * 128:(c + 1) * 128],
                                                                  identity=idb[:]) for c in range(4)],
                           reads=[Bmg, B_idb], writes=[Bps])
                S.op("act", lambda e, mgT_=mgT_: e.activation(out=mgT_[:], in_=psb[:, 0:512].rearrange("p (a b) -> p a b", a=4), func=AF.Copy),
                     reads=[Bps], writes=[BmgT])
                for half in range(2):
                    po, Bpo = banks[4 + half]
                    S.mm_group([lambda pe, c=c, mgT_=mgT_, po=po, half=half: pe.matmul(
                        po[:], lhsT=mgT_[:, c, :], rhs=wo[:, c, half * 512:(half + 1) * 512], start=(c == 0), stop=(c == 3))
                        for c in range(4)], reads=[BmgT, Bwo], writes=[Bpo])
                    S.op("dve", lambda e, po=po, half=half, tt=tt: e.tensor_tensor(
                        out=po[:], in0=po[:], in1=G1[:, half * 512:(half + 1) * 512], op=ALU.mult),
                        reads=[Bpo, BG1], writes=[Bpo])
                    S.op("dve", lambda e, po=po, half=half, tt=tt: e.tensor_tensor(
                        out=x_t[:, tt, half * 512:(half + 1) * 512], in0=x_t[:, tt, half * 512:(half + 1) * 512], in1=po[:], op=ALU.add),
                        reads=[Bpo, B_xt[tt]], parts=[B_xt[tt]])
        S.barrier()
        release(m3)
        return m, hT


    def peer_prep_gen(l, bufs):
        ut_f, ut_b, v_f, v_b = bufs
        NB_ = len(ut_f)
        for et in range(128):
            i2 = et % NB_
            (uf, Buf_), (ub, Bub), (vf, Bvf), (vb_, Bvb) = ut_f[i2], ut_b[i2], v_f[i2], v_b[i2]
            S.dma("sp", uf[:], p_u[l, et * 128:(et + 1) * 128, :], Buf_, writes=[Buf_])
            S.dma("sp", vf[:], p_v[l, et * 128:(et + 1) * 128, :], Bvf, writes=[Bvf])
            for hb_ in range(2):
                ps, Bps = banks[2 + hb_]
                S.mm_group([lambda pe, c=c, ps=ps, uf=uf, hb_=hb_: pe.transpose(
                    out=ps[:, c * 128:(c + 1) * 128], in_=uf[:, (hb_ * 4 + c) * 128:(hb_ * 4 + c + 1) * 128], identity=idf[:])
                    for c in range(4)], reads=[Buf_, B_idf], writes=[Bps])
                S.op("act", lambda e, ps=ps, ub=ub, hb_=hb_: e.activation(
                    out=ub[:, hb_ * 4:(hb_ + 1) * 4, :], in_=ps[:].rearrange("p (a b) -> p a b", a=4), func=AF.Copy),
                    reads=[Bps], parts=[Bub])
            S.dma("sp", ut_d[:, et, :, :], ub[:], B_ut, reads=[Bub], parts=[B_ut])
            S.op("pool", lambda e, vb_=vb_, vf=vf: e.tensor_copy(out=vb_[:], in_=vf[:]), reads=[Bvf], writes=[Bvb])
            S.dma("sp", vb_d[:, et, :], vb_[:], B_vb, reads=[Bvb], parts=[B_vb])
            yield

    def peer_phase(l):
        m = mark()
        h2T, B_h2T = sb("h2T", [128, 8, T], BF16)
        G2, BG2 = sb("G2", [128, D], F32)
        S.dma("sp", G2[:], mod_d[:, 5 * D:6 * D], BG2, reads=[B_mod], writes=[BG2])
        mA = mark()
        pbufs = ([sb("utf%d" % i, [128, D], F32) for i in range(2)], [sb("utb%d" % i, [128, 8, 128], BF16) for i in range(2)],
                 [sb("vf%d" % i, [128, D], F32) for i in range(2)], [sb("vb%d" % i, [128, D], BF16) for i in range(2)])
        prep = peer_prep_gen(l, pbufs)
        A, BA, Bt, BB = make_AB(l, 2)
        tmp = norm_tmp()
        skT, B_skT = sb("skT", [128, 2, 128], F32)
        skl, B_skl = sb("skl", [128, 128], F32)
        for p in range(2):
            S.dma("sp", skl[:], p_sk[l, p, :, :], B_skl, writes=[B_skl])
            ps, Bps = banks[4]
            S.mm_group([lambda pe, ps=ps: pe.transpose(out=ps[:, 0:128], in_=skl[:], identity=idf[:])],
                       reads=[B_skl, B_idf], writes=[Bps])
            S.op("act", lambda e, ps=ps, p=p: e.activation(out=skT[:, p, :], in_=ps[:, 0:128], func=AF.Copy),
                 reads=[Bps], parts=[B_skT])
        wqb = [sb("wqb%d" % i, [128, 8, 128], BF16) for i in range(4)]
        qTs = [sb("qTs%d" % i, [128, 128], F32) for i in range(2)]
        sc_l = [sb("sc%d" % i, [128, 16, 128], F32) for i in range(2)]
        sc2, B_sc2 = sb("sc2", [128, 16, 128], F32)
        T16, B_T16 = sb("T16", [128, 16, 16], F32)
        cand, B_cand = sb("cand", [128, 8, 256], F32)
        S24, B_S24 = sb("S24", [128, 8, 24], F32)
        sm, B_sm = sb("sm", [128, 8, 16], F32)
        sv_ = {}
        for nm in ["negM", "tau", "nh", "Z", "e1"]:
            sv_[nm] = sb(nm, [128, 8], F32)
        kap_l = [sb("kapA%d" % i, [128, 8], F32) for i in range(2)]
        wsrc = p_wq[l].rearrange("(a p) n -> p a n", p=128)
        cand2v = sc2[:].rearrange("p (g two) k -> p g (two k)", two=2)
        cn = {"w": 0}
        NG = 8
        def hploop(tt):
            for _ in range(8):
                next(prep, None)
            norm_tile(tt, A, BA, Bt, BB, h2T, B_h2T, tt * 128, tmp, banks[7])
            sc, B_sc = sc_l[tt % 2]
            st = {}

            def hp_front(hp):
                i2 = cn["w"] % 2
                (wb, Bwb), (qs, Bqs) = wqb[cn["w"] % 4], qTs[i2]
                cn["w"] += 1
                st[hp] = (qs, Bqs)
                S.dma("pool", wb[:], wsrc[:, :, hp * 128:(hp + 1) * 128], Bwb, writes=[Bwb])
                ps, Bps = banks[4 + i2]
                S.mm_group([lambda pe, kc=kc: pe.matmul(ps[:, :128], lhsT=wb[:, kc, :], rhs=h2T[:, kc, tt * 128:(tt + 1) * 128],
                                                        start=(kc == 0), stop=(kc == 7)) for kc in range(8)],
                           reads=[Bwb, B_h2T], writes=[Bps])
                S.op("act", lambda e: e.activation(out=qs[:], in_=ps[:, :128], func=AF.Copy), reads=[Bps], writes=[Bqs])

            def hp_back(hp):
                qs, Bqs = st[hp]
                ps2, Bps2 = banks[6 + (hp % 2)]
                S.mm_group([lambda pe: pe.matmul(ps2[:, 0:128], lhsT=qs[:], rhs=skT[:, hp % 2, :], start=True, stop=True)],
                           reads=[Bqs, B_skT], writes=[Bps2])
                S.op("act", lambda e: e.activation(out=sc[:, hp, :], in_=ps2[:, 0:128], func=AF.Copy),
                     reads=[Bps2], parts=[B_sc])

            hp_front(0)
            for hp in range(16):
                if hp + 1 < 16:
                    hp_front(hp + 1)
                hp_back(hp)

        def topk(tt):
            sc, B_sc = sc_l[tt % 2]
            kap, Bkap = kap_l[tt % 2]
            for g in range(16):
                S.op("dve", lambda e, g=g: e.max(out=T16[:, g, 0:8], in_=sc[:, g, :]), reads=[B_sc], parts=[B_T16])
            for g in range(16):
                S.op("dve", lambda e, g=g: e.match_replace(out=sc2[:, g, :], in_to_replace=T16[:, g, 0:8], in_values=sc[:, g, :], imm_value=-1e30),
                     reads=[B_sc, B_T16], parts=[B_sc2])
            for g in range(16):
                S.op("dve", lambda e, g=g: e.max(out=T16[:, g, 8:16], in_=sc2[:, g, :]), reads=[B_sc2], parts=[B_T16])
            T16v = T16[:].rearrange("p (g two) r -> p g two r", two=2)
            S.op("dve", lambda e: e.tensor_tensor(out=cand[:].rearrange("p g (r s) -> p g r s", r=16),
                                                  in0=T16v[:, :, 0, :].unsqueeze(3).to_broadcast([128, NG, 16, 16]),
                                                  in1=T16v[:, :, 1, :].unsqueeze(2).to_broadcast([128, NG, 16, 16]), op=ALU.add),
                 reads=[B_T16], writes=[B_cand])
            for g in range(NG):
                S.op("dve", lambda e, g=g: e.max(out=S24[:, g, 0:8], in_=cand[:, g, :]), reads=[B_cand], parts=[B_S24])
            for g in range(NG):
                S.op("dve", lambda e, g=g: e.match_replace(out=cand2v[:, g, :], in_to_replace=S24[:, g, 0:8], in_values=cand[:, g, :], imm_value=-1e30),
                     reads=[B_cand, B_S24, B_sc2], parts=[B_sc2])
            for g in range(NG):
                S.op("dve", lambda e, g=g: e.max(out=S24[:, g, 8:16], in_=cand2v[:, g, :]), reads=[B_sc2], parts=[B_S24])
            for g in range(NG):
                S.op("dve", lambda e, g=g: e.match_replace(out=cand2v[:, g, :], in_to_replace=S24[:, g, 8:16], in_values=cand2v[:, g, :], imm_value=-1e30),
                     reads=[B_sc2, B_S24], parts=[B_sc2])
            for g in range(NG):
                S.op("dve", lambda e, g=g: e.max(out=S24[:, g, 16:24], in_=cand2v[:, g, :]), reads=[B_sc2], parts=[B_S24])
            (negM, BnegM), (tau, Btau), (nh, Bnh), (Z, BZ), (e1, Be1) = [sv_[n] for n in ["negM", "tau", "nh", "Z", "e1"]]
            S.op("dve", lambda e: e.tensor_scalar(out=negM[:], in0=S24[:, :, 0], scalar1=-1.0, scalar2=None, op0=ALU.mult),
                 reads=[B_S24], writes=[BnegM])
            S.op("dve", lambda e: e.tensor_tensor(out=tau[:], in0=S24[:, :, 15], in1=S24[:, :, 16], op=ALU.add), reads=[B_S24], writes=[Btau])
            S.op("dve", lambda e: e.tensor_scalar(out=tau[:], in0=tau[:], scalar1=0.5, scalar2=None, op0=ALU.mult), reads=[Btau], writes=[Btau])
            S.op("dve", lambda e: e.tensor_scalar(out=nh[:], in0=tau[:], scalar1=-0.5, scalar2=None, op0=ALU.mult), reads=[Btau], writes=[Bnh])
            S.op("dve", lambda e: e.tensor_tensor(out=sm[:], in0=S24[:, :, 0:16], in1=negM[:].unsqueeze(2).to_broadcast([128, NG, 16]), op=ALU.add),
                 reads=[B_S24, BnegM], writes=[B_sm])
            S.op("act", lambda e: e.activation(out=sm[:], in_=sm[:], func=AF.Exp), reads=[B_sm], writes=[B_sm])
            S.op("dve", lambda e: e.reduce_sum(out=Z[:], in_=sm[:], axis=AX.X), reads=[B_sm], writes=[BZ])
            S.op("dve", lambda e: e.tensor_tensor(out=e1[:], in0=tau[:], in1=negM[:], op=ALU.add), reads=[Btau, BnegM], writes=[Be1])
            S.op("act", lambda e: e.activation(out=e1[:], in_=e1[:], func=AF.Exp), reads=[Be1], writes=[Be1])
            S.op("dve", lambda e: e.reciprocal(out=Z[:], in_=Z[:]), reads=[BZ], writes=[BZ])
            S.op("dve", lambda e, kap=kap: e.tensor_tensor(out=kap[:], in0=e1[:], in1=Z[:], op=ALU.mult), reads=[Be1, BZ], writes=[Bkap])
            scv = sc[:].rearrange("p (g two) k -> p g (two k)", two=2)
            S.op("dve", lambda e, scv=scv: e.tensor_tensor(out=scv, in0=scv, in1=nh[:].unsqueeze(2).to_broadcast([128, NG, 256]), op=ALU.add),
                 reads=[B_sc, Bnh], writes=[B_sc])
            S.op("act", lambda e, sc=sc: e.activation(out=sc[:], in_=sc[:], func=AF.Exp), reads=[B_sc], writes=[B_sc])
            S.op("dve", lambda e, scv=scv, kap=kap: e.tensor_tensor(out=scv[:, :, 0:128], in0=scv[:, :, 0:128],
                                                                  in1=kap[:].unsqueeze(2).to_broadcast([128, NG, 128]), op=ALU.mult),
                 reads=[B_sc, Bkap], writes=[B_sc])
            S.dma("act", scx_d[tt], scv, B_scx, reads=[B_sc], parts=[B_scx])
            S.dma("act", kap_d[tt], kap[:], B_kapd, reads=[Bkap], parts=[B_kapd])

        hploop(0)
        for tt in range(NTT):
            if tt + 1 < NTT:
                hploop(tt + 1)
            topk(tt)
        for _ in prep:
            pass
        S.barrier()
        release(mA)
        NT = 2
        NBLK = NTT // NT
        TB = NT * 128
        NGB = NT * 8
        IC = 4
        scx, B_scxs = sb("scx", [128, NGB, 256], F32)
        kapB, B_kapB = sb("kapB", [128, NGB], F32)
        Ft = [[sb("F%d_%d" % (bf, g), [128, IC * 128], BF16) for g in range(NGB)] for bf in range(2)]
        u_pool = [sb("up%d" % i, [128, IC * 128], F32) for i in range(3)]
        u_act = [sb("ua%d" % i, [128, IC * 128], F32) for i in range(3)]
        utc = [sb("utc%d" % i, [128, IC, 8, 128], BF16) for i in range(2)]
        vbc = [sb("vbc%d" % i, [128, IC, D], BF16) for i in range(2)]
        gl_t = [sb("gl%d" % i, [128, TB], F32) for i in range(2)]
        ga_t = [sb("ga%d" % i, [128, TB], BF16) for i in range(2)]
        xo, Bxo = sb("xo", [128, 512], F32)
        accO = [banks[0], banks[1], banks[2], banks[3]]
        cu = {"p": 0, "a": 0}
        NCH = 128 // IC
        for blk in range(NBLK):
            for j in range(NT):
                S.dma("sp", scx[:, j * 8:(j + 1) * 8, :], scx_d[blk * NT + j], B_scxs, reads=[B_scx],
                      **({"writes": [B_scxs]} if j == 0 else {"parts": [B_scxs]}))
                S.dma("sp", kapB[:, j * 8:(j + 1) * 8], kap_d[blk * NT + j], B_kapB, reads=[B_kapd],
                      **({"writes": [B_kapB]} if j == 0 else {"parts": [B_kapB]}))
            hcols = slice(blk * TB, (blk + 1) * TB)

            def load_chunk(ic):
                bf = ic % 2
                (uc, Buc) = utc[bf]
                S.dma("sp", uc[:], ut_d[:, ic * IC:(ic + 1) * IC, :, :], Buc, reads=[B_ut], writes=[Buc])

            def load_chunk_v(ic):
                bf = ic % 2
                (vc, Bvc) = vbc[bf]
                S.dma("sp", vc[:], vb_d[:, ic * IC:(ic + 1) * IC, :], Bvc, reads=[B_vb], writes=[Bvc])

            def build_quarter(ic, q):
                bf = ic % 2
                for g in range(q * 4, q * 4 + 4):
                    Fg, BFg = Ft[bf][g]
                    if g % 2 == 0:
                        u_, Bu_ = u_pool[cu["p"] % 3]
                        cu["p"] += 1
                        S.op("pool", lambda e, u_=u_, g=g, ic=ic: e.tensor_tensor(
                            out=u_[:].rearrange("p (i j) -> p i j", i=IC),
                            in0=scx[:, g, ic * IC:(ic + 1) * IC].unsqueeze(2).to_broadcast([128, IC, 128]),
                            in1=scx[:, g, 128:256].unsqueeze(1).to_broadcast([128, IC, 128]), op=ALU.mult),
                            reads=[B_scxs], writes=[Bu_])
                    else:
                        u_, Bu_ = u_act[cu["a"] % 3]
                        cu["a"] += 1
                        for i in range(IC):
                            S.op("act", lambda e, u_=u_, g=g, ic=ic, i=i: e.activation(
                                out=u_[:, i * 128:(i + 1) * 128], in_=scx[:, g, 128:256], func=AF.Copy,
                                scale=scx[:, g, ic * IC + i:ic * IC + i + 1]),
                                reads=[B_scxs], **({"writes": [Bu_]} if i == 0 else {"parts": [Bu_]}))
                    S.op("dve", lambda e, u_=u_, Fg=Fg, g=g: e.scalar_tensor_tensor(
                        out=Fg[:], in0=u_[:], scalar=kapB[:, g:g + 1], in1=u_[:], op0=ALU.is_ge, op1=ALU.mult),
                        reads=[Bu_, B_kapB], writes=[BFg])

            def emit_out(et):
                ic, il = et // IC, et % IC
                vc, Bvc = vbc[ic % 2]
                ga, Bga = ga_t[et % 2]
                for j in range(NT):
                    for half in range(2):
                        ab, Bab = accO[j * 2 + half]
                        S.mm_group([lambda pe, j=j, half=half, ab=ab, ga=ga, vc=vc, il=il, et=et: pe.matmul(
                            ab[:], lhsT=ga[:, j * 128:(j + 1) * 128], rhs=vc[:, il, half * 512:(half + 1) * 512],
                            start=(et == 0), stop=(et == 127), skip_group_check=True)],
                            reads=[Bga, Bvc], **({"writes": [Bab]} if et == 0 else {"parts": [Bab]}))

            load_chunk(0)
            load_chunk_v(0)
            for q in range(4):
                build_quarter(0, q)
            for et in range(128):
                ic, il = et // IC, et % IC
                bf = ic % 2
                if il == 0 and ic + 1 < NCH:
                    load_chunk(ic + 1)
                if ic + 1 < NCH:
                    build_quarter(ic + 1, il)
                uc, Buc = utc[bf]
                hbk, Bhbk = banks[4 + et % 2]
                gbk, Bgbk = banks[6 + et % 2]
                S.mm_group([lambda pe, kc=kc, il=il, hbk=hbk, uc=uc: pe.matmul(hbk[:, :TB], lhsT=uc[:, il, kc, :], rhs=h2T[:, kc, hcols],
                                                                             start=(kc == 0), stop=(kc == 7)) for kc in range(8)],
                           reads=[Buc, B_h2T], writes=[Bhbk])
                S.mm_group([lambda pe, j=j, h=h, il=il, gbk=gbk, bf=bf: pe.matmul(
                    gbk[:, j * 128:(j + 1) * 128], lhsT=Ft[bf][j * 8 + h][0][:, il * 128:(il + 1) * 128], rhs=idb[:],
                    start=(j == 0 and h == 0), stop=(h == 7), skip_group_check=True) for j in range(NT) for h in range(8)],
                    reads=[Ft[bf][g][1] for g in range(NGB)] + [B_idb], writes=[Bgbk])
                (gl, Bgl), (ga, Bga) = gl_t[et % 2], ga_t[et % 2]
                S.op("act", lambda e, gl=gl, hbk=hbk: e.activation(out=gl[:], in_=hbk[:, :TB], func=AF.Gelu_apprx_tanh),
                     reads=[Bhbk], writes=[Bgl])
                S.op("dve", lambda e, gl=gl, ga=ga, gbk=gbk: e.tensor_tensor(out=ga[:], in0=gl[:], in1=gbk[:, :TB], op=ALU.mult),
                     reads=[Bgl, Bgbk], writes=[Bga])
                if et > 0:
                    emit_out(et - 1)
                if il == 0 and ic + 1 < NCH:
                    load_chunk_v(ic + 1)
            emit_out(127)
            for j in range(NT):
                tt = blk * NT + j
                for half in range(2):
                    ab, Bab = accO[j * 2 + half]
                    S.op("dve", lambda e, ab=ab, half=half: e.tensor_tensor(out=xo[:], in0=ab[:], in1=G2[:, half * 512:(half + 1) * 512], op=ALU.mult),
                         reads=[Bab, BG2], writes=[Bxo])
                    S.op("dve", lambda e, half=half, tt=tt: e.tensor_tensor(
                        out=x_t[:, tt, half * 512:(half + 1) * 512], in0=x_t[:, tt, half * 512:(half + 1) * 512], in1=xo[:], op=ALU.add),
                        reads=[Bxo, B_xt[tt]], parts=[B_xt[tt]])
        S.barrier()
        release(m)

    def final_phase():
        m = mark()
        fg, Bfg = load_bc("fg_bc", fin_g.rearrange("(o n) -> o n", o=1), D)
        tmp = norm_tmp()
        (sq, Bsq), (ss, Bss), (rs, Brs), (hn, Bhn), (hb, Bhb) = tmp
        for tt in range(NTT):
            norm_tile(tt, fg, Bfg, None, None, None, None, 0, tmp, None)
            S.dma("sp", y_d[tt * 128:(tt + 1) * 128, :], hn[:], B_y, reads=[Bhn], parts=[B_y])
        S.barrier()
        release(m)

    def dump_x():
        for tt in range(NTT):
            S.dma("sp", y_d[tt * 128:(tt + 1) * 128, :], x_t[:, tt, :], B_y, reads=[B_xt[tt]], parts=[B_y])
        S.barrier()

    for l in range(l0, l1):
        mod_phase(l)
        if stop_after == "mod":
            S.dma("sp", y_d[0:128, :].rearrange("p (a n) -> p a n", a=1)[:, 0, :], x_t[:, 0, :], B_y, reads=[B_xt[0]], parts=[B_y])
            break
        mm, hT = mixer_phase(l)
        if stop_after == "h1":
            hf, Bhf = sb("hf", [128, 8, 128], F32)
            break
        release(mm)
        if stop_after == "mix":
            break
        peer_phase(l)
    if stop_after is None and last:
        final_phase()
    else:
        dump_x()
    S.finish()
    return nc


def t5_bucket_np(d):
    d = np.maximum(d, 0)
    ratio = np.log(np.maximum(d, 1).astype(np.float32) / np.float32(16)) / np.float32(math.log(2048 / 16))
    large = np.minimum(16 + (ratio * np.float32(16)).astype(np.int32), 31)
    return np.where(d < 16, d, large)


def make_btab(rel_bias):
    ext = np.concatenate([rel_bias.astype(np.float32), np.full((1, 32), -30000.0, np.float32)], axis=0)
    out = np.empty((128, TABCOLS), np.float32)
    k = np.arange(128)[:, None, None]
    q = np.arange(128)[None, None, :]
    for typ in range(4):
        nb = TAB_NB[typ]
        Dd = (np.arange(nb) - 3)[None, :, None]
        delta = 128 * Dd + q - k
        valid = delta >= 0
        if TAB_W[typ] is not None:
            r = TAB_R[typ]
            valid = valid & (delta % r == 0) & (delta // r <= 128)
        idx = np.where(valid, t5_bucket_np(delta), 32)
        for h in range(8):
            hg = typ * 8 + h
            o = tab_off(typ, h)
            out[:, o:o + nb * 128] = ext[idx, hg].reshape(128, nb * 128)
    return out


_CACHE = {}


def run_layers(inputs, xin, l0, l1, first, last, stop_after=None, cores=8, with_peer=True):
    nc = build(l0, l1, first, last, stop_after, with_peer)
    btab = make_btab(np.asarray(inputs["rel_bias"]))
    ident = np.eye(128, dtype=np.float32)
    shared = {k: np.ascontiguousarray(np.asarray(inputs[k], dtype=np.float32)) for k in
              ["w_ada", "b_ada", "norm1_g", "norm2_g", "w_in", "w_proj_a", "w_proj_b", "w_out", "lam_q1", "lam_k1",
               "lam_q2", "lam_k2", "subln_g", "peer_wq", "peer_subkeys", "peer_u", "peer_v", "final_g"]}
    if not with_peer:
        del shared["peer_u"], shared["peer_v"]
    shared["btab"] = btab
    shared["ident"] = ident
    c = np.asarray(inputs["c"], dtype=np.float32)
    in_maps = []
    for b in range(cores):
        mp = dict(shared)
        mp["x"] = np.ascontiguousarray(xin[b])
        mp["c"] = np.ascontiguousarray(c[b].reshape(8, 128).T)
        in_maps.append(mp)
    res = run_bass_kernel_spmd(nc, in_maps, core_ids=list(range(cores)))
    return np.stack([np.asarray(r["y"]) for r in res.results], axis=0)


def kernel(**inputs):
    x = np.asarray(inputs["x"], dtype=np.float32)
    out = run_layers(inputs, x, 0, DEPTH, True, True)
    return out.astype(np.float32)
```
